# Optimizing a Trainium2 kernel written in Bass

```python
import math
import jax, jax.numpy as jnp
from jax import lax
import numpy as np

D_MODEL = 1024
BATCH = 4
SEQ = 8192
DEPTH = 2

D_MIX = D_MODEL
M_HEADS = 4
M_HEAD_DIM = D_MIX // 16
M_WIDTH = M_HEADS * M_HEAD_DIM
M_CHUNK = 64
M_QK_CONV = 4
C_WIDTH = D_MIX // 4
C_KERNEL = 31
A_HEADS = 4
A_HEAD_DIM = D_MIX // 16
A_WIDTH = A_HEADS * 2 * A_HEAD_DIM
Q_BLOCK = 128
SPLIT_SIZES = (M_WIDTH, M_WIDTH, M_WIDTH, M_WIDTH, M_HEADS, M_HEADS,
               C_WIDTH, C_WIDTH, A_WIDTH, A_WIDTH, A_WIDTH)
D_IN = 4 * M_WIDTH + 2 * M_HEADS + 2 * C_WIDTH + 3 * A_WIDTH
FF_DENSE = ((8 * D_MODEL // 3 + 127) // 128) * 128
N_EXPERTS = 8
TOP_K = 2
FF_EXPERT = 7 * D_MODEL // 2
N_DENSE = (DEPTH + 1) // 2
N_MOE = DEPTH // 2
PLE_DIM = 256
EPS = 1e-6

kernel_name = "hybrid_mlstm_conformer_diffattn_moe_trunk"


def _rmsnorm(x, g):
    xf = x.astype(jnp.float32)
    return xf * lax.rsqrt(jnp.mean(xf * xf, axis=-1, keepdims=True) + EPS) * g.astype(jnp.float32)


def _layernorm(x, g, b):
    xf = x.astype(jnp.float32)
    mu = jnp.mean(xf, axis=-1, keepdims=True)
    var = jnp.mean(jnp.square(xf - mu), axis=-1, keepdims=True)
    return (xf - mu) * lax.rsqrt(var + EPS) * g.astype(jnp.float32) + b.astype(jnp.float32)


def _causal_depthwise_conv(x, w):
    k = w.shape[0]
    return lax.conv_general_dilated(
        x.astype(jnp.float32), w.astype(jnp.float32)[:, None, :],
        window_strides=(1,), padding=[(k - 1, 0)],
        dimension_numbers=('NWC', 'WIO', 'NWC'), feature_group_count=x.shape[-1])


def _mlstm_chunkwise(q, k, v, i_pre, f_pre):
    b, s, h, dh = q.shape
    nc = s // M_CHUNK
    q = q.astype(jnp.float32).reshape(b, nc, M_CHUNK, h, dh) * dh ** -0.5
    k = k.astype(jnp.float32).reshape(b, nc, M_CHUNK, h, dh)
    v = v.astype(jnp.float32).reshape(b, nc, M_CHUNK, h, dh)
    logf = jax.nn.log_sigmoid(f_pre.astype(jnp.float32)).reshape(b, nc, M_CHUNK, h)
    ig = i_pre.astype(jnp.float32).reshape(b, nc, M_CHUNK, h)
    bcum = jnp.cumsum(logf, axis=2)
    g = bcum[:, :, -1]
    w_loc = g[:, :, None] - bcum + ig
    m_loc = jnp.max(w_loc, axis=2)
    e_loc = jnp.exp(w_loc - m_loc[:, :, None])
    c_loc = jnp.einsum('bclh,bclhd,bclhe->bchde', e_loc, k, v)
    n_loc = jnp.einsum('bclh,bclhd->bchd', e_loc, k)

    def step(carry, inp):
        c_st, n_st, m_st = carry
        cl, nl, ml, gc = inp
        m_new = jnp.maximum(gc + m_st, ml)
        a = jnp.exp(gc + m_st - m_new)
        bb = jnp.exp(ml - m_new)
        c_new = a[..., None, None] * c_st + bb[..., None, None] * cl
        n_new = a[..., None] * n_st + bb[..., None] * nl
        return (c_new, n_new, m_new), (c_st, n_st, m_st)

    init = (jnp.zeros((b, h, dh, dh), jnp.float32), jnp.zeros((b, h, dh), jnp.float32),
            jnp.zeros((b, h), jnp.float32))
    xs = (jnp.moveaxis(c_loc, 1, 0), jnp.moveaxis(n_loc, 1, 0), jnp.moveaxis(m_loc, 1, 0), jnp.moveaxis(g, 1, 0))
    _, (c_prev, n_prev, m_prev) = lax.scan(step, init, xs)
    c_prev = jnp.moveaxis(c_prev, 0, 1)
    n_prev = jnp.moveaxis(n_prev, 0, 1)
    m_prev = jnp.moveaxis(m_prev, 0, 1)

    bt = jnp.moveaxis(bcum, -1, 2)
    it = jnp.moveaxis(ig, -1, 2)
    causal = jnp.tril(jnp.ones((M_CHUNK, M_CHUNK), dtype=bool))
    dmat = jnp.where(causal, bt[..., :, None] - bt[..., None, :] + it[..., None, :], -jnp.inf)
    m_inter = bt + m_prev[..., None]
    m_out = jnp.maximum(m_inter, jnp.max(dmat, axis=-1))
    wts = jnp.exp(dmat - m_out[..., None]) * jnp.einsum('bclhd,bcshd->bchls', q, k)
    inter_scale = jnp.exp(m_inter - m_out)
    num = (jnp.einsum('bchls,bcshe->bchle', wts, v)
           + inter_scale[..., None] * jnp.einsum('bclhd,bchde->bchle', q, c_prev))
    den = jnp.sum(wts, axis=-1) + inter_scale * jnp.einsum('bclhd,bchd->bchl', q, n_prev)
    out = num / jnp.maximum(jnp.abs(den), jnp.exp(-m_out))[..., None]
    return jnp.moveaxis(out, 2, 3).reshape(b, s, h, dh)


def _diff_attention(q, k, v, lam):
    b, s, _, dh = q.shape
    slopes = 2.0 ** (-8.0 * jnp.arange(1, A_HEADS + 1, dtype=jnp.float32) / A_HEADS)
    outs = []
    for blk in range(s // Q_BLOCK):
        s0 = blk * Q_BLOCK
        e = s0 + Q_BLOCK
        logits = jnp.einsum('bqhd,bkhd->bhqk', q[:, s0:e], k[:, :e]) * dh ** -0.5
        logits = logits.reshape(b, A_HEADS, 2, Q_BLOCK, e)
        dist = (jnp.arange(s0, e)[:, None] - jnp.arange(e)[None, :]).astype(jnp.float32)
        bias = jnp.where(dist >= 0, -slopes[:, None, None, None] * dist, -jnp.inf)
        probs = jax.nn.softmax(logits + bias, axis=-1)
        attn = probs[:, :, 0] - lam * probs[:, :, 1]
        outs.append(jnp.einsum('bhqk,bkhe->bqhe', attn, v[:, :e].astype(jnp.float32)))
    return jnp.concatenate(outs, axis=1)


def _swiglu(x, wg, wu, wd):
    return (jax.nn.silu(x @ wg) * (x @ wu)) @ wd


def _moe_swiglu(x, router_w, wg, wu, wd):
    b, s, d = x.shape
    xt = x.reshape(b * s, d)
    logits = (xt @ router_w).astype(jnp.float32)
    top_val, top_idx = lax.top_k(logits, TOP_K)
    gates = jax.nn.softmax(top_val, axis=-1)
    combine = jnp.einsum('nk,nke->ne', gates, jax.nn.one_hot(top_idx, N_EXPERTS, dtype=jnp.float32))
    out = jnp.zeros((b * s, d), jnp.float32)
    for e in range(N_EXPERTS):
        out = out + combine[:, e:e + 1] * _swiglu(xt, wg[e], wu[e], wd[e])
    return out.reshape(b, s, d)


def setup_inputs(seed: int = 0) -> dict:
    key = jax.random.key(seed)
    ks = jax.random.split(key, 40)
    f32 = jnp.float32

    def nrm(k, shape, scale):
        return jax.random.normal(k, shape, f32) * scale

    def gain(k, shape):
        return 1.0 + 0.02 * jax.random.normal(k, shape, f32)

    return {
        'x': nrm(ks[0], (BATCH, SEQ, D_MODEL), 1.0),
        'p': nrm(ks[1], (DEPTH, BATCH, SEQ, PLE_DIM), 1.0),
        'mix_norm_g': gain(ks[2], (DEPTH, D_MODEL)),
        'w_in': nrm(ks[3], (DEPTH, D_MODEL, D_IN), D_MODEL ** -0.5),
        'b_igate': nrm(ks[4], (DEPTH, M_HEADS), 0.1),
        'b_fgate': jnp.linspace(3.0, 6.0, M_HEADS, dtype=f32)[None, :] + nrm(ks[5], (DEPTH, M_HEADS), 0.1),
        'm_qk_conv_w': nrm(ks[6], (DEPTH, M_QK_CONV, 2 * M_WIDTH), M_QK_CONV ** -0.5),
        'm_out_norm_g': gain(ks[7], (DEPTH, M_WIDTH)),
        'c_conv_w': nrm(ks[8], (DEPTH, C_KERNEL, C_WIDTH), C_KERNEL ** -0.5),
        'c_conv_b': nrm(ks[9], (DEPTH, C_WIDTH), 0.02),
        'c_ln_g': gain(ks[10], (DEPTH, C_WIDTH)),
        'c_ln_b': nrm(ks[11], (DEPTH, C_WIDTH), 0.02),
        'a_q_norm_g': gain(ks[12], (DEPTH, A_HEAD_DIM)),
        'a_k_norm_g': gain(ks[13], (DEPTH, A_HEAD_DIM)),
        'a_lambda_q1': nrm(ks[14], (DEPTH, A_HEAD_DIM), 0.1),
        'a_lambda_k1': nrm(ks[15], (DEPTH, A_HEAD_DIM), 0.1),
        'a_lambda_q2': nrm(ks[16], (DEPTH, A_HEAD_DIM), 0.1),
        'a_lambda_k2': nrm(ks[17], (DEPTH, A_HEAD_DIM), 0.1),
        'a_subln_g': gain(ks[18], (DEPTH, 2 * A_HEAD_DIM)),
        'w_out': nrm(ks[19], (DEPTH, D_MIX, D_MODEL), D_MIX ** -0.5),
        'ffn_norm_g': gain(ks[20], (DEPTH, D_MODEL)),
        'dense_w_gate': nrm(ks[21], (N_DENSE, D_MODEL, FF_DENSE), D_MODEL ** -0.5),
        'dense_w_up': nrm(ks[22], (N_DENSE, D_MODEL, FF_DENSE), D_MODEL ** -0.5),
        'dense_w_down': nrm(ks[23], (N_DENSE, FF_DENSE, D_MODEL), FF_DENSE ** -0.5),
        'router_w': nrm(ks[24], (N_MOE, D_MODEL, N_EXPERTS), D_MODEL ** -0.5),
        'moe_w_gate': nrm(ks[25], (N_MOE, N_EXPERTS, D_MODEL, FF_EXPERT), D_MODEL ** -0.5),
        'moe_w_up': nrm(ks[26], (N_MOE, N_EXPERTS, D_MODEL, FF_EXPERT), D_MODEL ** -0.5),
        'moe_w_down': nrm(ks[27], (N_MOE, N_EXPERTS, FF_EXPERT, D_MODEL), FF_EXPERT ** -0.5),
        'ple_norm_g': gain(ks[28], (DEPTH, D_MODEL)),
        'w_ple_gate': nrm(ks[29], (DEPTH, D_MODEL, D_MODEL), D_MODEL ** -0.5),
        'w_ple_proj': nrm(ks[30], (DEPTH, PLE_DIM, D_MODEL), PLE_DIM ** -0.5),
    }


def reference(x, p, mix_norm_g, w_in, b_igate, b_fgate, m_qk_conv_w, m_out_norm_g,
              c_conv_w, c_conv_b, c_ln_g, c_ln_b, a_q_norm_g, a_k_norm_g,
              a_lambda_q1, a_lambda_k1, a_lambda_q2, a_lambda_k2, a_subln_g, w_out,
              ffn_norm_g, dense_w_gate, dense_w_up, dense_w_down, router_w,
              moe_w_gate, moe_w_up, moe_w_down, ple_norm_g, w_ple_gate, w_ple_proj):
    b, s, _ = x.shape
    split_idx = np.cumsum(SPLIT_SIZES)[:-1].tolist()
    h = x.astype(jnp.float32)
    for layer in range(DEPTH):
        a = _rmsnorm(h, mix_norm_g[layer])
        u = a @ w_in[layer]
        mq, mk, mv, mo, mi, mf, ca, cg, aq, ak, av = jnp.split(u, split_idx, axis=-1)

        qk = jax.nn.silu(_causal_depthwise_conv(jnp.concatenate([mq, mk], axis=-1), m_qk_conv_w[layer]))
        mq, mk = jnp.split(qk, 2, axis=-1)
        hm = _mlstm_chunkwise(mq.reshape(b, s, M_HEADS, M_HEAD_DIM), mk.reshape(b, s, M_HEADS, M_HEAD_DIM),
                              mv.reshape(b, s, M_HEADS, M_HEAD_DIM),
                              mi + b_igate[layer], mf + b_fgate[layer])
        hm = _rmsnorm(hm, m_out_norm_g[layer].reshape(M_HEADS, M_HEAD_DIM))
        y_m = (hm * jax.nn.sigmoid(mo.astype(jnp.float32)).reshape(b, s, M_HEADS, M_HEAD_DIM)).reshape(b, s, M_WIDTH)

        z = ca * jax.nn.sigmoid(cg)
        z = _causal_depthwise_conv(z, c_conv_w[layer]) + c_conv_b[layer]
        y_c = jax.nn.silu(_layernorm(z, c_ln_g[layer], c_ln_b[layer]))

        lam_init = 0.8 - 0.6 * math.exp(-0.3 * layer)
        lam = (jnp.exp(jnp.sum(a_lambda_q1[layer].astype(jnp.float32) * a_lambda_k1[layer].astype(jnp.float32)))
               - jnp.exp(jnp.sum(a_lambda_q2[layer].astype(jnp.float32) * a_lambda_k2[layer].astype(jnp.float32)))
               + lam_init)
        qa = _rmsnorm(aq.reshape(b, s, 2 * A_HEADS, A_HEAD_DIM), a_q_norm_g[layer])
        ka = _rmsnorm(ak.reshape(b, s, 2 * A_HEADS, A_HEAD_DIM), a_k_norm_g[layer])
        ya = _diff_attention(qa, ka, av.reshape(b, s, A_HEADS, 2 * A_HEAD_DIM), lam)
        y_a = (_rmsnorm(ya, a_subln_g[layer]) * (1.0 - lam_init)).reshape(b, s, A_WIDTH)

        y = jnp.concatenate([y_m, y_c, y_a], axis=-1)
        h = h + y @ w_out[layer]

        c = _rmsnorm(h, ffn_norm_g[layer])
        if layer % 2 == 0:
            j = layer // 2
            f = _swiglu(c, dense_w_gate[j], dense_w_up[j], dense_w_down[j])
        else:
            j = layer // 2
            f = _moe_swiglu(c, router_w[j], moe_w_gate[j], moe_w_up[j], moe_w_down[j])
        h = h + f

        gate = jax.nn.sigmoid(_rmsnorm(h, ple_norm_g[layer]) @ w_ple_gate[layer])
        h = h + gate * (p[layer] @ w_ple_proj[layer])
    return h.astype(x.dtype)
```

```python
import contextlib
import math
import numpy as np
import concourse.bass as bass
import concourse.mybir as mybir
from concourse.bass_utils import run_bass_kernel_spmd

F32 = mybir.dt.float32
BF16 = mybir.dt.bfloat16
I32 = mybir.dt.int32
AF = mybir.ActivationFunctionType
ALU = mybir.AluOpType
AX = mybir.AxisListType

ENGS = ("pe", "act", "dve", "pool", "sp")
D = 1024
DIN = 3080
FFD = 2816
FFE = 3584
NEXP = 8
EPS = 1e-6
NDSEM = 56


class Buf:
    def __init__(self, name, ap=None, dsem=None):
        self.name = name
        self.ap = ap
        self.w = None
        self.r = {}
        self.dsem = dsem


class Ring:
    def __init__(self, bufs):
        self.bufs = bufs
        self.i = -1

    def next(self):
        self.i = (self.i + 1) % len(self.bufs)
        return self.bufs[self.i]


class Sched:
    def __init__(self, nc, stack):
        self.nc = nc
        self.stack = stack
        self.stacks = [stack]
        self.sems = {}
        self.cnt = {}
        self.prog = {e: [] for e in ENGS}
        self.seen = {e: {} for e in ENGS}
        for e in ENGS:
            self.newsem("E_" + e)
        self.free_ds = [self.newsem("D_%d" % i) for i in range(NDSEM)]
        self.scope_ds = [[]]
        self.banks = []
        self.bank_i = -1
        self.reserved = set()

    def newsem(self, key):
        h = self.stack.enter_context(self.nc.semaphore(key))
        self.sems[key] = h
        self.cnt[key] = 0
        return key

    def sbuf(self, name, shape, dtype, dma=False):
        self.uid = getattr(self, "uid", 0) + 1
        name = "%s_u%d" % (name, self.uid)
        t = self.stacks[-1].enter_context(self.nc.sbuf_tensor(name, list(shape), dtype))
        b = Buf(name, t)
        if dma:
            b.dsem = self.free_ds.pop()
            self.scope_ds[-1].append(b.dsem)
        return b

    def ring(self, name, shape, dtype, n, dma=False):
        return Ring([self.sbuf("%s_%d" % (name, i), shape, dtype, dma) for i in range(n)])

    def dram(self, name, shape, dtype):
        t = self.nc.dram_tensor(name, list(shape), dtype, kind="Internal")
        return Buf(name, t.ap())

    def make_banks(self):
        for i in range(8):
            t = self.stack.enter_context(self.nc.psum_tensor("bank%d" % i, [128, 512], F32))
            self.banks.append(Buf("bank%d" % i, t))

    def pbank(self):
        for _ in range(8):
            self.bank_i = (self.bank_i + 1) % 8
            if self.bank_i not in self.reserved:
                return self.banks[self.bank_i]
        raise RuntimeError("no psum bank")

    def reserve(self, n):
        out = []
        for i in range(8):
            if i not in self.reserved and len(out) < n:
                self.reserved.add(i)
                out.append(self.banks[i])
        return out

    def release(self, banks):
        for b in banks:
            self.reserved.discard(self.banks.index(b))

    @contextlib.contextmanager
    def scope(self):
        st = contextlib.ExitStack()
        self.stacks.append(st)
        self.scope_ds.append([])
        try:
            yield
        finally:
            self.barrier()
            self.free_ds.extend(self.scope_ds.pop())
            self.stacks.pop()
            st.close()

    def _waits(self, eng, reads, writes):
        need = {}

        def add(tok):
            if tok is None:
                return
            k, v = tok
            if eng == "pe" and k == "E_pe":
                return
            if v > need.get(k, 0):
                need[k] = v

        for b in reads:
            add(b.w)
        for b in writes:
            add(b.w)
            for k, v in b.r.items():
                add((k, v))
        out = []
        for k, v in need.items():
            if k.startswith("D_"):
                v = self.cnt[k]
            if self.seen[eng].get(k, 0) >= v:
                continue
            self.seen[eng][k] = v
            out.append((k, v))
        return out

    def _commit(self, tok, reads, writes):
        k, v = tok
        for b in reads:
            if v > b.r.get(k, 0):
                b.r[k] = v
        for b in writes:
            b.w = tok
            b.r = {}

    def op(self, eng, fn, reads=(), writes=()):
        waits = self._waits(eng, reads, writes)
        k = "E_" + eng
        self.cnt[k] += 1
        tok = (k, self.cnt[k])
        self.prog[eng].append((waits, fn, k, 1, 1))
        self._commit(tok, reads, writes)
        return tok

    def dma(self, fn, semb, reads=(), writes=(), n=1, q="sp"):
        waits = self._waits(q, reads, writes)
        k = semb.dsem
        self.cnt[k] += 16 * n
        tok = (k, self.cnt[k])
        self.prog[q].append((waits, fn, k, 16, n))
        self._commit(tok, reads, writes)
        return tok

    def barrier(self):
        for e in ENGS:
            out = []
            for k, v in self.cnt.items():
                if v == 0 or (e == "pe" and k == "E_pe"):
                    continue
                if self.seen[e].get(k, 0) >= v:
                    continue
                self.seen[e][k] = v
                out.append((k, v))
            if out:
                self.prog[e].append((out, None, None, 0, 0))

    def emit(self, block):
        def run(e):
            def body(engine):
                for waits, fn, k, inc, n in self.prog[e]:
                    for wk, wv in waits:
                        engine.wait_ge(self.sems[wk], wv)
                    if fn is None:
                        continue
                    r = fn(engine)
                    if inc == 16:
                        assert len(r) == n, (len(r), n)
                        for ins in r:
                            ins.then_inc(self.sems[k], 16)
                    else:
                        r.then_inc(self.sems[k], 1)
            return body
        block.tensor(run("pe"))
        block.scalar(run("act"))
        block.vector(run("dve"))
        block.gpsimd(run("pool"))
        block.sync(run("sp"))


PARAMS = [
    ("mix_norm_g", (2, 1024)), ("w_in", (2, 1024, 3080)), ("b_igate", (2, 4)), ("b_fgate", (2, 4)),
    ("m_qk_conv_w", (2, 4, 512)), ("m_out_norm_g", (2, 256)), ("c_conv_w", (2, 31, 256)),
    ("c_conv_b", (2, 256)), ("c_ln_g", (2, 256)), ("c_ln_b", (2, 256)), ("a_q_norm_g", (2, 64)),
    ("a_k_norm_g", (2, 64)), ("a_lambda_q1", (2, 64)), ("a_lambda_k1", (2, 64)), ("a_lambda_q2", (2, 64)),
    ("a_lambda_k2", (2, 64)), ("a_subln_g", (2, 128)), ("w_out", (2, 1024, 1024)), ("ffn_norm_g", (2, 1024)),
    ("dense_w_gate", (1, 1024, 2816)), ("dense_w_up", (1, 1024, 2816)), ("dense_w_down", (1, 2816, 1024)),
    ("router_w", (1, 1024, 8)), ("moe_w_gate", (1, 8, 1024, 3584)), ("moe_w_up", (1, 8, 1024, 3584)),
    ("moe_w_down", (1, 8, 3584, 1024)), ("ple_norm_g", (2, 1024)), ("w_ple_gate", (2, 1024, 1024)),
    ("w_ple_proj", (2, 256, 1024)),
]


def build(S_tok, layers=(0, 1), taps=(), nexp=NEXP):
    NT = S_tok // 128
    NG = S_tok // 512
    NCH = S_tok // 64
    nc = bass.Bass("TRN2", target_bir_lowering=False)
    I = {}
    I["x"] = nc.dram_tensor("x", [S_tok, D], F32, kind="ExternalInput").ap()
    I["p"] = nc.dram_tensor("p", [2, S_tok, 256], F32, kind="ExternalInput").ap()
    for name, shp in PARAMS:
        I[name] = nc.dram_tensor(name, list(shp), F32, kind="ExternalInput").ap()
    out_ap = nc.dram_tensor("out", [S_tok, D], F32, kind="ExternalOutput").ap()
    tap_aps = {}
    if "yT" in taps:
        tap_aps["yT"] = nc.dram_tensor("tap_yT", [1024, S_tok], BF16, kind="ExternalOutput").ap()

    with contextlib.ExitStack() as st:
        S = Sched(nc, st)
        S.make_banks()
        IN = {k: Buf("in_" + k, v) for k, v in I.items()}
        OUT = Buf("out", out_ap)

        def ACT(out, in_, func, R, W, **kw):
            S.op("act", lambda e: e.activation(out=out, in_=in_, func=func, **kw), R, W)

        def TT(eng, out, a, b, op, R, W):
            S.op(eng, lambda e: e.tensor_tensor(out=out, in0=a, in1=b, op=op), R, W)

        def TS(eng, out, a, s1, s2, op0, op1, R, W):
            if s2 is None:
                S.op(eng, lambda e: e.tensor_scalar(out=out, in0=a, scalar1=s1, scalar2=None, op0=op0), R, W)
            else:
                S.op(eng, lambda e: e.tensor_scalar(out=out, in0=a, scalar1=s1, scalar2=s2, op0=op0, op1=op1), R, W)

        def STT(eng, out, a, s, b, op0, op1, R, W):
            S.op(eng, lambda e: e.scalar_tensor_tensor(out=out, in0=a, scalar=s, in1=b, op0=op0, op1=op1), R, W)

        def CP(eng, out, in_, R, W):
            if eng == "act":
                S.op("act", lambda e: e.copy(out=out, in_=in_), R, W)
            else:
                S.op(eng, lambda e: e.tensor_copy(out=out, in_=in_), R, W)

        def MSET(eng, ap, val, W):
            S.op(eng, lambda e: e.memset(ap, val), (), W)

        def MM(items, R, W):
            def f(e):
                r = None
                for it in items:
                    (o, l, rh, s0, s1) = it[:5]
                    if len(it) > 5:
                        r = e.matmul(o, lhsT=l, rhs=rh, start=s0, stop=s1, skip_group_check=True)
                    else:
                        r = e.matmul(o, lhsT=l, rhs=rh, start=s0, stop=s1)
                return r
            S.op("pe", f, R, W)

        def TRN(items, R, W):
            def f(e):
                r = None
                for (o, i_) in items:
                    r = e.transpose(out=o, in_=i_, identity=ident.ap[0:i_.shape[0], 0:i_.shape[0]])
                return r
            S.op("pe", f, list(R) + [ident], W)

        def LD(out, in_, semb, R, W, q="sp"):
            S.dma(lambda e: [e.dma_start(out=out, in_=in_)], semb, R, W, 1, q)

        def LDS(pairs, semb, R, W, q="sp", slow=False):
            def f(e):
                if slow:
                    return [e.dma_start(out=o, in_=i_, allow_slow_non_contiguous=True) for (o, i_) in pairs]
                return [e.dma_start(out=o, in_=i_) for (o, i_) in pairs]
            S.dma(f, semb, R, W, len(pairs), q)

        def bview(bank):
            return bank.ap[:].bitcast(BF16)

        def rsqrt_act(out, in_, scale, R, W):
            ACT(out, in_, AF.Ln, R, W, scale=scale, bias=epsb.ap[0:in_.shape[0], 0:1])
            ACT(out, out, AF.Exp, W, W, scale=-0.5)

        ident = S.sbuf("ident", [128, 128], BF16)
        identf = S.sbuf("identf", [128, 128], F32)
        triT = S.sbuf("triT", [128, 128], F32)
        triTb = S.sbuf("triTb", [128, 128], BF16)
        triU = S.sbuf("triU", [128, 128], BF16)
        selA = S.sbuf("selA", [128, 128], F32)
        selB = S.sbuf("selB", [128, 128], F32)
        onesln = S.sbuf("onesln", [128, 128], F32)
        epsb = S.sbuf("epsb", [128, 1], F32)
        oneb = S.sbuf("oneb", [128, 1], F32)
        ln8b = S.sbuf("ln8b", [128, 1], F32)
        kcol_i = S.sbuf("kcol_i", [128, 1], I32)
        kcol = S.sbuf("kcol", [128, 1], F32)
        NDD = NT + 4
        abt = S.sbuf("abt", [128, 4, NDD], F32)
        A_tm = S.sbuf("A_tm", [128, NT, 4], F32)
        Gb = S.sbuf("Gb", [128, NCH, 4], F32)

        def mk_consts(e):
            e.memset(identf.ap[:], 1.0)
            e.affine_select(out=identf.ap[:], in_=identf.ap[:], compare_op=ALU.is_ge, fill=0.0,
                            base=0, pattern=[[-1, 128]], channel_multiplier=1)
            e.affine_select(out=identf.ap[:], in_=identf.ap[:], compare_op=ALU.is_ge, fill=0.0,
                            base=0, pattern=[[1, 128]], channel_multiplier=-1)
            e.memset(triT.ap[:], 1.0)
            e.affine_select(out=triT.ap[:], in_=triT.ap[:], compare_op=ALU.is_ge, fill=0.0,
                            base=0, pattern=[[1, 128]], channel_multiplier=-1)
            e.memset(triT.ap[0:64, 64:128], 0.0)
            e.memset(selA.ap[:], 0.0)
            e.memset(selA.ap[0:64, :], 1.0)
            e.memset(selB.ap[:], 0.0)
            e.memset(selB.ap[64:128, :], 1.0)
            e.memset(onesln.ap[:], 1.0 / 256.0)
            e.memset(epsb.ap[:], EPS)
            e.memset(oneb.ap[:], 1.0)
            e.memset(ln8b.ap[:], -math.log(8.0))
            return e.iota(kcol_i.ap[:], pattern=[[0, 1]], base=0, channel_multiplier=1)
        S.op("pool", mk_consts, (), [identf, triT, selA, selB, onesln, epsb, oneb, ln8b, kcol_i])
        CP("pool", ident.ap[:], identf.ap[:], [identf], [ident])
        CP("pool", triTb.ap[:], triT.ap[:], [triT], [triTb])
        CP("pool", kcol.ap[:], kcol_i.ap[:], [kcol_i], [kcol])

        def mk_triU(e):
            e.memset(triU.ap[:], 1.0)
            return e.affine_select(out=triU.ap[:], in_=triU.ap[:], compare_op=ALU.is_ge, fill=0.0,
                                   base=0, pattern=[[1, 128]], channel_multiplier=-1)
        S.op("pool", mk_triU, (), [triU])
        for h in range(4):
            slope = 2.0 ** (-8.0 * (h + 1) / 4)
            for di in range(NDD):
                dd = di - NT
                TS("pool", abt.ap[:, h, di:di + 1], kcol.ap[:], slope, slope * (128.0 * dd - 256.0),
                   ALU.mult, ALU.add, [kcol], [abt])

        Wb = {}
        Wb["w_in"] = S.dram("wb_in", [2, 1024, DIN], BF16)
        Wb["w_out"] = S.dram("wb_out", [2, 1024, 1024], BF16)
        Wb["dense_w_gate"] = S.dram("wb_dg", [1, 1024, FFD], BF16)
        Wb["dense_w_up"] = S.dram("wb_du", [1, 1024, FFD], BF16)
        Wb["dense_w_down"] = S.dram("wb_dd", [1, FFD, 1024], BF16)
        Wb["moe_w_gate"] = S.dram("wb_mg", [1, 8, 1024, FFE], BF16)
        Wb["moe_w_up"] = S.dram("wb_mu", [1, 8, 1024, FFE], BF16)
        Wb["moe_w_down"] = S.dram("wb_md", [1, 8, FFE, 1024], BF16)
        Wb["w_ple_gate"] = S.dram("wb_pg", [2, 1024, 1024], BF16)
        Wb["w_ple_proj"] = S.dram("wb_pp", [2, 256, 1024], BF16)
        h1 = S.dram("h1", [S_tok, D], F32)
        mqT = S.dram("mqT", [256, S_tok], BF16)
        mkT = S.dram("mkT", [256, S_tok], BF16)
        mktm = S.dram("mktm", [S_tok, 256], BF16)
        mrv = S.dram("mrv", [S_tok, 4, 65], BF16)
        mso = S.dram("mso", [S_tok, 256], BF16)
        aqT = S.dram("aqT", [512, S_tok], BF16)
        akT = S.dram("akT", [512, S_tok], BF16)
        avd = S.dram("avd", [S_tok, 512], BF16)
        yT = S.dram("yT", [1024, S_tok], BF16)

        block = st.enter_context(nc.Block())

        def convert(names_layers):
            with S.scope():
                CB = 3584
                fr = S.ring("cvf", [128, CB], F32, 3, dma=True)
                br = S.ring("cvb", [128, CB], BF16, 3, dma=True)
                ei = 0
                for (name, idx) in names_layers:
                    src = IN[name].ap
                    dst = Wb[name].ap
                    for ix in idx:
                        src = src[ix]
                        dst = dst[ix]
                    R_, C_ = src.shape
                    for r0 in range(0, R_, 128):
                        for c0 in range(0, C_, CB):
                            cw = min(CB, C_ - c0)
                            fb = fr.next()
                            bb = br.next()
                            LD(fb.ap[:, 0:cw], src[r0:r0 + 128, c0:c0 + cw], fb, [IN[name]], [fb])
                            eng = ("act", "dve", "pool")[ei % 3]
                            ei += 1
                            CP(eng, bb.ap[:, 0:cw], fb.ap[:, 0:cw], [fb], [bb])
                            LD(dst[r0:r0 + 128, c0:c0 + cw], bb.ap[:, 0:cw], bb, [bb], [Wb[name]], q="pool")

        def phase_A(L, hsrc):
            with S.scope():
                win = S.sbuf("win", [128, 8, DIN], BF16, dma=True)
                wv = Wb["w_in"].ap[L].rearrange("(k p) n -> p k n", p=128)
                LDS([(win.ap[:, :, c:c + 770], wv[:, :, c:c + 770]) for c in range(0, DIN, 770)],
                    win, [Wb["w_in"]], [win])
                prm = S.sbuf("prmA", [128, 1], F32, dma=True)
                gbc = S.sbuf("gbcA", [128, 1024], F32)
                bif = S.sbuf("bif", [128, 8], F32)
                wq4 = S.sbuf("wq4", [128, 4, 4], F32)
                wc31 = S.sbuf("wc31", [128, 2, 31], F32)
                cvec = S.sbuf("cvec", [128, 3, 2], F32)
                gq = S.sbuf("gq", [128, 64], F32)
                gk = S.sbuf("gk", [128, 64], F32)
                LDS([(gbc.ap[:], I["mix_norm_g"][L].partition_broadcast(128)),
                     (bif.ap[:, 0:4], I["b_igate"][L].partition_broadcast(128)),
                     (bif.ap[:, 4:8], I["b_fgate"][L].partition_broadcast(128)),
                     (gq.ap[:], I["a_q_norm_g"][L].partition_broadcast(128)),
                     (gk.ap[:], I["a_k_norm_g"][L].partition_broadcast(128))],
                    prm, [], [gbc, bif, gq, gk])
                LDS([(wq4.ap[:, t, :], I["m_qk_conv_w"][L][:, t * 128:(t + 1) * 128].rearrange("j p -> p j"))
                     for t in range(4)] +
                    [(wc31.ap[:, t, :], I["c_conv_w"][L][:, t * 128:(t + 1) * 128].rearrange("j p -> p j"))
                     for t in range(2)] +
                    [(cvec.ap[:, 0, :], I["c_conv_b"][L].rearrange("(t p) -> p t", p=128)),
                     (cvec.ap[:, 1, :], I["c_ln_g"][L].rearrange("(t p) -> p t", p=128)),
                     (cvec.ap[:, 2, :], I["c_ln_b"][L].rearrange("(t p) -> p t", p=128))],
                    prm, [], [wq4, wc31, cvec], slow=True)
                dq = S.sbuf("dq", [128, 16, 128], BF16)
                dc = S.sbuf("dc", [128, 62, 128], BF16)
                for c in range(4):
                    for j in range(4):
                        TS("pool", dq.ap[:, c * 4 + j, :], identf.ap[:], wq4.ap[:, c, j:j + 1], None, ALU.mult, None,
                           [identf, wq4], [dq])
                for c in range(2):
                    for j in range(31):
                        TS("pool", dc.ap[:, c * 31 + j, :], identf.ap[:], wc31.ap[:, c, j:j + 1], None, ALU.mult, None,
                           [identf, wc31], [dc])

                hin = S.ring("hinA", [128, 1024], F32, 6, dma=True)
                junk = S.ring("junkA", [128, 1024], BF16, 2)
                ssr = S.ring("ssA", [128, 4], F32, 2)
                abf = S.ring("abfA", [128, 1024], BF16, 2)
                aTr = S.ring("aTA", [128, 8, 512], BF16, 2)
                xqk = S.sbuf("xqk", [128, 4, 515], BF16)
                zbuf = S.sbuf("zbuf", [128, 2, 542], BF16)
                qko = S.ring("qko", [128, 512], BF16, 3, dma=True)
                ktmo = S.ring("ktmo", [128, 4, 256], BF16, 2, dma=True)
                sgr = S.ring("sgr", [128, 512], F32, 2)
                zc = S.sbuf("zc", [128, 2, 512], F32)
                zc2 = S.sbuf("zc2", [128, 2, 512], F32)
                mean_sb = S.sbuf("mean_sb", [128, 512], F32)
                var_sb = S.sbuf("var_sb", [128, 512], F32)
                dtmp = S.ring("dtmp", [128, 512], F32, 2)
                yco = S.ring("yco", [128, 512], BF16, 2, dma=True)
                gat = S.ring("gat", [128, 16], F32, 2)
                rvo = S.ring("rvo", [128, 4, 65], BF16, 2, dma=True)
                soo = S.ring("soo", [128, 256], BF16, 2, dma=True)
                sqj = S.ring("sqj", [128, 512], F32, 2)
                ssq = S.ring("ssq", [128, 16], F32, 2)
                qn1 = S.ring("qn1", [128, 512], F32, 2)
                qnb = S.ring("qnb", [128, 512], BF16, 2)
                qTo = S.ring("qTo", [128, 4, 512], BF16, 2, dma=True)
                kTo = S.ring("kTo", [128, 4, 512], BF16, 2, dma=True)
                vo = S.ring("vo", [128, 512], BF16, 2, dma=True)

                MSET("pool", xqk.ap[:, :, 0:3], 0.0, [xqk])
                MSET("pool", zbuf.ap[:, :, 0:30], 0.0, [zbuf])

                for g in range(NG):
                    t0g = g * 512
                    aT = aTr.next()
                    ss = ssr.next()
                    hbs = []
                    for j in range(4):
                        hb = hin.next()
                        hbs.append(hb)
                        r0 = t0g + j * 128
                        LD(hb.ap[:], hsrc.ap[r0:r0 + 128, :], hb, [hsrc], [hb])
                        jk = junk.next()
                        ACT(jk.ap[:], hb.ap[:], AF.Square, [hb], [jk, ss], accum_out=ss.ap[:, j:j + 1])
                    rsqrt_act(ss.ap[:], ss.ap[:], 1.0 / 1024, [ss], [ss])
                    for j in range(4):
                        ab = abf.next()
                        STT("dve", ab.ap[:], hbs[j].ap[:], ss.ap[:, j:j + 1], gbc.ap[:], ALU.mult, ALU.mult,
                            [hbs[j], ss, gbc], [ab])
                        pb = S.pbank()
                        pv = bview(pb).rearrange("p (k n) -> p k n", k=8)
                        TRN([(pv[:, k, :], ab.ap[:, k * 128:(k + 1) * 128]) for k in range(8)], [ab], [pb])
                        CP("act" if j % 2 == 0 else "dve", aT.ap[:, :, j * 128:(j + 1) * 128], pv, [pb], [aT])

                    def fm(col0):
                        pb = S.pbank()
                        MM([(pb.ap[:], win.ap[:, k, col0:col0 + 128], aT.ap[:, k, :], k == 0, k == 7)
                            for k in range(8)], [win, aT], [pb])
                        return pb

                    def tm(j, col0, n):
                        pb = S.pbank()
                        MM([(pb.ap[:, 0:n], aT.ap[:, k, j * 128:(j + 1) * 128], win.ap[:, k, col0:col0 + n],
                             k == 0, k == 7) for k in range(8)], [win, aT], [pb])
                        return pb

                    if g > 0:
                        CP("pool", xqk.ap[:, :, 0:3], xqk.ap[:, :, 512:515], [xqk], [xqk])
                    for c in range(4):
                        pb = fm(c * 128)
                        CP("act", xqk.ap[:, c, 3:515], pb.ap[:], [pb], [xqk])
                    kt = ktmo.next()
                    for c in range(4):
                        pb = S.pbank()
                        MM([(pb.ap[:], dq.ap[:, c * 4 + j, :], xqk.ap[:, c, j:j + 512], j == 0, j == 3)
                            for j in range(4)], [dq, xqk], [pb])
                        qo = qko.next()
                        ACT(qo.ap[:], pb.ap[:], AF.Silu, [pb], [qo])
                        dstT = (mqT if c < 2 else mkT)
                        LD(dstT.ap[(c % 2) * 128:(c % 2 + 1) * 128, t0g:t0g + 512], qo.ap[:], qo, [qo], [dstT], q="pool")
                        if c >= 2:
                            pb2 = S.pbank()
                            pv2 = bview(pb2).rearrange("p (k n) -> p k n", k=8)
                            TRN([(pv2[:, j, :], qo.ap[:, j * 128:(j + 1) * 128]) for j in range(4)], [qo], [pb2])
                            CP("dve", kt.ap[:, :, (c - 2) * 128:(c - 1) * 128], pv2[:, 0:4, :], [pb2], [kt])
                    LD(mktm.ap[t0g:t0g + 512, :].rearrange("(j p) c -> p j c", p=128), kt.ap[:], kt, [kt], [mktm], q="pool")

                    if g > 0:
                        CP("pool", zbuf.ap[:, :, 0:30], zbuf.ap[:, :, 512:542], [zbuf], [zbuf])
                    for c in range(2):
                        pa = fm(1032 + c * 128)
                        pg = fm(1288 + c * 128)
                        sg = sgr.next()
                        ACT(sg.ap[:], pg.ap[:], AF.Sigmoid, [pg], [sg])
                        TT("dve", zbuf.ap[:, c, 30:542], pa.ap[:], sg.ap[:], ALU.mult, [pa, sg], [zbuf])
                    for c in range(2):
                        pb = S.pbank()
                        MM([(pb.ap[:], dc.ap[:, c * 31 + j, :], zbuf.ap[:, c, j:j + 512], j == 0, j == 30)
                            for j in range(31)], [dc, zbuf], [pb])
                        ACT(zc.ap[:, c, :], pb.ap[:], AF.Identity, [pb, cvec], [zc], bias=cvec.ap[:, 0, c:c + 1])
                        ACT(zc2.ap[:, c, :], zc.ap[:, c, :], AF.Square, [zc], [zc2])
                    pm = S.pbank()
                    MM([(pm.ap[:], onesln.ap[:], zc.ap[:, c, :], c == 0, c == 1) for c in range(2)], [onesln, zc], [pm])
                    pv_ = S.pbank()
                    MM([(pv_.ap[:], onesln.ap[:], zc2.ap[:, c, :], c == 0, c == 1) for c in range(2)], [onesln, zc2], [pv_])
                    CP("act", mean_sb.ap[:], pm.ap[:], [pm], [mean_sb])
                    TT("dve", var_sb.ap[:], mean_sb.ap[:], mean_sb.ap[:], ALU.mult, [mean_sb], [var_sb])
                    TT("dve", var_sb.ap[:], pv_.ap[:], var_sb.ap[:], ALU.subtract, [pv_, var_sb], [var_sb])
                    rsqrt_act(var_sb.ap[:], var_sb.ap[:], 1.0, [var_sb], [var_sb])
                    for c in range(2):
                        dt_ = dtmp.next()
                        TT("dve", dt_.ap[:], zc.ap[:, c, :], mean_sb.ap[:], ALU.subtract, [zc, mean_sb], [dt_])
                        TT("dve", dt_.ap[:], dt_.ap[:], var_sb.ap[:], ALU.mult, [dt_, var_sb], [dt_])
                        yo = yco.next()
                        ACT(yo.ap[:], dt_.ap[:], AF.Silu, [dt_, cvec], [yo], scale=cvec.ap[:, 1, c:c + 1],
                            bias=cvec.ap[:, 2, c:c + 1])
                        LD(yT.ap[256 + c * 128:256 + (c + 1) * 128, t0g:t0g + 512], yo.ap[:], yo, [yo], [yT], q="pool")

                    qT_ = qTo.next()
                    kT_ = kTo.next()
                    for j in range(4):
                        t = g * 4 + j
                        r0 = t * 128
                        pvo = tm(j, 512, 512)
                        pif = tm(j, 1024, 8)
                        ga = gat.next()
                        TT("dve", ga.ap[:, 0:8], pif.ap[:, 0:8], bif.ap[:], ALU.add, [pif, bif], [ga])
                        ACT(ga.ap[:, 4:8], ga.ap[:, 4:8], AF.Exp, [ga], [ga], scale=-1.0)
                        ACT(ga.ap[:, 4:8], ga.ap[:, 4:8], AF.Ln, [ga], [ga], bias=oneb.ap[:, 0:1])
                        pc = S.pbank()
                        MM([(pc.ap[:, 0:4], triT.ap[:], ga.ap[:, 4:8], True, True),
                            (pc.ap[:, 4:8], selA.ap[:], ga.ap[:, 4:8], True, True),
                            (pc.ap[:, 8:12], selB.ap[:], ga.ap[:, 4:8], True, True)], [triT, selA, selB, ga], [pc])
                        ACT(A_tm.ap[:, t, :], pc.ap[:, 0:4], AF.Exp, [pc], [A_tm], scale=-1.0, bias=ln8b.ap[:, 0:1])
                        ACT(Gb.ap[:, 2 * t:2 * t + 2, :], pc.ap[:, 4:12].rearrange("p (a b) -> p a b", a=2), AF.Exp,
                            [pc], [Gb], scale=-1.0)
                        TT("dve", ga.ap[:, 8:12], ga.ap[:, 0:4], pc.ap[:, 0:4], ALU.add, [ga, pc], [ga])
                        ACT(ga.ap[:, 8:12], ga.ap[:, 8:12], AF.Exp, [ga], [ga])
                        rv = rvo.next()
                        TT("dve", rv.ap[:, :, 0:64], pvo.ap[:, 0:256].rearrange("p (h d) -> p h d", h=4),
                           ga.ap[:, 8:12].unsqueeze(2).to_broadcast([128, 4, 64]), ALU.mult, [pvo, ga], [rv])
                        CP("dve", rv.ap[:, :, 64:65], ga.ap[:, 8:12].unsqueeze(2), [ga], [rv])
                        LD(mrv.ap[r0:r0 + 128, :, :], rv.ap[:], rv, [rv], [mrv], q="pool")
                        so = soo.next()
                        ACT(so.ap[:], pvo.ap[:, 256:512], AF.Sigmoid, [pvo], [so])
                        LD(mso.ap[r0:r0 + 128, :], so.ap[:], so, [so], [mso], q="pool")
                        pq = tm(j, 1544, 512)
                        pk = tm(j, 2056, 512)
                        pvv = tm(j, 2568, 512)
                        sq_ = ssq.next()
                        for (pp_, off) in ((pq, 0), (pk, 8)):
                            sj = sqj.next()
                            ACT(sj.ap[:], pp_.ap[:], AF.Square, [pp_], [sj])
                            S.op("dve", (lambda sj=sj, off=off, sq_=sq_: lambda e: e.reduce_sum(
                                out=sq_.ap[:, off:off + 8], in_=sj.ap[:].rearrange("p (m d) -> p m d", m=8),
                                axis=AX.X))(), [sj], [sq_])
                        rsqrt_act(sq_.ap[:], sq_.ap[:], 1.0 / 64, [sq_], [sq_])
                        for (pp_, off, gg, dstT_) in ((pq, 0, gq, qT_), (pk, 8, gk, kT_)):
                            q1 = qn1.next()
                            TT("dve", q1.ap[:].rearrange("p (m d) -> p m d", m=8),
                               pp_.ap[:].rearrange("p (m d) -> p m d", m=8),
                               sq_.ap[:, off:off + 8].unsqueeze(2).to_broadcast([128, 8, 64]), ALU.mult,
                               [pp_, sq_], [q1])
                            qb = qnb.next()
                            TT("pool", qb.ap[:].rearrange("p (m d) -> p m d", m=8),
                               q1.ap[:].rearrange("p (m d) -> p m d", m=8),
                               gg.ap[:].unsqueeze(1).to_broadcast([128, 8, 64]), ALU.mult, [q1, gg], [qb])
                            pb = S.pbank()
                            pvw = bview(pb).rearrange("p (k n) -> p k n", k=8)
                            TRN([(pvw[:, m, :], qb.ap[:, m * 128:(m + 1) * 128]) for m in range(4)], [qb], [pb])
                            CP("act", dstT_.ap[:, :, j * 128:(j + 1) * 128], pvw[:, 0:4, :], [pb], [dstT_])
                        v_ = vo.next()
                        CP("act", v_.ap[:], pvv.ap[:], [pvv], [v_])
                        LD(avd.ap[r0:r0 + 128, :], v_.ap[:], v_, [v_], [avd], q="pool")
                    LD(aqT.ap[:, t0g:t0g + 512].rearrange("(m p) s -> p m s", p=128), qT_.ap[:], qT_, [qT_], [aqT], q="pool")
                    LD(akT.ap[:, t0g:t0g + 512].rearrange("(m p) s -> p m s", p=128), kT_.ap[:], kT_, [kT_], [akT], q="pool")

        def phase_B(L):
            with S.scope():
                prm = S.sbuf("prmB", [128, 1], F32, dma=True)
                gm = S.sbuf("gmB", [128, 256], F32)
                LDS([(gm.ap[:], I["m_out_norm_g"][L].partition_broadcast(128))], prm, [], [gm])
                qTh = S.sbuf("qTh", [64, S_tok], BF16, dma=True)
                kTh = S.sbuf("kTh", [64, S_tok], BF16, dma=True)
                ktm = S.sbuf("ktmB", [128, NT, 64], BF16, dma=True)
                rvbd = S.sbuf("rvbd", [128, NT, 130], BF16, dma=True)
                soh = S.sbuf("soh", [128, NT, 64], BF16, dma=True)
                gso = S.sbuf("gso", [128, NT, 64], F32)
                X = S.sbuf("Xst", [64, NCH, 65], F32)
                Cst = S.sbuf("Cst", [64, NCH + 1, 65], BF16)
                yTm = S.sbuf("yTm", [64, S_tok], BF16, dma=True)
                smt = S.ring("smt", [128, 128], BF16, 3)
                ndr = S.ring("ndr", [128, 65], F32, 3)
                sm = S.ring("smB", [128, 8], F32, 3)
                jk = S.ring("jkB", [128, 64], F32, 2)
                ybr = S.ring("ybB", [128, 64], BF16, 3)
                MSET("pool", rvbd.ap[:], 0.0, [rvbd])
                MSET("pool", Cst.ap[:, 0, :], 0.0, [Cst])
                for h in range(4):
                    LD(qTh.ap[:], mqT.ap[h * 64:(h + 1) * 64, :], qTh, [mqT], [qTh])
                    LD(kTh.ap[:], mkT.ap[h * 64:(h + 1) * 64, :], kTh, [mkT], [kTh])
                    LDS([(ktm.ap[:], mktm.ap[:, h * 64:(h + 1) * 64].rearrange("(i p) c -> p i c", p=128))],
                        ktm, [mktm], [ktm], slow=True)
                    mv = mrv.ap.rearrange("(i two p) h c -> two p i h c", two=2, p=64)
                    LDS([(rvbd.ap[0:64, :, 0:65], mv[0][:, :, h, :]),
                         (rvbd.ap[64:128, :, 65:130], mv[1][:, :, h, :])], rvbd, [mrv], [rvbd], slow=True)
                    LDS([(soh.ap[:], mso.ap[:, h * 64:(h + 1) * 64].rearrange("(i p) c -> p i c", p=128))],
                        soh, [mso], [soh], slow=True)
                    TT("pool", gso.ap[:], soh.ap[:], gm.ap[:, h * 64:(h + 1) * 64].unsqueeze(1).to_broadcast([128, NT, 64]),
                       ALU.mult, [soh, gm], [gso])
                    for i in range(NT):
                        pb = S.pbank()
                        MM([(pb.ap[0:64, 0:130], ktm.ap[:, i, :], rvbd.ap[:, i, :], True, True)], [ktm, rvbd], [pb])
                        ACT(X.ap[:, 2 * i, :], pb.ap[0:64, 0:65], AF.Copy, [pb, Gb], [X], scale=Gb.ap[0:64, 2 * i, h:h + 1])
                        ACT(X.ap[:, 2 * i + 1, :], pb.ap[0:64, 65:130], AF.Copy, [pb, Gb], [X],
                            scale=Gb.ap[0:64, 2 * i + 1, h:h + 1])
                    for c in range(1, NCH):
                        STT("dve", X.ap[:, c, :], X.ap[:, c - 1, :], Gb.ap[0:64, c, h:h + 1], X.ap[:, c, :],
                            ALU.mult, ALU.add, [X, Gb], [X])
                    CP("act", Cst.ap[:, 1:NCH + 1, :], X.ap[:, :, :], [X], [Cst])
                    for i in range(NT):
                        ts_ = slice(i * 128, (i + 1) * 128)
                        ps = S.pbank()
                        MM([(ps.ap[:, 0:128], kTh.ap[:, ts_], qTh.ap[:, ts_], True, True)], [kTh, qTh], [ps])
                        sm_ = smt.next()
                        TT("dve", sm_.ap[:], ps.ap[:, 0:128], triT.ap[:], ALU.mult, [ps, triT], [sm_])
                        po = S.pbank()
                        MM([(po.ap[:, 0:130], qTh.ap[:, ts_], Cst.ap[:, 2 * i:2 * i + 2, :].rearrange("p a b -> p (a b)"),
                             True, False),
                            (po.ap[:, 0:130], sm_.ap[:], rvbd.ap[:, i, :], False, True)], [qTh, Cst, sm_, rvbd], [po])
                        nd = ndr.next()
                        ACT(nd.ap[0:64, :], po.ap[0:64, 0:65], AF.Copy, [po, A_tm], [nd], scale=A_tm.ap[0:64, i, h:h + 1])
                        ACT(nd.ap[64:128, :], po.ap[64:128, 65:130], AF.Copy, [po, A_tm], [nd],
                            scale=A_tm.ap[64:128, i, h:h + 1])
                        s_ = sm.next()
                        ACT(s_.ap[:, 0:1], nd.ap[:, 64:65], AF.Abs, [nd], [s_])
                        TS("dve", s_.ap[:, 0:1], s_.ap[:, 0:1], 1.0, None, ALU.max, None, [s_], [s_])
                        S.op("dve", (lambda s_=s_: lambda e: e.reciprocal(out=s_.ap[:, 1:2], in_=s_.ap[:, 0:1]))(), [s_], [s_])
                        j_ = jk.next()
                        ACT(j_.ap[:], nd.ap[:, 0:64], AF.Square, [nd, s_], [j_, s_], scale=s_.ap[:, 1:2],
                            accum_out=s_.ap[:, 2:3])
                        rsqrt_act(s_.ap[:, 3:4], s_.ap[:, 2:3], 1.0 / 64, [s_], [s_])
                        TT("dve", s_.ap[:, 4:5], s_.ap[:, 3:4], s_.ap[:, 1:2], ALU.mult, [s_], [s_])
                        yb = ybr.next()
                        STT("dve", yb.ap[:], nd.ap[:, 0:64], s_.ap[:, 4:5], gso.ap[:, i, :], ALU.mult, ALU.mult,
                            [nd, s_, gso], [yb])
                        pt = S.pbank()
                        ptv = bview(pt)
                        TRN([(ptv[0:64, 0:128], yb.ap[:, :])], [yb], [pt])
                        CP("act", yTm.ap[:, ts_], ptv[0:64, 0:128], [pt], [yTm])
                    LD(yT.ap[h * 64:(h + 1) * 64, :], yTm.ap[:], yTm, [yTm], [yT], q="pool")

        def phase_C(L):
            lam_init = 0.8 - 0.6 * math.exp(-0.3 * L)
            with S.scope():
                prm = S.sbuf("prmC", [128, 1], F32, dma=True)
                lv = S.sbuf("lvC", [128, 4, 64], F32)
                gsub = S.sbuf("gsub", [128, 128], F32)
                lam = S.sbuf("lam", [128, 4], F32)
                ljk = S.sbuf("ljk", [128, 64], F32)
                LDS([(lv.ap[:, 0, :], I["a_lambda_q1"][L].partition_broadcast(128)),
                     (lv.ap[:, 1, :], I["a_lambda_k1"][L].partition_broadcast(128)),
                     (lv.ap[:, 2, :], I["a_lambda_q2"][L].partition_broadcast(128)),
                     (lv.ap[:, 3, :], I["a_lambda_k2"][L].partition_broadcast(128)),
                     (gsub.ap[:], I["a_subln_g"][L].partition_broadcast(128))], prm, [], [lv, gsub])
                MSET("dve", lam.ap[:], 0.0, [lam])
                S.op("dve", lambda e: e.tensor_tensor(out=ljk.ap[:], in0=lv.ap[:, 0, :], in1=lv.ap[:, 1, :], op=ALU.mult),
                     [lv], [ljk])
                S.op("dve", lambda e: e.reduce_sum(out=lam.ap[:, 0:1], in_=ljk.ap[:], axis=AX.X), [ljk], [lam])
                S.op("dve", lambda e: e.tensor_tensor(out=ljk.ap[:], in0=lv.ap[:, 2, :], in1=lv.ap[:, 3, :], op=ALU.mult),
                     [lv, lam], [ljk])
                S.op("dve", lambda e: e.reduce_sum(out=lam.ap[:, 1:2], in_=ljk.ap[:], axis=AX.X), [ljk], [lam])
                ACT(lam.ap[:, 0:2], lam.ap[:, 0:2], AF.Exp, [lam], [lam])
                TT("dve", lam.ap[:, 2:3], lam.ap[:, 0:1], lam.ap[:, 1:2], ALU.subtract, [lam], [lam])
                TS("dve", lam.ap[:, 3:4], lam.ap[:, 2:3], lam_init, None, ALU.add, None, [lam], [lam])
                TS("dve", gsub.ap[:], gsub.ap[:], 1.0 - lam_init, None, ALU.mult, None, [gsub], [gsub])

                KT = S.ring("KTC", [128, S_tok], BF16, 2, dma=True)
                VV = S.ring("VVC", [128, NT, 129], BF16, 2, dma=True)
                QT = S.ring("QTC", [128, 512], BF16, 3, dma=True)
                PT = S.ring("PTC", [128, 512], BF16, 6)
                yta = S.ring("ytaC", [128, 512], BF16, 2, dma=True)
                sm = S.ring("smC", [128, 8], F32, 4)
                t2r = S.ring("t2C", [128, 128], F32, 2)
                yar = S.ring("yaC", [128, 128], F32, 2)
                jkr = S.ring("jkC", [128, 128], BF16, 2)
                ybr = S.ring("ybC", [128, 128], BF16, 2)
                for vb in VV.bufs:
                    MSET("pool", vb.ap[:, :, 128:129], 1.0, [vb])
                accb = S.reserve(3)

                def acc(m, j):
                    i_ = m * 4 + j
                    return accb[i_ // 3], (i_ % 3) * 132

                for h in range(4):
                    kt = KT.next()
                    vv = VV.next()
                    LD(kt.ap[:], akT.ap[h * 128:(h + 1) * 128, :], kt, [akT], [kt])
                    LDS([(vv.ap[:, :, 0:128], avd.ap[:, h * 128:(h + 1) * 128].rearrange("(i p) c -> p i c", p=128))],
                        vv, [avd], [vv])
                    for qg in range(NG):
                        qt = QT.next()
                        LD(qt.ap[:], aqT.ap[h * 128:(h + 1) * 128, qg * 512:(qg + 1) * 512], qt, [aqT], [qt])
                        nkb = 4 * qg + 4
                        for kb in range(nkb):
                            jj = max(0, kb - 4 * qg)
                            c0 = jj * 128
                            di = kb - 4 * qg + NT
                            pts = []
                            for m in range(2):
                                pb = S.pbank()
                                MM([(pb.ap[:, c0:512], kt.ap[m * 64:(m + 1) * 64, kb * 128:(kb + 1) * 128],
                                     qt.ap[m * 64:(m + 1) * 64, c0:512], True, True)], [kt, qt], [pb])
                                pt = PT.next()
                                ACT(pt.ap[:, c0:512], pb.ap[:, c0:512], AF.Exp, [pb, abt], [pt], scale=0.125,
                                    bias=abt.ap[:, h, di:di + 1])
                                if kb >= 4 * qg:
                                    TT("pool", pt.ap[:, c0:c0 + 128], pt.ap[:, c0:c0 + 128], triU.ap[:], ALU.mult,
                                       [pt, triU], [pt])
                                pts.append(pt)
                            items = []
                            for m in range(2):
                                for j in range(jj, 4):
                                    bk, off = acc(m, j)
                                    items.append((bk.ap[:, off:off + 129], pts[m].ap[:, j * 128:(j + 1) * 128],
                                                  vv.ap[:, kb, :], kb == 0 and off == 0, kb == 4 * qg + j, True))
                            MM(items, pts + [vv], accb)
                        yt = yta.next()
                        for j in range(4):
                            b1, o1 = acc(0, j)
                            b2, o2 = acc(1, j)
                            s_ = sm.next()
                            S.op("dve", (lambda s_=s_, b1=b1, o1=o1: lambda e: e.reciprocal(
                                out=s_.ap[:, 0:1], in_=b1.ap[:, o1 + 128:o1 + 129]))(), accb, [s_])
                            S.op("dve", (lambda s_=s_, b2=b2, o2=o2: lambda e: e.reciprocal(
                                out=s_.ap[:, 1:2], in_=b2.ap[:, o2 + 128:o2 + 129]))(), accb, [s_])
                            t2 = t2r.next()
                            TS("dve", t2.ap[:], b2.ap[:, o2:o2 + 128], s_.ap[:, 1:2], lam.ap[:, 3:4], ALU.mult, ALU.mult,
                               accb + [s_, lam], [t2])
                            ya = yar.next()
                            STT("dve", ya.ap[:], b1.ap[:, o1:o1 + 128], s_.ap[:, 0:1], t2.ap[:], ALU.mult, ALU.subtract,
                                accb + [s_, t2], [ya])
                            jk_ = jkr.next()
                            ACT(jk_.ap[:], ya.ap[:], AF.Square, [ya], [jk_, s_], accum_out=s_.ap[:, 2:3])
                            rsqrt_act(s_.ap[:, 3:4], s_.ap[:, 2:3], 1.0 / 128, [s_], [s_])
                            yb = ybr.next()
                            STT("dve", yb.ap[:], ya.ap[:], s_.ap[:, 3:4], gsub.ap[:], ALU.mult, ALU.mult,
                                [ya, s_, gsub], [yb])
                            pt_ = S.pbank()
                            ptv = bview(pt_)
                            TRN([(ptv[:, 0:128], yb.ap[:, :])], [yb], [pt_])
                            CP("act", yt.ap[:, j * 128:(j + 1) * 128], ptv[:, 0:128], [pt_], [yt])
                        LD(yT.ap[512 + h * 128:512 + (h + 1) * 128, qg * 512:(qg + 1) * 512], yt.ap[:], yt, [yt], [yT],
                           q="pool")
                S.release(accb)

        def phase_D(L, hsrc, hdst):
            moe = (L % 2 == 1)
            with S.scope():
                prm = S.sbuf("prmD", [128, 1], F32, dma=True)
                gff = S.sbuf("gffD", [128, 1024], F32)
                gpl = S.sbuf("gplD", [128, 1024], F32)
                LDS([(gff.ap[:], I["ffn_norm_g"][L].partition_broadcast(128)),
                     (gpl.ap[:], I["ple_norm_g"][L].partition_broadcast(128))], prm, [], [gff, gpl])
                wpp = S.sbuf("wppD", [128, 2, 1024], BF16, dma=True)
                LD(wpp.ap[:], Wb["w_ple_proj"].ap[L].rearrange("(k p) n -> p k n", p=128), wpp, [Wb["w_ple_proj"]], [wpp])
                if moe:
                    wrt = S.sbuf("wrtD", [128, 8, 8], F32, dma=True)
                    LDS([(wrt.ap[:], I["router_w"][0].rearrange("(k p) n -> p k n", p=128))], wrt, [], [wrt])
                wblk = S.ring("wblk", [128, 8, 512], BF16, 4, dma=True)
                wdblk = S.ring("wdblk", [128, 4, 512], BF16, 3, dma=True)
                hin = S.ring("hinD", [128, 1024], F32, 5, dma=True)
                hw = [S.sbuf("hw%d" % j, [128, 1024], F32, dma=True) for j in range(4)]
                yTg = S.ring("yTg", [128, 8, 512], BF16, 2, dma=True)
                junk = S.ring("junkD", [128, 1024], BF16, 2)
                ssr = S.ring("ssD", [128, 4], F32, 2)
                cbf = S.ring("cbfD", [128, 1024], BF16, 2)
                cT = S.sbuf("cTD", [128, 8, 512], BF16)
                FT = (FFE if moe else FFD) // 128
                hT = S.sbuf("hTD", [128, FT, 512], BF16)
                sgr = S.ring("sgD", [128, 512], F32, 3)
                pin = S.ring("pinD", [128, 256], F32, 4, dma=True)
                pbf = S.ring("pbfD", [128, 256], BF16, 2)
                pTt = S.sbuf("pTD", [128, 2, 512], BF16)
                gsb = S.ring("gsbD", [128, 512], F32, 2)
                if moe:
                    cf32 = S.ring("cf32", [128, 1024], F32, 2)
                    cTf = S.ring("cTf", [128, 8, 128], F32, 2)
                    rl = S.ring("rlD", [128, 8], F32, 2)
                    mx8 = S.ring("mx8", [128, 8], F32, 2)
                    rex = S.ring("rexD", [128, 8], F32, 2)
                    rsm = S.ring("rsmD", [128, 4], F32, 2)
                    comb = [S.sbuf("comb%d" % j, [128, 8], F32) for j in range(4)]

                def norm_T(gb_, dst, extra=None):
                    ss = ssr.next()
                    for j in range(4):
                        jk = junk.next()
                        ACT(jk.ap[:], hw[j].ap[:], AF.Square, [hw[j]], [jk, ss], accum_out=ss.ap[:, j:j + 1])
                    rsqrt_act(ss.ap[:], ss.ap[:], 1.0 / 1024, [ss], [ss])
                    for j in range(4):
                        cb = cbf.next()
                        STT("dve", cb.ap[:], hw[j].ap[:], ss.ap[:, j:j + 1], gb_.ap[:], ALU.mult, ALU.mult,
                            [hw[j], ss, gb_], [cb])
                        pb = S.pbank()
                        pv = bview(pb).rearrange("p (k n) -> p k n", k=8)
                        TRN([(pv[:, k, :], cb.ap[:, k * 128:(k + 1) * 128]) for k in range(8)], [cb], [pb])
                        CP("act" if j % 2 == 0 else "dve", dst.ap[:, :, j * 128:(j + 1) * 128], pv, [pb], [dst])
                        if extra is not None:
                            extra(j, ss)

                def ffn_expert(wg_ap, wu_ap, wd_ap, F_, scale_cols):
                    nfb = (F_ + 511) // 512
                    wgv = wg_ap.rearrange("(k p) n -> p k n", p=128)
                    wuv = wu_ap.rearrange("(k p) n -> p k n", p=128)
                    wdv = wd_ap.rearrange("(f p) n -> p f n", p=128)
                    for fb in range(nfb):
                        fw = min(512, F_ - fb * 512)
                        wg = wblk.next()
                        LD(wg.ap[:, :, 0:fw], wgv[:, :, fb * 512:fb * 512 + fw], wg, [WSRC], [wg])
                        wu = wblk.next()
                        LD(wu.ap[:, :, 0:fw], wuv[:, :, fb * 512:fb * 512 + fw], wu, [WSRC], [wu])
                        for ft in range(fw // 128):
                            f = fb * 4 + ft
                            pg = S.pbank()
                            MM([(pg.ap[:], wg.ap[:, k, ft * 128:(ft + 1) * 128], cT.ap[:, k, :], k == 0, k == 7)
                                for k in range(8)], [wg, cT], [pg])
                            pu = S.pbank()
                            MM([(pu.ap[:], wu.ap[:, k, ft * 128:(ft + 1) * 128], cT.ap[:, k, :], k == 0, k == 7)
                                for k in range(8)], [wu, cT], [pu])
                            sg = sgr.next()
                            ACT(sg.ap[:], pg.ap[:], AF.Silu, [pg], [sg])
                            TT("dve", hT.ap[:, f, :], pu.ap[:], sg.ap[:], ALU.mult, [pu, sg], [hT])
                    nft = F_ // 128
                    for half in range(2):
                        accs = [S.pbank() for _ in range(4)]
                        for fb in range(nfb):
                            nf = min(4, nft - fb * 4)
                            wd = wdblk.next()
                            LD(wd.ap[:, 0:nf, :], wdv[:, fb * 4:fb * 4 + nf, half * 512:(half + 1) * 512], wd, [WSRC], [wd])
                            items = []
                            for j in range(4):
                                for ft in range(nf):
                                    f = fb * 4 + ft
                                    items.append((accs[j].ap[:], hT.ap[:, f, j * 128:(j + 1) * 128], wd.ap[:, ft, :],
                                                  f == 0, f == nft - 1))
                            MM(items, [hT, wd], accs)
                        for j in range(4):
                            hs = hw[j].ap[:, half * 512:(half + 1) * 512]
                            if scale_cols is None:
                                TT("dve", hs, accs[j].ap[:], hs, ALU.add, [accs[j], hw[j]], [hw[j]])
                            else:
                                STT("dve", hs, accs[j].ap[:], scale_cols[j], hs, ALU.mult, ALU.add,
                                    [accs[j], hw[j]] + comb, [hw[j]])

                WSRC = Buf("wsrc_all")
                for g in range(NG):
                    t0g = g * 512
                    yg = yTg.next()
                    LD(yg.ap[:], yT.ap[:, t0g:t0g + 512].rearrange("(k p) s -> p k s", p=128), yg, [yT], [yg])
                    hbs = []
                    for j in range(4):
                        hb = hin.next()
                        hbs.append(hb)
                        LD(hb.ap[:], hsrc.ap[t0g + j * 128:t0g + (j + 1) * 128, :], hb, [hsrc], [hb])
                    wos = []
                    for half in range(2):
                        wo = wblk.next()
                        LD(wo.ap[:], Wb["w_out"].ap[L].rearrange("(k p) n -> p k n", p=128)[:, :, half * 512:(half + 1) * 512],
                           wo, [WSRC], [wo])
                        wos.append(wo)
                    for j in range(4):
                        for half in range(2):
                            pb = S.pbank()
                            MM([(pb.ap[:], yg.ap[:, k, j * 128:(j + 1) * 128], wos[half].ap[:, k, :], k == 0, k == 7)
                                for k in range(8)], [yg, wos[half]], [pb])
                            TT("dve", hw[j].ap[:, half * 512:(half + 1) * 512], pb.ap[:],
                               hbs[j].ap[:, half * 512:(half + 1) * 512], ALU.add, [pb, hbs[j]], [hw[j]])
                    if not moe:
                        norm_T(gff, cT)
                        ffn_expert(Wb["dense_w_gate"].ap[0], Wb["dense_w_up"].ap[0], Wb["dense_w_down"].ap[0], FFD, None)
                    else:
                        def router(j, ss):
                            cf = cf32.next()
                            STT("dve", cf.ap[:], hw[j].ap[:], ss.ap[:, j:j + 1], gff.ap[:], ALU.mult, ALU.mult,
                                [hw[j], ss, gff], [cf])
                            ct = cTf.next()
                            for kk in range(2):
                                pb = S.pbank()
                                pv = pb.ap[:].rearrange("p (k n) -> p k n", k=4)

                                def f(e, pv=pv, cf=cf, kk=kk):
                                    r = None
                                    for k in range(4):
                                        r = e.transpose(out=pv[:, k, :], in_=cf.ap[:, (kk * 4 + k) * 128:(kk * 4 + k + 1) * 128],
                                                        identity=identf.ap[:])
                                    return r
                                S.op("pe", f, [cf, identf], [pb])
                                CP("act", ct.ap[:, kk * 4:kk * 4 + 4, :], pv, [pb], [ct])
                            pl = S.pbank()
                            MM([(pl.ap[:, 0:8], ct.ap[:, k, :], wrt.ap[:, k, :], k == 0, k == 7) for k in range(8)],
                               [ct, wrt], [pl])
                            lg = rl.next()
                            CP("act", lg.ap[:], pl.ap[:, 0:8], [pl], [lg])
                            m8 = mx8.next()
                            S.op("dve", (lambda m8=m8, lg=lg: lambda e: e.max(out=m8.ap[:], in_=lg.ap[:]))(), [lg], [m8])
                            ex = rex.next()
                            r4 = rsm.next()
                            TS("dve", r4.ap[:, 0:1], m8.ap[:, 0:1], -1.0, None, ALU.mult, None, [m8], [r4])
                            ACT(ex.ap[:], lg.ap[:], AF.Exp, [lg, r4], [ex], bias=r4.ap[:, 0:1])
                            ACT(r4.ap[:, 1:2], m8.ap[:, 1:2], AF.Exp, [m8, r4], [r4], bias=r4.ap[:, 0:1])
                            TS("dve", r4.ap[:, 1:2], r4.ap[:, 1:2], 1.0, None, ALU.add, None, [r4], [r4])
                            S.op("dve", (lambda r4=r4: lambda e: e.reciprocal(out=r4.ap[:, 2:3], in_=r4.ap[:, 1:2]))(), [r4], [r4])
                            TS("dve", comb[j].ap[:], lg.ap[:], m8.ap[:, 1:2], None, ALU.is_ge, None, [lg, m8], [comb[j]])
                            TT("dve", comb[j].ap[:], comb[j].ap[:], ex.ap[:], ALU.mult, [ex, comb[j]], [comb[j]])
                            TS("dve", comb[j].ap[:], comb[j].ap[:], r4.ap[:, 2:3], None, ALU.mult, None, [r4, comb[j]], [comb[j]])
                        norm_T(gff, cT, router)
                        for ex_ in range(nexp):
                            ffn_expert(Wb["moe_w_gate"].ap[0][ex_], Wb["moe_w_up"].ap[0][ex_], Wb["moe_w_down"].ap[0][ex_],
                                       FFE, [comb[j].ap[:, ex_:ex_ + 1] for j in range(4)])
                    norm_T(gpl, cT)
                    for j in range(4):
                        pi_ = pin.next()
                        LD(pi_.ap[:], I["p"][L][t0g + j * 128:t0g + (j + 1) * 128, :], pi_, [], [pi_])
                        pb_ = pbf.next()
                        CP("pool", pb_.ap[:], pi_.ap[:], [pi_], [pb_])
                        pk_ = S.pbank()
                        pv = bview(pk_).rearrange("p (k n) -> p k n", k=8)
                        TRN([(pv[:, k, :], pb_.ap[:, k * 128:(k + 1) * 128]) for k in range(2)], [pb_], [pk_])
                        CP("act", pTt.ap[:, :, j * 128:(j + 1) * 128], pv[:, 0:2, :], [pk_], [pTt])
                    wgs = []
                    for half in range(2):
                        wo = wblk.next()
                        LD(wo.ap[:], Wb["w_ple_gate"].ap[L].rearrange("(k p) n -> p k n", p=128)[:, :, half * 512:(half + 1) * 512],
                           wo, [WSRC], [wo])
                        wgs.append(wo)
                    for j in range(4):
                        for half in range(2):
                            pg = S.pbank()
                            MM([(pg.ap[:], cT.ap[:, k, j * 128:(j + 1) * 128], wgs[half].ap[:, k, :], k == 0, k == 7)
                                for k in range(8)], [cT, wgs[half]], [pg])
                            pp2 = S.pbank()
                            MM([(pp2.ap[:], pTt.ap[:, k, j * 128:(j + 1) * 128], wpp.ap[:, k, half * 512:(half + 1) * 512],
                                 k == 0, k == 1) for k in range(2)], [pTt, wpp], [pp2])
                            gs = gsb.next()
                            ACT(gs.ap[:], pg.ap[:], AF.Sigmoid, [pg], [gs])
                            TT("dve", gs.ap[:], pp2.ap[:], gs.ap[:], ALU.mult, [pp2, gs], [gs])
                            hs = hw[j].ap[:, half * 512:(half + 1) * 512]
                            TT("dve", hs, hs, gs.ap[:], ALU.add, [gs, hw[j]], [hw[j]])
                        LD(hdst.ap[t0g + j * 128:t0g + (j + 1) * 128, :], hw[j].ap[:], hw[j], [hw[j]], [hdst], q="pool")

        conv_list = []
        for L in layers:
            conv_list += [("w_in", (L,)), ("w_out", (L,)), ("w_ple_gate", (L,)), ("w_ple_proj", (L,))]
            if L % 2 == 0:
                conv_list += [("dense_w_gate", (0,)), ("dense_w_up", (0,)), ("dense_w_down", (0,))]
            else:
                for ex_ in range(nexp):
                    conv_list += [("moe_w_gate", (0, ex_)), ("moe_w_up", (0, ex_)), ("moe_w_down", (0, ex_))]
        convert(conv_list)
        hcur = IN["x"]
        for li, L in enumerate(layers):
            hnext = OUT if li == len(layers) - 1 else h1
            phase_A(L, hcur)
            phase_B(L)
            phase_C(L)
            if "yT" in taps and li == len(layers) - 1:
                break
            phase_D(L, hcur, hnext)
            hcur = hnext
        if "yT" in taps:
            tp = tap_aps["yT"]
            with S.scope():
                tb = S.sbuf("tapb", [128, 8, S_tok], BF16, dma=True)
                LD(tb.ap[:], yT.ap.rearrange("(k p) s -> p k s", p=128), tb, [yT], [tb])
                LD(tp.rearrange("(k p) s -> p k s", p=128), tb.ap[:], tb, [tb], [OUT])
        S.barrier()
        S.emit(block)
    return nc


_CACHE = {}


def kernel(**inputs):
    x = np.asarray(inputs["x"], dtype=np.float32)
    B, S_tok, _ = x.shape
    p = np.asarray(inputs["p"], dtype=np.float32)
    key = S_tok
    if key not in _CACHE:
        _CACHE[key] = build(S_tok)
    nc = _CACHE[key]
    shared = {name: np.ascontiguousarray(np.asarray(inputs[name], dtype=np.float32)) for name, _ in PARAMS}
    ncores = 8
    in_maps = []
    for c in range(ncores):
        b = c % B
        m = dict(shared)
        m["x"] = np.ascontiguousarray(x[b])
        m["p"] = np.ascontiguousarray(p[:, b])
        in_maps.append(m)
    res = run_bass_kernel_spmd(nc, in_maps, core_ids=list(range(ncores)))
    out = np.stack([np.asarray(res.results[b]["out"], dtype=np.float32) for b in range(B)], axis=0)
    return out.astype(np.float32)
```

```python
import contextlib
import math
import numpy as np
import concourse.bass as bass
import concourse.mybir as mybir
from concourse.bass_utils import run_bass_kernel_spmd

F32 = mybir.dt.float32
BF16 = mybir.dt.bfloat16
I32 = mybir.dt.int32
AF = mybir.ActivationFunctionType
ALU = mybir.AluOpType
AX = mybir.AxisListType

ENGS = ("pe", "act", "dve", "pool", "sp")
D = 1024
DIN = 3080
FFD = 2816
FFE = 3584
NEXP = 8
EPS = 1e-6
NDSEM = 56


class Buf:
    def __init__(self, name, ap=None, dsem=None):
        self.name = name
        self.ap = ap
        self.w = None
        self.r = {}
        self.dsem = dsem


class Ring:
    def __init__(self, bufs):
        self.bufs = bufs
        self.i = -1

    def next(self):
        self.i = (self.i + 1) % len(self.bufs)
        return self.bufs[self.i]


class Sched:
    def __init__(self, nc, stack):
        self.nc = nc
        self.stack = stack
        self.stacks = [stack]
        self.sems = {}
        self.cnt = {}
        self.prog = {e: [] for e in ENGS}
        self.seen = {e: {} for e in ENGS}
        for e in ENGS:
            self.newsem("E_" + e)
        self.free_ds = [self.newsem("D_%d" % i) for i in range(NDSEM)]
        self.scope_ds = [[]]
        self.banks = []
        self.bank_i = -1
        self.reserved = set()

    def newsem(self, key):
        h = self.stack.enter_context(self.nc.semaphore(key))
        self.sems[key] = h
        self.cnt[key] = 0
        return key

    def sbuf(self, name, shape, dtype, dma=False):
        self.uid = getattr(self, "uid", 0) + 1
        name = "%s_u%d" % (name, self.uid)
        t = self.stacks[-1].enter_context(self.nc.sbuf_tensor(name, list(shape), dtype))
        b = Buf(name, t)
        if dma:
            b.dsem = self.free_ds.pop()
            self.scope_ds[-1].append(b.dsem)
        return b

    def ring(self, name, shape, dtype, n, dma=False):
        return Ring([self.sbuf("%s_%d" % (name, i), shape, dtype, dma) for i in range(n)])

    def dram(self, name, shape, dtype):
        t = self.nc.dram_tensor(name, list(shape), dtype, kind="Internal")
        return Buf(name, t.ap())

    def make_banks(self):
        for i in range(8):
            t = self.stack.enter_context(self.nc.psum_tensor("bank%d" % i, [128, 512], F32))
            self.banks.append(Buf("bank%d" % i, t))

    def pbank(self):
        for _ in range(8):
            self.bank_i = (self.bank_i + 1) % 8
            if self.bank_i not in self.reserved:
                return self.banks[self.bank_i]
        raise RuntimeError("no psum bank")

    def reserve(self, n):
        out = []
        for i in range(8):
            if i not in self.reserved and len(out) < n:
                self.reserved.add(i)
                out.append(self.banks[i])
        return out

    def release(self, banks):
        for b in banks:
            self.reserved.discard(self.banks.index(b))

    @contextlib.contextmanager
    def scope(self):
        st = contextlib.ExitStack()
        self.stacks.append(st)
        self.scope_ds.append([])
        try:
            yield
        finally:
            self.barrier()
            self.free_ds.extend(self.scope_ds.pop())
            self.stacks.pop()
            st.close()

    def _waits(self, eng, reads, writes):
        need = {}

        def add(tok):
            if tok is None:
                return
            k, v = tok
            if eng == "pe" and k == "E_pe":
                return
            if v > need.get(k, 0):
                need[k] = v

        for b in reads:
            add(b.w)
        for b in writes:
            add(b.w)
            for k, v in b.r.items():
                add((k, v))
        out = []
        for k, v in need.items():
            if k.startswith("D_"):
                v = self.cnt[k]
            if self.seen[eng].get(k, 0) >= v:
                continue
            self.seen[eng][k] = v
            out.append((k, v))
        return out

    def _commit(self, tok, reads, writes):
        k, v = tok
        for b in reads:
            if v > b.r.get(k, 0):
                b.r[k] = v
        for b in writes:
            b.w = tok
            b.r = {}

    def op(self, eng, fn, reads=(), writes=()):
        waits = self._waits(eng, reads, writes)
        k = "E_" + eng
        self.cnt[k] += 1
        tok = (k, self.cnt[k])
        self.prog[eng].append((waits, fn, k, 1, 1))
        self._commit(tok, reads, writes)
        return tok

    def dma(self, fn, semb, reads=(), writes=(), n=1, q="sp"):
        waits = self._waits(q, reads, writes)
        k = semb.dsem
        self.cnt[k] += 16 * n
        tok = (k, self.cnt[k])
        self.prog[q].append((waits, fn, k, 16, n))
        self._commit(tok, reads, writes)
        return tok

    def barrier(self):
        for e in ENGS:
            out = []
            for k, v in self.cnt.items():
                if v == 0 or (e == "pe" and k == "E_pe"):
                    continue
                if self.seen[e].get(k, 0) >= v:
                    continue
                self.seen[e][k] = v
                out.append((k, v))
            if out:
                self.prog[e].append((out, None, None, 0, 0))

    def emit(self, block):
        def run(e):
            def body(engine):
                for waits, fn, k, inc, n in self.prog[e]:
                    for wk, wv in waits:
                        engine.wait_ge(self.sems[wk], wv)
                    if fn is None:
                        continue
                    r = fn(engine)
                    if inc == 16:
                        assert len(r) == n, (len(r), n)
                        for ins in r:
                            ins.then_inc(self.sems[k], 16)
                    else:
                        r.then_inc(self.sems[k], 1)
            return body
        block.tensor(run("pe"))
        block.scalar(run("act"))
        block.vector(run("dve"))
        block.gpsimd(run("pool"))
        block.sync(run("sp"))


PARAMS = [
    ("mix_norm_g", (2, 1024)), ("w_in", (2, 1024, 3080)), ("b_igate", (2, 4)), ("b_fgate", (2, 4)),
    ("m_qk_conv_w", (2, 4, 512)), ("m_out_norm_g", (2, 256)), ("c_conv_w", (2, 31, 256)),
    ("c_conv_b", (2, 256)), ("c_ln_g", (2, 256)), ("c_ln_b", (2, 256)), ("a_q_norm_g", (2, 64)),
    ("a_k_norm_g", (2, 64)), ("a_lambda_q1", (2, 64)), ("a_lambda_k1", (2, 64)), ("a_lambda_q2", (2, 64)),
    ("a_lambda_k2", (2, 64)), ("a_subln_g", (2, 128)), ("w_out", (2, 1024, 1024)), ("ffn_norm_g", (2, 1024)),
    ("dense_w_gate", (1, 1024, 2816)), ("dense_w_up", (1, 1024, 2816)), ("dense_w_down", (1, 2816, 1024)),
    ("router_w", (1, 1024, 8)), ("moe_w_gate", (1, 8, 1024, 3584)), ("moe_w_up", (1, 8, 1024, 3584)),
    ("moe_w_down", (1, 8, 3584, 1024)), ("ple_norm_g", (2, 1024)), ("w_ple_gate", (2, 1024, 1024)),
    ("w_ple_proj", (2, 256, 1024)),
]


def build(S_tok, layers=(0, 1), taps=(), nexp=NEXP, skip=()):
    NT = S_tok // 128
    NG = S_tok // 512
    NCH = S_tok // 64
    nc = bass.Bass("TRN2", target_bir_lowering=False)
    I = {}
    I["x"] = nc.dram_tensor("x", [S_tok, D], F32, kind="ExternalInput").ap()
    I["p"] = nc.dram_tensor("p", [2, S_tok, 256], F32, kind="ExternalInput").ap()
    for name, shp in PARAMS:
        I[name] = nc.dram_tensor(name, list(shp), F32, kind="ExternalInput").ap()
    out_ap = nc.dram_tensor("out", [S_tok, D], F32, kind="ExternalOutput").ap()
    tap_aps = {}
    if "yT" in taps:
        tap_aps["yT"] = nc.dram_tensor("tap_yT", [1024, S_tok], BF16, kind="ExternalOutput").ap()

    with contextlib.ExitStack() as st:
        S = Sched(nc, st)
        S.make_banks()
        IN = {k: Buf("in_" + k, v) for k, v in I.items()}
        OUT = Buf("out", out_ap)

        def ACT(out, in_, func, R, W, **kw):
            S.op("act", lambda e: e.activation(out=out, in_=in_, func=func, **kw), R, W)

        def TT(eng, out, a, b, op, R, W):
            S.op(eng, lambda e: e.tensor_tensor(out=out, in0=a, in1=b, op=op), R, W)

        def TS(eng, out, a, s1, s2, op0, op1, R, W):
            if s2 is None:
                S.op(eng, lambda e: e.tensor_scalar(out=out, in0=a, scalar1=s1, scalar2=None, op0=op0), R, W)
            else:
                S.op(eng, lambda e: e.tensor_scalar(out=out, in0=a, scalar1=s1, scalar2=s2, op0=op0, op1=op1), R, W)

        def STT(eng, out, a, s, b, op0, op1, R, W):
            S.op(eng, lambda e: e.scalar_tensor_tensor(out=out, in0=a, scalar=s, in1=b, op0=op0, op1=op1), R, W)

        def CP(eng, out, in_, R, W):
            if eng == "act":
                S.op("act", lambda e: e.copy(out=out, in_=in_), R, W)
            else:
                S.op(eng, lambda e: e.tensor_copy(out=out, in_=in_), R, W)

        def MSET(eng, ap, val, W):
            S.op(eng, lambda e: e.memset(ap, val), (), W)

        def MM(items, R, W):
            def f(e):
                r = None
                for it in items:
                    (o, l, rh, s0, s1) = it[:5]
                    if len(it) > 5:
                        r = e.matmul(o, lhsT=l, rhs=rh, start=s0, stop=s1, skip_group_check=True)
                    else:
                        r = e.matmul(o, lhsT=l, rhs=rh, start=s0, stop=s1)
                return r
            S.op("pe", f, R, W)

        def TRN(items, R, W):
            def f(e):
                r = None
                for (o, i_) in items:
                    r = e.transpose(out=o, in_=i_, identity=ident.ap[0:i_.shape[0], 0:i_.shape[0]])
                return r
            S.op("pe", f, list(R) + [ident], W)

        def LD(out, in_, semb, R, W, q="sp"):
            S.dma(lambda e: [e.dma_start(out=out, in_=in_)], semb, R, W, 1, q)

        def LDS(pairs, semb, R, W, q="sp", slow=False):
            def f(e):
                if slow:
                    return [e.dma_start(out=o, in_=i_, allow_slow_non_contiguous=True) for (o, i_) in pairs]
                return [e.dma_start(out=o, in_=i_) for (o, i_) in pairs]
            S.dma(f, semb, R, W, len(pairs), q)

        def bview(bank):
            return bank.ap[:].bitcast(BF16)

        def rsqrt_act(out, in_, scale, R, W):
            ACT(out, in_, AF.Ln, R, W, scale=scale, bias=epsb.ap[0:in_.shape[0], 0:1])
            ACT(out, out, AF.Exp, W, W, scale=-0.5)

        ident = S.sbuf("ident", [128, 128], BF16)
        identf = S.sbuf("identf", [128, 128], F32)
        triT = S.sbuf("triT", [128, 128], F32)
        triTb = S.sbuf("triTb", [128, 128], BF16)
        triU = S.sbuf("triU", [128, 128], BF16)
        selA = S.sbuf("selA", [128, 128], F32)
        selB = S.sbuf("selB", [128, 128], F32)
        onesln = S.sbuf("onesln", [128, 128], F32)
        epsb = S.sbuf("epsb", [128, 1], F32)
        oneb = S.sbuf("oneb", [128, 1], F32)
        ln8b = S.sbuf("ln8b", [128, 1], F32)
        kcol_i = S.sbuf("kcol_i", [128, 1], I32)
        kcol = S.sbuf("kcol", [128, 1], F32)
        NDD = NT + 4
        abt = S.sbuf("abt", [128, 4, NDD], F32)
        A_tm = S.sbuf("A_tm", [128, NT, 4], F32)
        Gb = S.sbuf("Gb", [128, NCH, 4], F32)

        def mk_consts(e):
            e.memset(identf.ap[:], 1.0)
            e.affine_select(out=identf.ap[:], in_=identf.ap[:], compare_op=ALU.is_ge, fill=0.0,
                            base=0, pattern=[[-1, 128]], channel_multiplier=1)
            e.affine_select(out=identf.ap[:], in_=identf.ap[:], compare_op=ALU.is_ge, fill=0.0,
                            base=0, pattern=[[1, 128]], channel_multiplier=-1)
            e.memset(triT.ap[:], 1.0)
            e.affine_select(out=triT.ap[:], in_=triT.ap[:], compare_op=ALU.is_ge, fill=0.0,
                            base=0, pattern=[[1, 128]], channel_multiplier=-1)
            e.memset(triT.ap[0:64, 64:128], 0.0)
            e.memset(selA.ap[:], 0.0)
            e.memset(selA.ap[0:64, :], 1.0)
            e.memset(selB.ap[:], 0.0)
            e.memset(selB.ap[64:128, :], 1.0)
            e.memset(onesln.ap[:], 1.0 / 256.0)
            e.memset(epsb.ap[:], EPS)
            e.memset(oneb.ap[:], 1.0)
            e.memset(ln8b.ap[:], -math.log(8.0))
            return e.iota(kcol_i.ap[:], pattern=[[0, 1]], base=0, channel_multiplier=1)
        S.op("pool", mk_consts, (), [identf, triT, selA, selB, onesln, epsb, oneb, ln8b, kcol_i])
        CP("pool", ident.ap[:], identf.ap[:], [identf], [ident])
        CP("pool", triTb.ap[:], triT.ap[:], [triT], [triTb])
        CP("pool", kcol.ap[:], kcol_i.ap[:], [kcol_i], [kcol])

        def mk_triU(e):
            e.memset(triU.ap[:], 1.0)
            return e.affine_select(out=triU.ap[:], in_=triU.ap[:], compare_op=ALU.is_ge, fill=0.0,
                                   base=0, pattern=[[1, 128]], channel_multiplier=-1)
        S.op("pool", mk_triU, (), [triU])
        for h in range(4):
            slope = 2.0 ** (-8.0 * (h + 1) / 4)
            for di in range(NDD):
                dd = di - NT
                TS("pool", abt.ap[:, h, di:di + 1], kcol.ap[:], slope, slope * (128.0 * dd - 256.0),
                   ALU.mult, ALU.add, [kcol], [abt])

        Wb = {}
        Wb["w_in"] = S.dram("wb_in", [2, 1024, DIN], BF16)
        Wb["w_out"] = S.dram("wb_out", [2, 1024, 1024], BF16)
        Wb["dense_w_gate"] = S.dram("wb_dg", [1, 1024, FFD], BF16)
        Wb["dense_w_up"] = S.dram("wb_du", [1, 1024, FFD], BF16)
        Wb["dense_w_down"] = S.dram("wb_dd", [1, FFD, 1024], BF16)
        Wb["moe_w_gate"] = S.dram("wb_mg", [1, 8, 1024, FFE], BF16)
        Wb["moe_w_up"] = S.dram("wb_mu", [1, 8, 1024, FFE], BF16)
        Wb["moe_w_down"] = S.dram("wb_md", [1, 8, FFE, 1024], BF16)
        Wb["w_ple_gate"] = S.dram("wb_pg", [2, 1024, 1024], BF16)
        Wb["w_ple_proj"] = S.dram("wb_pp", [2, 256, 1024], BF16)
        h1 = S.dram("h1", [S_tok, D], F32)
        mqT = S.dram("mqT", [256, S_tok], BF16)
        mkT = S.dram("mkT", [256, S_tok], BF16)
        mktm = S.dram("mktm", [S_tok, 256], BF16)
        mrv = S.dram("mrv", [S_tok, 4, 65], BF16)
        mso = S.dram("mso", [S_tok, 256], BF16)
        aqT = S.dram("aqT", [512, S_tok], BF16)
        akT = S.dram("akT", [512, S_tok], BF16)
        avd = S.dram("avd", [S_tok, 512], BF16)
        yT = S.dram("yT", [1024, S_tok], BF16)

        block = st.enter_context(nc.Block())

        def convert_bufs():
            return (S.ring("cvf", [128, 3584], F32, 3, dma=True), S.ring("cvb", [128, 3584], BF16, 3, dma=True))

        def convert_gen(names_layers, bufs, engs=("act", "dve", "pool")):
            CB = 3584
            fr, br = bufs
            ei = 0
            for (name, idx) in names_layers:
                src = IN[name].ap
                dst = Wb[name].ap
                for ix in idx:
                    src = src[ix]
                    dst = dst[ix]
                R_, C_ = src.shape
                for r0 in range(0, R_, 128):
                    for c0 in range(0, C_, CB):
                        cw = min(CB, C_ - c0)
                        fb = fr.next()
                        bb = br.next()
                        LD(fb.ap[:, 0:cw], src[r0:r0 + 128, c0:c0 + cw], fb, [IN[name]], [fb])
                        eng = engs[ei % len(engs)]
                        ei += 1
                        CP(eng, bb.ap[:, 0:cw], fb.ap[:, 0:cw], [fb], [bb])
                        LD(dst[r0:r0 + 128, c0:c0 + cw], bb.ap[:, 0:cw], bb, [bb], [Wb[name]], q="pool")
                        yield

        def convert(names_layers):
            with S.scope():
                for _ in convert_gen(names_layers, convert_bufs()):
                    pass

        def phase_A(L, hsrc):
            with S.scope():
                win = S.sbuf("win", [128, 8, DIN], BF16, dma=True)
                wv = Wb["w_in"].ap[L].rearrange("(k p) n -> p k n", p=128)
                LDS([(win.ap[:, :, c:c + 770], wv[:, :, c:c + 770]) for c in range(0, DIN, 770)],
                    win, [Wb["w_in"]], [win])
                prm = S.sbuf("prmA", [128, 1], F32, dma=True)
                gbc = S.sbuf("gbcA", [128, 1024], F32)
                bif = S.sbuf("bif", [128, 8], F32)
                wq4 = S.sbuf("wq4", [128, 4, 4], F32)
                wc31 = S.sbuf("wc31", [128, 2, 31], F32)
                cvec = S.sbuf("cvec", [128, 3, 2], F32)
                gq = S.sbuf("gq", [128, 64], F32)
                gk = S.sbuf("gk", [128, 64], F32)
                LDS([(gbc.ap[:], I["mix_norm_g"][L].partition_broadcast(128)),
                     (bif.ap[:, 0:4], I["b_igate"][L].partition_broadcast(128)),
                     (bif.ap[:, 4:8], I["b_fgate"][L].partition_broadcast(128)),
                     (gq.ap[:], I["a_q_norm_g"][L].partition_broadcast(128)),
                     (gk.ap[:], I["a_k_norm_g"][L].partition_broadcast(128))],
                    prm, [], [gbc, bif, gq, gk])
                LDS([(wq4.ap[:, t, :], I["m_qk_conv_w"][L][:, t * 128:(t + 1) * 128].rearrange("j p -> p j"))
                     for t in range(4)] +
                    [(wc31.ap[:, t, :], I["c_conv_w"][L][:, t * 128:(t + 1) * 128].rearrange("j p -> p j"))
                     for t in range(2)] +
                    [(cvec.ap[:, 0, :], I["c_conv_b"][L].rearrange("(t p) -> p t", p=128)),
                     (cvec.ap[:, 1, :], I["c_ln_g"][L].rearrange("(t p) -> p t", p=128)),
                     (cvec.ap[:, 2, :], I["c_ln_b"][L].rearrange("(t p) -> p t", p=128))],
                    prm, [], [wq4, wc31, cvec], slow=True)
                dq = S.sbuf("dq", [128, 16, 128], BF16)
                dc = S.sbuf("dc", [128, 62, 128], BF16)
                for c in range(4):
                    for j in range(4):
                        TS("pool", dq.ap[:, c * 4 + j, :], identf.ap[:], wq4.ap[:, c, j:j + 1], None, ALU.mult, None,
                           [identf, wq4], [dq])
                for c in range(2):
                    for j in range(31):
                        TS("pool", dc.ap[:, c * 31 + j, :], identf.ap[:], wc31.ap[:, c, j:j + 1], None, ALU.mult, None,
                           [identf, wc31], [dc])

                hin = S.ring("hinA", [128, 1024], F32, 6, dma=True)
                junk = S.ring("junkA", [128, 1024], BF16, 2)
                ssr = S.ring("ssA", [128, 4], F32, 2)
                abf = S.ring("abfA", [128, 1024], BF16, 2)
                aTr = S.ring("aTA", [128, 8, 512], BF16, 2)
                xqk = S.sbuf("xqk", [128, 4, 515], BF16)
                zbuf = S.sbuf("zbuf", [128, 2, 542], BF16)
                qko = S.ring("qko", [128, 512], BF16, 3, dma=True)
                ktmo = S.ring("ktmo", [128, 4, 256], BF16, 2, dma=True)
                sgr = S.ring("sgr", [128, 512], F32, 2)
                zc = S.sbuf("zc", [128, 2, 512], F32)
                zc2 = S.sbuf("zc2", [128, 2, 512], F32)
                mean_sb = S.sbuf("mean_sb", [128, 512], F32)
                var_sb = S.sbuf("var_sb", [128, 512], F32)
                dtmp = S.ring("dtmp", [128, 512], F32, 2)
                yco = S.ring("yco", [128, 512], BF16, 2, dma=True)
                gat = S.ring("gat", [128, 16], F32, 2)
                rvo = S.ring("rvo", [128, 4, 65], BF16, 2, dma=True)
                soo = S.ring("soo", [128, 256], BF16, 2, dma=True)
                sqj = S.ring("sqj", [128, 512], F32, 2)
                ssq = S.ring("ssq", [128, 16], F32, 2)
                qn1 = S.ring("qn1", [128, 512], F32, 2)
                qnb = S.ring("qnb", [128, 512], BF16, 2)
                qTo = S.ring("qTo", [128, 4, 512], BF16, 2, dma=True)
                kTo = S.ring("kTo", [128, 4, 512], BF16, 2, dma=True)
                vo = S.ring("vo", [128, 512], BF16, 2, dma=True)

                MSET("pool", xqk.ap[:, :, 0:3], 0.0, [xqk])
                MSET("pool", zbuf.ap[:, :, 0:30], 0.0, [zbuf])

                for g in range(NG):
                    t0g = g * 512
                    aT = aTr.next()
                    ss = ssr.next()
                    hbs = []
                    for j in range(4):
                        hb = hin.next()
                        hbs.append(hb)
                        r0 = t0g + j * 128
                        LD(hb.ap[:], hsrc.ap[r0:r0 + 128, :], hb, [hsrc], [hb])
                        jk = junk.next()
                        ACT(jk.ap[:], hb.ap[:], AF.Square, [hb], [jk, ss], accum_out=ss.ap[:, j:j + 1])
                    rsqrt_act(ss.ap[:], ss.ap[:], 1.0 / 1024, [ss], [ss])
                    for j in range(4):
                        ab = abf.next()
                        STT("dve", ab.ap[:], hbs[j].ap[:], ss.ap[:, j:j + 1], gbc.ap[:], ALU.mult, ALU.mult,
                            [hbs[j], ss, gbc], [ab])
                        pb = S.pbank()
                        pv = bview(pb).rearrange("p (k n) -> p k n", k=8)
                        TRN([(pv[:, k, :], ab.ap[:, k * 128:(k + 1) * 128]) for k in range(8)], [ab], [pb])
                        CP("act" if j % 2 == 0 else "dve", aT.ap[:, :, j * 128:(j + 1) * 128], pv, [pb], [aT])

                    def fm(col0):
                        pb = S.pbank()
                        MM([(pb.ap[:], win.ap[:, k, col0:col0 + 128], aT.ap[:, k, :], k == 0, k == 7)
                            for k in range(8)], [win, aT], [pb])
                        return pb

                    def tm(j, col0, n):
                        pb = S.pbank()
                        MM([(pb.ap[:, 0:n], aT.ap[:, k, j * 128:(j + 1) * 128], win.ap[:, k, col0:col0 + n],
                             k == 0, k == 7) for k in range(8)], [win, aT], [pb])
                        return pb

                    if g > 0:
                        CP("pool", xqk.ap[:, :, 0:3], xqk.ap[:, :, 512:515], [xqk], [xqk])
                    for c in range(4):
                        pb = fm(c * 128)
                        CP("act", xqk.ap[:, c, 3:515], pb.ap[:], [pb], [xqk])
                    kt = ktmo.next()
                    for c in range(4):
                        pb = S.pbank()
                        MM([(pb.ap[:], dq.ap[:, c * 4 + j, :], xqk.ap[:, c, j:j + 512], j == 0, j == 3)
                            for j in range(4)], [dq, xqk], [pb])
                        qo = qko.next()
                        ACT(qo.ap[:], pb.ap[:], AF.Silu, [pb], [qo])
                        dstT = (mqT if c < 2 else mkT)
                        LD(dstT.ap[(c % 2) * 128:(c % 2 + 1) * 128, t0g:t0g + 512], qo.ap[:], qo, [qo], [dstT], q="pool")
                        if c >= 2:
                            pb2 = S.pbank()
                            pv2 = bview(pb2).rearrange("p (k n) -> p k n", k=8)
                            TRN([(pv2[:, j, :], qo.ap[:, j * 128:(j + 1) * 128]) for j in range(4)], [qo], [pb2])
                            CP("dve", kt.ap[:, :, (c - 2) * 128:(c - 1) * 128], pv2[:, 0:4, :], [pb2], [kt])
                    LD(mktm.ap[t0g:t0g + 512, :].rearrange("(j p) c -> p j c", p=128), kt.ap[:], kt, [kt], [mktm], q="pool")

                    if g > 0:
                        CP("pool", zbuf.ap[:, :, 0:30], zbuf.ap[:, :, 512:542], [zbuf], [zbuf])
                    for c in range(2):
                        pa = fm(1032 + c * 128)
                        pg = fm(1288 + c * 128)
                        sg = sgr.next()
                        ACT(sg.ap[:], pg.ap[:], AF.Sigmoid, [pg], [sg])
                        TT("dve", zbuf.ap[:, c, 30:542], pa.ap[:], sg.ap[:], ALU.mult, [pa, sg], [zbuf])
                    for c in range(2):
                        pb = S.pbank()
                        MM([(pb.ap[:], dc.ap[:, c * 31 + j, :], zbuf.ap[:, c, j:j + 512], j == 0, j == 30)
                            for j in range(31)], [dc, zbuf], [pb])
                        ACT(zc.ap[:, c, :], pb.ap[:], AF.Identity, [pb, cvec], [zc], bias=cvec.ap[:, 0, c:c + 1])
                        ACT(zc2.ap[:, c, :], zc.ap[:, c, :], AF.Square, [zc], [zc2])
                    pm = S.pbank()
                    MM([(pm.ap[:], onesln.ap[:], zc.ap[:, c, :], c == 0, c == 1) for c in range(2)], [onesln, zc], [pm])
                    pv_ = S.pbank()
                    MM([(pv_.ap[:], onesln.ap[:], zc2.ap[:, c, :], c == 0, c == 1) for c in range(2)], [onesln, zc2], [pv_])
                    CP("act", mean_sb.ap[:], pm.ap[:], [pm], [mean_sb])
                    TT("dve", var_sb.ap[:], mean_sb.ap[:], mean_sb.ap[:], ALU.mult, [mean_sb], [var_sb])
                    TT("dve", var_sb.ap[:], pv_.ap[:], var_sb.ap[:], ALU.subtract, [pv_, var_sb], [var_sb])
                    rsqrt_act(var_sb.ap[:], var_sb.ap[:], 1.0, [var_sb], [var_sb])
                    for c in range(2):
                        dt_ = dtmp.next()
                        TT("dve", dt_.ap[:], zc.ap[:, c, :], mean_sb.ap[:], ALU.subtract, [zc, mean_sb], [dt_])
                        TT("dve", dt_.ap[:], dt_.ap[:], var_sb.ap[:], ALU.mult, [dt_, var_sb], [dt_])
                        yo = yco.next()
                        ACT(yo.ap[:], dt_.ap[:], AF.Silu, [dt_, cvec], [yo], scale=cvec.ap[:, 1, c:c + 1],
                            bias=cvec.ap[:, 2, c:c + 1])
                        LD(yT.ap[256 + c * 128:256 + (c + 1) * 128, t0g:t0g + 512], yo.ap[:], yo, [yo], [yT], q="pool")

                    qT_ = qTo.next()
                    kT_ = kTo.next()
                    for j in range(4):
                        t = g * 4 + j
                        r0 = t * 128
                        pvo = tm(j, 512, 512)
                        pif = tm(j, 1024, 8)
                        ga = gat.next()
                        TT("dve", ga.ap[:, 0:8], pif.ap[:, 0:8], bif.ap[:], ALU.add, [pif, bif], [ga])
                        ACT(ga.ap[:, 4:8], ga.ap[:, 4:8], AF.Exp, [ga], [ga], scale=-1.0)
                        ACT(ga.ap[:, 4:8], ga.ap[:, 4:8], AF.Ln, [ga], [ga], bias=oneb.ap[:, 0:1])
                        pc = S.pbank()
                        MM([(pc.ap[:, 0:4], triT.ap[:], ga.ap[:, 4:8], True, True),
                            (pc.ap[:, 4:8], selA.ap[:], ga.ap[:, 4:8], True, True),
                            (pc.ap[:, 8:12], selB.ap[:], ga.ap[:, 4:8], True, True)], [triT, selA, selB, ga], [pc])
                        ACT(A_tm.ap[:, t, :], pc.ap[:, 0:4], AF.Exp, [pc], [A_tm], scale=-1.0, bias=ln8b.ap[:, 0:1])
                        ACT(Gb.ap[:, 2 * t:2 * t + 2, :], pc.ap[:, 4:12].rearrange("p (a b) -> p a b", a=2), AF.Exp,
                            [pc], [Gb], scale=-1.0)
                        TT("dve", ga.ap[:, 8:12], ga.ap[:, 0:4], pc.ap[:, 0:4], ALU.add, [ga, pc], [ga])
                        ACT(ga.ap[:, 8:12], ga.ap[:, 8:12], AF.Exp, [ga], [ga])
                        rv = rvo.next()
                        TT("dve", rv.ap[:, :, 0:64], pvo.ap[:, 0:256].rearrange("p (h d) -> p h d", h=4),
                           ga.ap[:, 8:12].unsqueeze(2).to_broadcast([128, 4, 64]), ALU.mult, [pvo, ga], [rv])
                        CP("dve", rv.ap[:, :, 64:65], ga.ap[:, 8:12].unsqueeze(2), [ga], [rv])
                        LD(mrv.ap[r0:r0 + 128, :, :], rv.ap[:], rv, [rv], [mrv], q="pool")
                        so = soo.next()
                        ACT(so.ap[:], pvo.ap[:, 256:512], AF.Sigmoid, [pvo], [so])
                        LD(mso.ap[r0:r0 + 128, :], so.ap[:], so, [so], [mso], q="pool")
                        pq = tm(j, 1544, 512)
                        pk = tm(j, 2056, 512)
                        pvv = tm(j, 2568, 512)
                        sq_ = ssq.next()
                        for (pp_, off) in ((pq, 0), (pk, 8)):
                            sj = sqj.next()
                            ACT(sj.ap[:], pp_.ap[:], AF.Square, [pp_], [sj])
                            S.op("dve", (lambda sj=sj, off=off, sq_=sq_: lambda e: e.reduce_sum(
                                out=sq_.ap[:, off:off + 8], in_=sj.ap[:].rearrange("p (m d) -> p m d", m=8),
                                axis=AX.X))(), [sj], [sq_])
                        rsqrt_act(sq_.ap[:], sq_.ap[:], 1.0 / 64, [sq_], [sq_])
                        for (pp_, off, gg, dstT_) in ((pq, 0, gq, qT_), (pk, 8, gk, kT_)):
                            q1 = qn1.next()
                            TT("dve", q1.ap[:].rearrange("p (m d) -> p m d", m=8),
                               pp_.ap[:].rearrange("p (m d) -> p m d", m=8),
                               sq_.ap[:, off:off + 8].unsqueeze(2).to_broadcast([128, 8, 64]), ALU.mult,
                               [pp_, sq_], [q1])
                            qb = qnb.next()
                            TT("pool", qb.ap[:].rearrange("p (m d) -> p m d", m=8),
                               q1.ap[:].rearrange("p (m d) -> p m d", m=8),
                               gg.ap[:].unsqueeze(1).to_broadcast([128, 8, 64]), ALU.mult, [q1, gg], [qb])
                            pb = S.pbank()
                            pvw = bview(pb).rearrange("p (k n) -> p k n", k=8)
                            TRN([(pvw[:, m, :], qb.ap[:, m * 128:(m + 1) * 128]) for m in range(4)], [qb], [pb])
                            CP("act", dstT_.ap[:, :, j * 128:(j + 1) * 128], pvw[:, 0:4, :], [pb], [dstT_])
                        v_ = vo.next()
                        CP("act", v_.ap[:], pvv.ap[:], [pvv], [v_])
                        LD(avd.ap[r0:r0 + 128, :], v_.ap[:], v_, [v_], [avd], q="pool")
                    LD(aqT.ap[:, t0g:t0g + 512].rearrange("(m p) s -> p m s", p=128), qT_.ap[:], qT_, [qT_], [aqT], q="pool")
                    LD(akT.ap[:, t0g:t0g + 512].rearrange("(m p) s -> p m s", p=128), kT_.ap[:], kT_, [kT_], [akT], q="pool")

        def phase_B(L):
            with S.scope():
                prm = S.sbuf("prmB", [128, 1], F32, dma=True)
                gm = S.sbuf("gmB", [128, 256], F32)
                LDS([(gm.ap[:], I["m_out_norm_g"][L].partition_broadcast(128))], prm, [], [gm])
                qTh = S.sbuf("qTh", [64, S_tok], BF16, dma=True)
                kTh = S.sbuf("kTh", [64, S_tok], BF16, dma=True)
                ktm = S.sbuf("ktmB", [128, NT, 64], BF16, dma=True)
                rvbd = S.sbuf("rvbd", [128, NT, 130], BF16, dma=True)
                soh = S.sbuf("soh", [128, NT, 64], BF16, dma=True)
                gso = S.sbuf("gso", [128, NT, 64], F32)
                X = S.sbuf("Xst", [64, NCH, 65], F32)
                Cst = S.sbuf("Cst", [64, NCH + 1, 65], BF16)
                yTm = S.sbuf("yTm", [64, S_tok], BF16, dma=True)
                smt = S.ring("smt", [128, 128], BF16, 3)
                ndr = S.ring("ndr", [128, 65], F32, 3)
                sm = S.ring("smB", [128, 8], F32, 3)
                jk = S.ring("jkB", [128, 64], F32, 2)
                ybr = S.ring("ybB", [128, 64], BF16, 3)
                MSET("pool", rvbd.ap[:], 0.0, [rvbd])
                MSET("pool", Cst.ap[:, 0, :], 0.0, [Cst])
                for h in range(4):
                    LD(qTh.ap[:], mqT.ap[h * 64:(h + 1) * 64, :], qTh, [mqT], [qTh])
                    LD(kTh.ap[:], mkT.ap[h * 64:(h + 1) * 64, :], kTh, [mkT], [kTh])
                    LDS([(ktm.ap[:], mktm.ap[:, h * 64:(h + 1) * 64].rearrange("(i p) c -> p i c", p=128))],
                        ktm, [mktm], [ktm], slow=True)
                    mv = mrv.ap.rearrange("(i two p) h c -> two p i h c", two=2, p=64)
                    LDS([(rvbd.ap[0:64, :, 0:65], mv[0][:, :, h, :]),
                         (rvbd.ap[64:128, :, 65:130], mv[1][:, :, h, :])], rvbd, [mrv], [rvbd], slow=True)
                    LDS([(soh.ap[:], mso.ap[:, h * 64:(h + 1) * 64].rearrange("(i p) c -> p i c", p=128))],
                        soh, [mso], [soh], slow=True)
                    TT("pool", gso.ap[:], soh.ap[:], gm.ap[:, h * 64:(h + 1) * 64].unsqueeze(1).to_broadcast([128, NT, 64]),
                       ALU.mult, [soh, gm], [gso])
                    for i in range(NT):
                        pb = S.pbank()
                        MM([(pb.ap[0:64, 0:130], ktm.ap[:, i, :], rvbd.ap[:, i, :], True, True)], [ktm, rvbd], [pb])
                        ACT(X.ap[:, 2 * i, :], pb.ap[0:64, 0:65], AF.Copy, [pb, Gb], [X], scale=Gb.ap[0:64, 2 * i, h:h + 1])
                        ACT(X.ap[:, 2 * i + 1, :], pb.ap[0:64, 65:130], AF.Copy, [pb, Gb], [X],
                            scale=Gb.ap[0:64, 2 * i + 1, h:h + 1])
                    for c in range(1, NCH):
                        STT("dve", X.ap[:, c, :], X.ap[:, c - 1, :], Gb.ap[0:64, c, h:h + 1], X.ap[:, c, :],
                            ALU.mult, ALU.add, [X, Gb], [X])
                    CP("act", Cst.ap[:, 1:NCH + 1, :], X.ap[:, :, :], [X], [Cst])
                    for i in range(NT):
                        ts_ = slice(i * 128, (i + 1) * 128)
                        ps = S.pbank()
                        MM([(ps.ap[:, 0:128], kTh.ap[:, ts_], qTh.ap[:, ts_], True, True)], [kTh, qTh], [ps])
                        sm_ = smt.next()
                        TT("dve", sm_.ap[:], ps.ap[:, 0:128], triT.ap[:], ALU.mult, [ps, triT], [sm_])
                        po = S.pbank()
                        MM([(po.ap[:, 0:130], qTh.ap[:, ts_], Cst.ap[:, 2 * i:2 * i + 2, :].rearrange("p a b -> p (a b)"),
                             True, False),
                            (po.ap[:, 0:130], sm_.ap[:], rvbd.ap[:, i, :], False, True)], [qTh, Cst, sm_, rvbd], [po])
                        nd = ndr.next()
                        ACT(nd.ap[0:64, :], po.ap[0:64, 0:65], AF.Copy, [po, A_tm], [nd], scale=A_tm.ap[0:64, i, h:h + 1])
                        ACT(nd.ap[64:128, :], po.ap[64:128, 65:130], AF.Copy, [po, A_tm], [nd],
                            scale=A_tm.ap[64:128, i, h:h + 1])
                        s_ = sm.next()
                        ACT(s_.ap[:, 0:1], nd.ap[:, 64:65], AF.Abs, [nd], [s_])
                        TS("dve", s_.ap[:, 0:1], s_.ap[:, 0:1], 1.0, None, ALU.max, None, [s_], [s_])
                        S.op("dve", (lambda s_=s_: lambda e: e.reciprocal(out=s_.ap[:, 1:2], in_=s_.ap[:, 0:1]))(), [s_], [s_])
                        j_ = jk.next()
                        ACT(j_.ap[:], nd.ap[:, 0:64], AF.Square, [nd, s_], [j_, s_], scale=s_.ap[:, 1:2],
                            accum_out=s_.ap[:, 2:3])
                        rsqrt_act(s_.ap[:, 3:4], s_.ap[:, 2:3], 1.0 / 64, [s_], [s_])
                        TT("dve", s_.ap[:, 4:5], s_.ap[:, 3:4], s_.ap[:, 1:2], ALU.mult, [s_], [s_])
                        yb = ybr.next()
                        STT("dve", yb.ap[:], nd.ap[:, 0:64], s_.ap[:, 4:5], gso.ap[:, i, :], ALU.mult, ALU.mult,
                            [nd, s_, gso], [yb])
                        pt = S.pbank()
                        ptv = bview(pt)
                        TRN([(ptv[0:64, 0:128], yb.ap[:, :])], [yb], [pt])
                        CP("act", yTm.ap[:, ts_], ptv[0:64, 0:128], [pt], [yTm])
                    LD(yT.ap[h * 64:(h + 1) * 64, :], yTm.ap[:], yTm, [yTm], [yT], q="pool")

        def phase_C(L, bg=None, bg_every=4):
            lam_init = 0.8 - 0.6 * math.exp(-0.3 * L)
            with S.scope():
                prm = S.sbuf("prmC", [128, 1], F32, dma=True)
                lv = S.sbuf("lvC", [128, 4, 64], F32)
                gsub = S.sbuf("gsub", [128, 128], F32)
                lam = S.sbuf("lam", [128, 4], F32)
                ljk = S.sbuf("ljk", [128, 64], F32)
                LDS([(lv.ap[:, 0, :], I["a_lambda_q1"][L].partition_broadcast(128)),
                     (lv.ap[:, 1, :], I["a_lambda_k1"][L].partition_broadcast(128)),
                     (lv.ap[:, 2, :], I["a_lambda_q2"][L].partition_broadcast(128)),
                     (lv.ap[:, 3, :], I["a_lambda_k2"][L].partition_broadcast(128)),
                     (gsub.ap[:], I["a_subln_g"][L].partition_broadcast(128))], prm, [], [lv, gsub])
                MSET("dve", lam.ap[:], 0.0, [lam])
                S.op("dve", lambda e: e.tensor_tensor(out=ljk.ap[:], in0=lv.ap[:, 0, :], in1=lv.ap[:, 1, :], op=ALU.mult),
                     [lv], [ljk])
                S.op("dve", lambda e: e.reduce_sum(out=lam.ap[:, 0:1], in_=ljk.ap[:], axis=AX.X), [ljk], [lam])
                S.op("dve", lambda e: e.tensor_tensor(out=ljk.ap[:], in0=lv.ap[:, 2, :], in1=lv.ap[:, 3, :], op=ALU.mult),
                     [lv, lam], [ljk])
                S.op("dve", lambda e: e.reduce_sum(out=lam.ap[:, 1:2], in_=ljk.ap[:], axis=AX.X), [ljk], [lam])
                ACT(lam.ap[:, 0:2], lam.ap[:, 0:2], AF.Exp, [lam], [lam])
                TT("dve", lam.ap[:, 2:3], lam.ap[:, 0:1], lam.ap[:, 1:2], ALU.subtract, [lam], [lam])
                TS("dve", lam.ap[:, 3:4], lam.ap[:, 2:3], lam_init, None, ALU.add, None, [lam], [lam])
                TS("dve", gsub.ap[:], gsub.ap[:], 1.0 - lam_init, None, ALU.mult, None, [gsub], [gsub])

                KT = S.ring("KTC", [128, S_tok], BF16, 2, dma=True)
                VV = S.ring("VVC", [128, NT, 129], BF16, 2, dma=True)
                QT = S.ring("QTC", [128, 512], BF16, 3, dma=True)
                PT = S.ring("PTC", [128, 512], BF16, 6)
                yta = S.ring("ytaC", [128, 512], BF16, 2, dma=True)
                sm = S.ring("smC", [128, 8], F32, 4)
                t2r = S.ring("t2C", [128, 128], F32, 2)
                yar = S.ring("yaC", [128, 128], F32, 2)
                jkr = S.ring("jkC", [128, 128], BF16, 2)
                ybr = S.ring("ybC", [128, 128], BF16, 2)
                for vb in VV.bufs:
                    MSET("pool", vb.ap[:, :, 128:129], 1.0, [vb])
                accb = S.reserve(3)

                def acc(m, j):
                    i_ = m * 4 + j
                    return accb[i_ // 3], (i_ % 3) * 132

                accs_r = S.ring("accsC", [128, 1056], F32, 2)
                fin_sm = S.ring("finsm", [128, 16], F32, 3)
                o_r = S.ring("oC", [128, 8, 128], F32, 2)
                ya_r = S.ring("yaC2", [128, 4, 128], F32, 2)
                sq_r = S.ring("sqC", [128, 4, 128], F32, 2)
                yb_r = S.ring("ybC2", [128, 4, 128], BF16, 2)
                deferred = []

                def finalize(h, qg):
                    ac = accs_r.next()
                    s_ = fin_sm.next()
                    o = o_r.next()
                    ya = ya_r.next()
                    sq = sq_r.next()
                    yb = yb_r.next()
                    yt = yta.next()
                    for bi in range(3):
                        w = 396 if bi < 2 else 264
                        CP("dve", ac.ap[:, bi * 396:bi * 396 + w], accb[bi].ap[:, 0:w], [accb[bi]], [ac])
                    acv = ac.ap[:].rearrange("p (i c) -> p i c", c=132)
                    S.op("dve", lambda e: e.reciprocal(out=s_.ap[:, 0:8].unsqueeze(2), in_=acv[:, :, 128:129]), [ac], [s_])
                    TS("dve", s_.ap[:, 4:8], s_.ap[:, 4:8], lam.ap[:, 3:4], None, ALU.mult, None, [s_, lam], [s_])
                    TT("dve", o.ap[:], acv[:, :, 0:128], s_.ap[:, 0:8].unsqueeze(2).to_broadcast([128, 8, 128]), ALU.mult,
                       [ac, s_], [o])
                    TT("dve", ya.ap[:], o.ap[:, 0:4, :], o.ap[:, 4:8, :], ALU.subtract, [o], [ya])
                    TT("dve", sq.ap[:], ya.ap[:], ya.ap[:], ALU.mult, [ya], [sq])
                    S.op("dve", lambda e: e.reduce_sum(out=s_.ap[:, 8:12], in_=sq.ap[:], axis=AX.X), [sq, s_], [s_])

                    def F2():
                        rsqrt_act(s_.ap[:, 12:16], s_.ap[:, 8:12], 1.0 / 128, [s_], [s_])

                    def F3():
                        TT("dve", sq.ap[:], ya.ap[:], s_.ap[:, 12:16].unsqueeze(2).to_broadcast([128, 4, 128]), ALU.mult,
                           [ya, s_, sq], [sq])
                        TT("dve", yb.ap[:], sq.ap[:], gsub.ap[:].unsqueeze(1).to_broadcast([128, 4, 128]), ALU.mult,
                           [sq, gsub], [yb])
                        pt_ = S.pbank()
                        ptv = bview(pt_).rearrange("p (k n) -> p k n", k=8)
                        TRN([(ptv[:, j, :], yb.ap[:, j, :]) for j in range(4)], [yb], [pt_])
                        CP("dve", yt.ap[:].rearrange("p (j n) -> p j n", j=4), ptv[:, 0:4, :], [pt_], [yt])
                        LD(yT.ap[512 + h * 128:512 + (h + 1) * 128, qg * 512:(qg + 1) * 512], yt.ap[:], yt, [yt], [yT],
                           q="pool")
                    deferred.append([3, F2])
                    deferred.append([6, F3])

                def tick(flush=False):
                    for d_ in list(deferred):
                        d_[0] -= 1
                        if d_[0] <= 0 or flush:
                            deferred.remove(d_)
                            d_[1]()

                def stage1(h, qg, kb, kt, vv, qt):
                    jj = max(0, kb - 4 * qg)
                    c0_ = jj * 128
                    di = kb - 4 * qg + NT
                    pts = []
                    for m in range(2):
                        pb = S.pbank()
                        MM([(pb.ap[:, c0_:512], kt.ap[m * 64:(m + 1) * 64, kb * 128:(kb + 1) * 128],
                             qt.ap[m * 64:(m + 1) * 64, c0_:512], True, True)], [kt, qt], [pb])
                        pt = PT.next()
                        ACT(pt.ap[:, c0_:512], pb.ap[:, c0_:512], AF.Exp, [pb, abt], [pt], scale=0.125,
                            bias=abt.ap[:, h, di:di + 1])
                        if kb >= 4 * qg:
                            TT("pool", pt.ap[:, c0_:c0_ + 128], pt.ap[:, c0_:c0_ + 128], triU.ap[:], ALU.mult,
                               [pt, triU], [pt])
                        pts.append(pt)

                    def stage2():
                        items = []
                        for m in range(2):
                            for j in range(jj, 4):
                                bk, off = acc(m, j)
                                items.append((bk.ap[:, off:off + 129], pts[m].ap[:, j * 128:(j + 1) * 128],
                                              vv.ap[:, kb, :], kb == 0 and off == 0, kb == 4 * qg + j, True))
                        MM(items, pts + [vv], accb)
                        if kb == 4 * qg + 3:
                            finalize(h, qg)
                    return stage2

                pending = None
                ucount = 0
                for h in range(4):
                    kt = KT.next()
                    vv = VV.next()
                    LD(kt.ap[:], akT.ap[h * 128:(h + 1) * 128, :], kt, [akT], [kt])
                    LDS([(vv.ap[:, :, 0:128], avd.ap[:, h * 128:(h + 1) * 128].rearrange("(i p) c -> p i c", p=128))],
                        vv, [avd], [vv])
                    for qg in range(NG):
                        qt = QT.next()
                        LD(qt.ap[:], aqT.ap[h * 128:(h + 1) * 128, qg * 512:(qg + 1) * 512], qt, [aqT], [qt])
                        for kb in range(4 * qg + 4):
                            s2 = stage1(h, qg, kb, kt, vv, qt)
                            if pending is not None:
                                pending()
                            pending = s2
                            ucount += 1
                            tick()
                            if bg is not None and ucount % bg_every == 0:
                                next(bg, None)
                pending()
                tick(flush=True)
                S.release(accb)

        def phase_D(L, hsrc, hdst):
            moe = (L % 2 == 1)
            with S.scope():
                prm = S.sbuf("prmD", [128, 1], F32, dma=True)
                gff = S.sbuf("gffD", [128, 1024], F32)
                gpl = S.sbuf("gplD", [128, 1024], F32)
                LDS([(gff.ap[:], I["ffn_norm_g"][L].partition_broadcast(128)),
                     (gpl.ap[:], I["ple_norm_g"][L].partition_broadcast(128))], prm, [], [gff, gpl])
                wpp = S.sbuf("wppD", [128, 2, 1024], BF16, dma=True)
                LD(wpp.ap[:], Wb["w_ple_proj"].ap[L].rearrange("(k p) n -> p k n", p=128), wpp, [Wb["w_ple_proj"]], [wpp])
                if moe:
                    wrt = S.sbuf("wrtD", [128, 8, 8], F32, dma=True)
                    LDS([(wrt.ap[:], I["router_w"][0].rearrange("(k p) n -> p k n", p=128))], wrt, [], [wrt])
                wblk = S.ring("wblk", [128, 8, 512], BF16, 4, dma=True)
                wdblk = S.ring("wdblk", [128, 4, 512], BF16, 3, dma=True)
                hin = S.ring("hinD", [128, 1024], F32, 5, dma=True)
                hw = [S.sbuf("hw%d" % j, [128, 1024], F32, dma=True) for j in range(4)]
                yTg = S.ring("yTg", [128, 8, 512], BF16, 2, dma=True)
                junk = S.ring("junkD", [128, 1024], BF16, 2)
                ssr = S.ring("ssD", [128, 4], F32, 2)
                cbf = S.ring("cbfD", [128, 1024], BF16, 2)
                cT = S.sbuf("cTD", [128, 8, 512], BF16)
                FT = (FFE if moe else FFD) // 128
                hT = S.sbuf("hTD", [128, FT, 512], BF16)
                sgr = S.ring("sgD", [128, 512], F32, 3)
                pin = S.ring("pinD", [128, 256], F32, 4, dma=True)
                pbf = S.ring("pbfD", [128, 256], BF16, 2)
                pTt = S.sbuf("pTD", [128, 2, 512], BF16)
                gsb = S.ring("gsbD", [128, 512], F32, 2)
                if moe:
                    cf32 = S.ring("cf32", [128, 1024], F32, 2)
                    cTf = S.ring("cTf", [128, 8, 128], F32, 2)
                    rl = S.ring("rlD", [128, 8], F32, 2)
                    mx8 = S.ring("mx8", [128, 8], F32, 2)
                    rex = S.ring("rexD", [128, 8], F32, 2)
                    rsm = S.ring("rsmD", [128, 4], F32, 2)
                    comb = [S.sbuf("comb%d" % j, [128, 8], F32) for j in range(4)]

                def norm_T(gb_, dst, extra=None):
                    ss = ssr.next()
                    for j in range(4):
                        jk = junk.next()
                        ACT(jk.ap[:], hw[j].ap[:], AF.Square, [hw[j]], [jk, ss], accum_out=ss.ap[:, j:j + 1])
                    rsqrt_act(ss.ap[:], ss.ap[:], 1.0 / 1024, [ss], [ss])
                    for j in range(4):
                        cb = cbf.next()
                        STT("dve", cb.ap[:], hw[j].ap[:], ss.ap[:, j:j + 1], gb_.ap[:], ALU.mult, ALU.mult,
                            [hw[j], ss, gb_], [cb])
                        pb = S.pbank()
                        pv = bview(pb).rearrange("p (k n) -> p k n", k=8)
                        TRN([(pv[:, k, :], cb.ap[:, k * 128:(k + 1) * 128]) for k in range(8)], [cb], [pb])
                        CP("act" if j % 2 == 0 else "dve", dst.ap[:, :, j * 128:(j + 1) * 128], pv, [pb], [dst])
                        if extra is not None:
                            extra(j, ss)

                def ffn_expert(wg_ap, wu_ap, wd_ap, F_, scale_cols):
                    nfb = (F_ + 511) // 512
                    wgv = wg_ap.rearrange("(k p) n -> p k n", p=128)
                    wuv = wu_ap.rearrange("(k p) n -> p k n", p=128)
                    wdv = wd_ap.rearrange("(f p) n -> p f n", p=128)
                    for fb in range(nfb):
                        fw = min(512, F_ - fb * 512)
                        wg = wblk.next()
                        LD(wg.ap[:, :, 0:fw], wgv[:, :, fb * 512:fb * 512 + fw], wg, [WSRC], [wg])
                        wu = wblk.next()
                        LD(wu.ap[:, :, 0:fw], wuv[:, :, fb * 512:fb * 512 + fw], wu, [WSRC], [wu])
                        for ft in range(fw // 128):
                            f = fb * 4 + ft
                            pg = S.pbank()
                            MM([(pg.ap[:], wg.ap[:, k, ft * 128:(ft + 1) * 128], cT.ap[:, k, :], k == 0, k == 7)
                                for k in range(8)], [wg, cT], [pg])
                            pu = S.pbank()
                            MM([(pu.ap[:], wu.ap[:, k, ft * 128:(ft + 1) * 128], cT.ap[:, k, :], k == 0, k == 7)
                                for k in range(8)], [wu, cT], [pu])
                            sg = sgr.next()
                            ACT(sg.ap[:], pg.ap[:], AF.Silu, [pg], [sg])
                            TT("dve", hT.ap[:, f, :], pu.ap[:], sg.ap[:], ALU.mult, [pu, sg], [hT])
                    nft = F_ // 128
                    for half in range(2):
                        accs = [S.pbank() for _ in range(4)]
                        for fb in range(nfb):
                            nf = min(4, nft - fb * 4)
                            wd = wdblk.next()
                            LD(wd.ap[:, 0:nf, :], wdv[:, fb * 4:fb * 4 + nf, half * 512:(half + 1) * 512], wd, [WSRC], [wd])
                            items = []
                            for j in range(4):
                                for ft in range(nf):
                                    f = fb * 4 + ft
                                    items.append((accs[j].ap[:], hT.ap[:, f, j * 128:(j + 1) * 128], wd.ap[:, ft, :],
                                                  f == 0, f == nft - 1))
                            MM(items, [hT, wd], accs)
                        for j in range(4):
                            hs = hw[j].ap[:, half * 512:(half + 1) * 512]
                            if scale_cols is None:
                                TT("dve", hs, accs[j].ap[:], hs, ALU.add, [accs[j], hw[j]], [hw[j]])
                            else:
                                STT("dve", hs, accs[j].ap[:], scale_cols[j], hs, ALU.mult, ALU.add,
                                    [accs[j], hw[j]] + comb, [hw[j]])

                WSRC = Buf("wsrc_all")
                for g in range(NG):
                    t0g = g * 512
                    yg = yTg.next()
                    LD(yg.ap[:], yT.ap[:, t0g:t0g + 512].rearrange("(k p) s -> p k s", p=128), yg, [yT], [yg])
                    hbs = []
                    for j in range(4):
                        hb = hin.next()
                        hbs.append(hb)
                        LD(hb.ap[:], hsrc.ap[t0g + j * 128:t0g + (j + 1) * 128, :], hb, [hsrc], [hb])
                    wos = []
                    for half in range(2):
                        wo = wblk.next()
                        LD(wo.ap[:], Wb["w_out"].ap[L].rearrange("(k p) n -> p k n", p=128)[:, :, half * 512:(half + 1) * 512],
                           wo, [WSRC], [wo])
                        wos.append(wo)
                    for j in range(4):
                        for half in range(2):
                            pb = S.pbank()
                            MM([(pb.ap[:], yg.ap[:, k, j * 128:(j + 1) * 128], wos[half].ap[:, k, :], k == 0, k == 7)
                                for k in range(8)], [yg, wos[half]], [pb])
                            TT("dve", hw[j].ap[:, half * 512:(half + 1) * 512], pb.ap[:],
                               hbs[j].ap[:, half * 512:(half + 1) * 512], ALU.add, [pb, hbs[j]], [hw[j]])
                    if not moe:
                        norm_T(gff, cT)
                        ffn_expert(Wb["dense_w_gate"].ap[0], Wb["dense_w_up"].ap[0], Wb["dense_w_down"].ap[0], FFD, None)
                    else:
                        def router(j, ss):
                            cf = cf32.next()
                            STT("dve", cf.ap[:], hw[j].ap[:], ss.ap[:, j:j + 1], gff.ap[:], ALU.mult, ALU.mult,
                                [hw[j], ss, gff], [cf])
                            ct = cTf.next()
                            for kk in range(2):
                                pb = S.pbank()
                                pv = pb.ap[:].rearrange("p (k n) -> p k n", k=4)

                                def f(e, pv=pv, cf=cf, kk=kk):
                                    r = None
                                    for k in range(4):
                                        r = e.transpose(out=pv[:, k, :], in_=cf.ap[:, (kk * 4 + k) * 128:(kk * 4 + k + 1) * 128],
                                                        identity=identf.ap[:])
                                    return r
                                S.op("pe", f, [cf, identf], [pb])
                                CP("act", ct.ap[:, kk * 4:kk * 4 + 4, :], pv, [pb], [ct])
                            pl = S.pbank()
                            MM([(pl.ap[:, 0:8], ct.ap[:, k, :], wrt.ap[:, k, :], k == 0, k == 7) for k in range(8)],
                               [ct, wrt], [pl])
                            lg = rl.next()
                            CP("act", lg.ap[:], pl.ap[:, 0:8], [pl], [lg])
                            m8 = mx8.next()
                            S.op("dve", (lambda m8=m8, lg=lg: lambda e: e.max(out=m8.ap[:], in_=lg.ap[:]))(), [lg], [m8])
                            ex = rex.next()
                            r4 = rsm.next()
                            TS("dve", r4.ap[:, 0:1], m8.ap[:, 0:1], -1.0, None, ALU.mult, None, [m8], [r4])
                            ACT(ex.ap[:], lg.ap[:], AF.Exp, [lg, r4], [ex], bias=r4.ap[:, 0:1])
                            ACT(r4.ap[:, 1:2], m8.ap[:, 1:2], AF.Exp, [m8, r4], [r4], bias=r4.ap[:, 0:1])
                            TS("dve", r4.ap[:, 1:2], r4.ap[:, 1:2], 1.0, None, ALU.add, None, [r4], [r4])
                            S.op("dve", (lambda r4=r4: lambda e: e.reciprocal(out=r4.ap[:, 2:3], in_=r4.ap[:, 1:2]))(), [r4], [r4])
                            TS("dve", comb[j].ap[:], lg.ap[:], m8.ap[:, 1:2], None, ALU.is_ge, None, [lg, m8], [comb[j]])
                            TT("dve", comb[j].ap[:], comb[j].ap[:], ex.ap[:], ALU.mult, [ex, comb[j]], [comb[j]])
                            TS("dve", comb[j].ap[:], comb[j].ap[:], r4.ap[:, 2:3], None, ALU.mult, None, [r4, comb[j]], [comb[j]])
                        norm_T(gff, cT, router)
                        for ex_ in range(nexp):
                            ffn_expert(Wb["moe_w_gate"].ap[0][ex_], Wb["moe_w_up"].ap[0][ex_], Wb["moe_w_down"].ap[0][ex_],
                                       FFE, [comb[j].ap[:, ex_:ex_ + 1] for j in range(4)])
                    norm_T(gpl, cT)
                    for j in range(4):
                        pi_ = pin.next()
                        LD(pi_.ap[:], I["p"][L][t0g + j * 128:t0g + (j + 1) * 128, :], pi_, [], [pi_])
                        pb_ = pbf.next()
                        CP("pool", pb_.ap[:], pi_.ap[:], [pi_], [pb_])
                        pk_ = S.pbank()
                        pv = bview(pk_).rearrange("p (k n) -> p k n", k=8)
                        TRN([(pv[:, k, :], pb_.ap[:, k * 128:(k + 1) * 128]) for k in range(2)], [pb_], [pk_])
                        CP("act", pTt.ap[:, :, j * 128:(j + 1) * 128], pv[:, 0:2, :], [pk_], [pTt])
                    wgs = []
                    for half in range(2):
                        wo = wblk.next()
                        LD(wo.ap[:], Wb["w_ple_gate"].ap[L].rearrange("(k p) n -> p k n", p=128)[:, :, half * 512:(half + 1) * 512],
                           wo, [WSRC], [wo])
                        wgs.append(wo)
                    for j in range(4):
                        for half in range(2):
                            pg = S.pbank()
                            MM([(pg.ap[:], cT.ap[:, k, j * 128:(j + 1) * 128], wgs[half].ap[:, k, :], k == 0, k == 7)
                                for k in range(8)], [cT, wgs[half]], [pg])
                            pp2 = S.pbank()
                            MM([(pp2.ap[:], pTt.ap[:, k, j * 128:(j + 1) * 128], wpp.ap[:, k, half * 512:(half + 1) * 512],
                                 k == 0, k == 1) for k in range(2)], [pTt, wpp], [pp2])
                            gs = gsb.next()
                            ACT(gs.ap[:], pg.ap[:], AF.Sigmoid, [pg], [gs])
                            TT("dve", gs.ap[:], pp2.ap[:], gs.ap[:], ALU.mult, [pp2, gs], [gs])
                            hs = hw[j].ap[:, half * 512:(half + 1) * 512]
                            TT("dve", hs, hs, gs.ap[:], ALU.add, [gs, hw[j]], [hw[j]])
                        LD(hdst.ap[t0g + j * 128:t0g + (j + 1) * 128, :], hw[j].ap[:], hw[j], [hw[j]], [hdst], q="pool")

        conv_list = []
        conv_moe = []
        for L in layers:
            conv_list += [("w_in", (L,)), ("w_out", (L,)), ("w_ple_gate", (L,)), ("w_ple_proj", (L,))]
            if L % 2 == 0:
                conv_list += [("dense_w_gate", (0,)), ("dense_w_up", (0,)), ("dense_w_down", (0,))]
            else:
                for ex_ in range(nexp):
                    conv_moe += [("moe_w_gate", (0, ex_)), ("moe_w_up", (0, ex_)), ("moe_w_down", (0, ex_))]
        bg_ok = (len(layers) == 2 and "C" not in skip and "V" not in skip)
        if not bg_ok:
            conv_list += conv_moe
        if 'V' not in skip:
            convert(conv_list)
        hcur = IN["x"]
        for li, L in enumerate(layers):
            hnext = OUT if li == len(layers) - 1 else h1
            if "A" not in skip:
                phase_A(L, hcur)
            if "B" not in skip:
                phase_B(L)
            if "C" not in skip:
                if bg_ok and li == 0:
                    with S.scope():
                        n_chunks = nexp * (8 + 8 + 28)
                        n_units = 4 * sum(4 * q + 4 for q in range(NG))
                        gen = convert_gen(conv_moe, convert_bufs(), engs=("dve", "pool"))
                        phase_C(L, bg=gen, bg_every=max(1, n_units // (n_chunks + 8)))
                        for _ in gen:
                            pass
                else:
                    phase_C(L)
            if "yT" in taps and li == len(layers) - 1:
                break
            if "D" not in skip:
                phase_D(L, hcur, hnext)
            hcur = hnext
        if "yT" in taps:
            tp = tap_aps["yT"]
            with S.scope():
                tb = S.sbuf("tapb", [128, 8, S_tok], BF16, dma=True)
                LD(tb.ap[:], yT.ap.rearrange("(k p) s -> p k s", p=128), tb, [yT], [tb])
                LD(tp.rearrange("(k p) s -> p k s", p=128), tb.ap[:], tb, [tb], [OUT])
        S.barrier()
        S.emit(block)
    return nc


_CACHE = {}


def kernel(**inputs):
    x = np.asarray(inputs["x"], dtype=np.float32)
    B, S_tok, _ = x.shape
    p = np.asarray(inputs["p"], dtype=np.float32)
    key = S_tok
    if key not in _CACHE:
        _CACHE[key] = build(S_tok)
    nc = _CACHE[key]
    shared = {name: np.ascontiguousarray(np.asarray(inputs[name], dtype=np.float32)) for name, _ in PARAMS}
    ncores = 8
    in_maps = []
    for c in range(ncores):
        b = c % B
        m = dict(shared)
        m["x"] = np.ascontiguousarray(x[b])
        m["p"] = np.ascontiguousarray(p[:, b])
        in_maps.append(m)
    res = run_bass_kernel_spmd(nc, in_maps, core_ids=list(range(ncores)))
    out = np.stack([np.asarray(res.results[b]["out"], dtype=np.float32) for b in range(B)], axis=0)
    return out.astype(np.float32)
```

```python
import contextlib
import math
import numpy as np
import concourse.bass as bass
import concourse.mybir as mybir
from concourse.bass_utils import run_bass_kernel_spmd

F32 = mybir.dt.float32
BF16 = mybir.dt.bfloat16
I32 = mybir.dt.int32
AF = mybir.ActivationFunctionType
ALU = mybir.AluOpType
AX = mybir.AxisListType

ENGS = ("pe", "act", "dve", "pool", "sp")
D = 1024
DIN = 3080
FFD = 2816
FFE = 3584
NEXP = 8
EPS = 1e-6
NDSEM = 56


class Buf:
    def __init__(self, name, ap=None, dsem=None):
        self.name = name
        self.ap = ap
        self.w = None
        self.r = {}
        self.dsem = dsem


class Ring:
    def __init__(self, bufs):
        self.bufs = bufs
        self.i = -1

    def next(self):
        self.i = (self.i + 1) % len(self.bufs)
        return self.bufs[self.i]


class Sched:
    def __init__(self, nc, stack):
        self.nc = nc
        self.stack = stack
        self.stacks = [stack]
        self.sems = {}
        self.cnt = {}
        self.prog = {e: [] for e in ENGS}
        self.seen = {e: {} for e in ENGS}
        for e in ENGS:
            self.newsem("E_" + e)
        self.free_ds = [self.newsem("D_%d" % i) for i in range(NDSEM)]
        self.scope_ds = [[]]
        self.banks = []
        self.bank_i = -1
        self.reserved = set()

    def newsem(self, key):
        h = self.stack.enter_context(self.nc.semaphore(key))
        self.sems[key] = h
        self.cnt[key] = 0
        return key

    def sbuf(self, name, shape, dtype, dma=False):
        self.uid = getattr(self, "uid", 0) + 1
        name = "%s_u%d" % (name, self.uid)
        t = self.stacks[-1].enter_context(self.nc.sbuf_tensor(name, list(shape), dtype))
        b = Buf(name, t)
        if dma:
            b.dsem = self.free_ds.pop()
            self.scope_ds[-1].append(b.dsem)
        return b

    def ring(self, name, shape, dtype, n, dma=False):
        return Ring([self.sbuf("%s_%d" % (name, i), shape, dtype, dma) for i in range(n)])

    def dram(self, name, shape, dtype):
        t = self.nc.dram_tensor(name, list(shape), dtype, kind="Internal")
        return Buf(name, t.ap())

    def make_banks(self):
        for i in range(8):
            t = self.stack.enter_context(self.nc.psum_tensor("bank%d" % i, [128, 512], F32))
            self.banks.append(Buf("bank%d" % i, t))

    def pbank(self):
        for _ in range(8):
            self.bank_i = (self.bank_i + 1) % 8
            if self.bank_i not in self.reserved:
                return self.banks[self.bank_i]
        raise RuntimeError("no psum bank")

    def reserve(self, n):
        out = []
        for i in range(8):
            if i not in self.reserved and len(out) < n:
                self.reserved.add(i)
                out.append(self.banks[i])
        return out

    def release(self, banks):
        for b in banks:
            self.reserved.discard(self.banks.index(b))

    @contextlib.contextmanager
    def scope(self):
        st = contextlib.ExitStack()
        self.stacks.append(st)
        self.scope_ds.append([])
        try:
            yield
        finally:
            self.barrier()
            self.free_ds.extend(self.scope_ds.pop())
            self.stacks.pop()
            st.close()

    def _waits(self, eng, reads, writes):
        need = {}

        def add(tok):
            if tok is None:
                return
            k, v = tok
            if eng == "pe" and k == "E_pe":
                return
            if v > need.get(k, 0):
                need[k] = v

        for b in reads:
            add(b.w)
        for b in writes:
            add(b.w)
            for k, v in b.r.items():
                add((k, v))
        out = []
        for k, v in need.items():
            if k.startswith("D_"):
                v = self.cnt[k]
            if self.seen[eng].get(k, 0) >= v:
                continue
            self.seen[eng][k] = v
            out.append((k, v))
        return out

    def _commit(self, tok, reads, writes):
        k, v = tok
        for b in reads:
            if v > b.r.get(k, 0):
                b.r[k] = v
        for b in writes:
            b.w = tok
            b.r = {}

    def op(self, eng, fn, reads=(), writes=()):
        waits = self._waits(eng, reads, writes)
        k = "E_" + eng
        self.cnt[k] += 1
        tok = (k, self.cnt[k])
        self.prog[eng].append((waits, fn, k, 1, 1))
        self._commit(tok, reads, writes)
        return tok

    def dma(self, fn, semb, reads=(), writes=(), n=1, q="sp"):
        waits = self._waits(q, reads, writes)
        k = semb.dsem
        self.cnt[k] += 16 * n
        tok = (k, self.cnt[k])
        self.prog[q].append((waits, fn, k, 16, n))
        self._commit(tok, reads, writes)
        return tok

    def barrier(self):
        for e in ENGS:
            out = []
            for k, v in self.cnt.items():
                if v == 0 or (e == "pe" and k == "E_pe"):
                    continue
                if self.seen[e].get(k, 0) >= v:
                    continue
                self.seen[e][k] = v
                out.append((k, v))
            if out:
                self.prog[e].append((out, None, None, 0, 0))

    def emit(self, block):
        def run(e):
            def body(engine):
                for waits, fn, k, inc, n in self.prog[e]:
                    for wk, wv in waits:
                        engine.wait_ge(self.sems[wk], wv)
                    if fn is None:
                        continue
                    r = fn(engine)
                    if inc == 16:
                        assert len(r) == n, (len(r), n)
                        for ins in r:
                            ins.then_inc(self.sems[k], 16)
                    else:
                        r.then_inc(self.sems[k], 1)
            return body
        block.tensor(run("pe"))
        block.scalar(run("act"))
        block.vector(run("dve"))
        block.gpsimd(run("pool"))
        block.sync(run("sp"))


PARAMS = [
    ("mix_norm_g", (2, 1024)), ("w_in", (2, 1024, 3080)), ("b_igate", (2, 4)), ("b_fgate", (2, 4)),
    ("m_qk_conv_w", (2, 4, 512)), ("m_out_norm_g", (2, 256)), ("c_conv_w", (2, 31, 256)),
    ("c_conv_b", (2, 256)), ("c_ln_g", (2, 256)), ("c_ln_b", (2, 256)), ("a_q_norm_g", (2, 64)),
    ("a_k_norm_g", (2, 64)), ("a_lambda_q1", (2, 64)), ("a_lambda_k1", (2, 64)), ("a_lambda_q2", (2, 64)),
    ("a_lambda_k2", (2, 64)), ("a_subln_g", (2, 128)), ("w_out", (2, 1024, 1024)), ("ffn_norm_g", (2, 1024)),
    ("dense_w_gate", (1, 1024, 2816)), ("dense_w_up", (1, 1024, 2816)), ("dense_w_down", (1, 2816, 1024)),
    ("router_w", (1, 1024, 8)), ("moe_w_gate", (1, 8, 1024, 3584)), ("moe_w_up", (1, 8, 1024, 3584)),
    ("moe_w_down", (1, 8, 3584, 1024)), ("ple_norm_g", (2, 1024)), ("w_ple_gate", (2, 1024, 1024)),
    ("w_ple_proj", (2, 256, 1024)),
]


def build(S_tok, layers=(0, 1), taps=(), nexp=NEXP, skip=()):
    NT = S_tok // 128
    NG = S_tok // 512
    NCH = S_tok // 64
    nc = bass.Bass("TRN2", target_bir_lowering=False)
    I = {}
    I["x"] = nc.dram_tensor("x", [S_tok, D], F32, kind="ExternalInput").ap()
    I["p"] = nc.dram_tensor("p", [2, S_tok, 256], F32, kind="ExternalInput").ap()
    for name, shp in PARAMS:
        I[name] = nc.dram_tensor(name, list(shp), F32, kind="ExternalInput").ap()
    out_ap = nc.dram_tensor("out", [S_tok, D], F32, kind="ExternalOutput").ap()
    tap_aps = {}
    if "yT" in taps:
        tap_aps["yT"] = nc.dram_tensor("tap_yT", [1024, S_tok], BF16, kind="ExternalOutput").ap()

    with contextlib.ExitStack() as st:
        S = Sched(nc, st)
        S.make_banks()
        IN = {k: Buf("in_" + k, v) for k, v in I.items()}
        OUT = Buf("out", out_ap)

        def ACT(out, in_, func, R, W, **kw):
            S.op("act", lambda e: e.activation(out=out, in_=in_, func=func, **kw), R, W)

        def TT(eng, out, a, b, op, R, W):
            S.op(eng, lambda e: e.tensor_tensor(out=out, in0=a, in1=b, op=op), R, W)

        def TS(eng, out, a, s1, s2, op0, op1, R, W):
            if s2 is None:
                S.op(eng, lambda e: e.tensor_scalar(out=out, in0=a, scalar1=s1, scalar2=None, op0=op0), R, W)
            else:
                S.op(eng, lambda e: e.tensor_scalar(out=out, in0=a, scalar1=s1, scalar2=s2, op0=op0, op1=op1), R, W)

        def STT(eng, out, a, s, b, op0, op1, R, W):
            S.op(eng, lambda e: e.scalar_tensor_tensor(out=out, in0=a, scalar=s, in1=b, op0=op0, op1=op1), R, W)

        def CP(eng, out, in_, R, W):
            if eng == "act":
                S.op("act", lambda e: e.copy(out=out, in_=in_), R, W)
            else:
                S.op(eng, lambda e: e.tensor_copy(out=out, in_=in_), R, W)

        def MSET(eng, ap, val, W):
            S.op(eng, lambda e: e.memset(ap, val), (), W)

        def MM(items, R, W):
            def f(e):
                r = None
                for it in items:
                    (o, l, rh, s0, s1) = it[:5]
                    if len(it) > 5:
                        r = e.matmul(o, lhsT=l, rhs=rh, start=s0, stop=s1, skip_group_check=True)
                    else:
                        r = e.matmul(o, lhsT=l, rhs=rh, start=s0, stop=s1)
                return r
            S.op("pe", f, R, W)

        def TRN(items, R, W):
            def f(e):
                r = None
                for (o, i_) in items:
                    r = e.transpose(out=o, in_=i_, identity=ident.ap[0:i_.shape[0], 0:i_.shape[0]])
                return r
            S.op("pe", f, list(R) + [ident], W)

        def LD(out, in_, semb, R, W, q="sp"):
            S.dma(lambda e: [e.dma_start(out=out, in_=in_)], semb, R, W, 1, q)

        def LDS(pairs, semb, R, W, q="sp", slow=False):
            def f(e):
                if slow:
                    return [e.dma_start(out=o, in_=i_, allow_slow_non_contiguous=True) for (o, i_) in pairs]
                return [e.dma_start(out=o, in_=i_) for (o, i_) in pairs]
            S.dma(f, semb, R, W, len(pairs), q)

        def bview(bank):
            return bank.ap[:].bitcast(BF16)

        def rsqrt_act(out, in_, scale, R, W):
            ACT(out, in_, AF.Ln, R, W, scale=scale, bias=epsb.ap[0:in_.shape[0], 0:1])
            ACT(out, out, AF.Exp, W, W, scale=-0.5)

        ident = S.sbuf("ident", [128, 128], BF16)
        identf = S.sbuf("identf", [128, 128], F32)
        triT = S.sbuf("triT", [128, 128], F32)
        triTb = S.sbuf("triTb", [128, 128], BF16)
        triU = S.sbuf("triU", [128, 128], BF16)
        selA = S.sbuf("selA", [128, 128], F32)
        selB = S.sbuf("selB", [128, 128], F32)
        onesln = S.sbuf("onesln", [128, 128], F32)
        epsb = S.sbuf("epsb", [128, 1], F32)
        oneb = S.sbuf("oneb", [128, 1], F32)
        ln8b = S.sbuf("ln8b", [128, 1], F32)
        kcol_i = S.sbuf("kcol_i", [128, 1], I32)
        kcol = S.sbuf("kcol", [128, 1], F32)
        NDD = NT + 4
        abt = S.sbuf("abt", [128, 4, NDD], F32)
        A_tm = S.sbuf("A_tm", [128, NT, 4], F32)
        Gb = S.sbuf("Gb", [128, NCH, 4], F32)

        def mk_consts(e):
            e.memset(identf.ap[:], 1.0)
            e.affine_select(out=identf.ap[:], in_=identf.ap[:], compare_op=ALU.is_ge, fill=0.0,
                            base=0, pattern=[[-1, 128]], channel_multiplier=1)
            e.affine_select(out=identf.ap[:], in_=identf.ap[:], compare_op=ALU.is_ge, fill=0.0,
                            base=0, pattern=[[1, 128]], channel_multiplier=-1)
            e.memset(triT.ap[:], 1.0)
            e.affine_select(out=triT.ap[:], in_=triT.ap[:], compare_op=ALU.is_ge, fill=0.0,
                            base=0, pattern=[[1, 128]], channel_multiplier=-1)
            e.memset(triT.ap[0:64, 64:128], 0.0)
            e.memset(selA.ap[:], 0.0)
            e.memset(selA.ap[0:64, :], 1.0)
            e.memset(selB.ap[:], 0.0)
            e.memset(selB.ap[64:128, :], 1.0)
            e.memset(onesln.ap[:], 1.0 / 256.0)
            e.memset(epsb.ap[:], EPS)
            e.memset(oneb.ap[:], 1.0)
            e.memset(ln8b.ap[:], -math.log(8.0))
            return e.iota(kcol_i.ap[:], pattern=[[0, 1]], base=0, channel_multiplier=1)
        S.op("pool", mk_consts, (), [identf, triT, selA, selB, onesln, epsb, oneb, ln8b, kcol_i])
        CP("pool", ident.ap[:], identf.ap[:], [identf], [ident])
        CP("pool", triTb.ap[:], triT.ap[:], [triT], [triTb])
        CP("pool", kcol.ap[:], kcol_i.ap[:], [kcol_i], [kcol])

        def mk_triU(e):
            e.memset(triU.ap[:], 1.0)
            return e.affine_select(out=triU.ap[:], in_=triU.ap[:], compare_op=ALU.is_ge, fill=0.0,
                                   base=0, pattern=[[1, 128]], channel_multiplier=-1)
        S.op("pool", mk_triU, (), [triU])
        for h in range(4):
            slope = 2.0 ** (-8.0 * (h + 1) / 4)
            for di in range(NDD):
                dd = di - NT
                TS("pool", abt.ap[:, h, di:di + 1], kcol.ap[:], slope, slope * (128.0 * dd - 256.0),
                   ALU.mult, ALU.add, [kcol], [abt])

        Wb = {}
        Wb["w_in"] = S.dram("wb_in", [2, 1024, DIN], BF16)
        Wb["w_out"] = S.dram("wb_out", [2, 1024, 1024], BF16)
        Wb["dense_w_gate"] = S.dram("wb_dg", [1, 1024, FFD], BF16)
        Wb["dense_w_up"] = S.dram("wb_du", [1, 1024, FFD], BF16)
        Wb["dense_w_down"] = S.dram("wb_dd", [1, FFD, 1024], BF16)
        Wb["moe_w_gate"] = S.dram("wb_mg", [1, 8, 1024, FFE], BF16)
        Wb["moe_w_up"] = S.dram("wb_mu", [1, 8, 1024, FFE], BF16)
        Wb["moe_w_down"] = S.dram("wb_md", [1, 8, FFE, 1024], BF16)
        Wb["w_ple_gate"] = S.dram("wb_pg", [2, 1024, 1024], BF16)
        Wb["w_ple_proj"] = S.dram("wb_pp", [2, 256, 1024], BF16)
        h1 = S.dram("h1", [S_tok, D], F32)
        mqT = S.dram("mqT", [256, S_tok], BF16)
        mkT = S.dram("mkT", [256, S_tok], BF16)
        mktm = S.dram("mktm", [S_tok, 256], BF16)
        mrv = S.dram("mrv", [S_tok, 4, 65], BF16)
        mso = S.dram("mso", [S_tok, 256], BF16)
        aqT = S.dram("aqT", [512, S_tok], BF16)
        akT = S.dram("akT", [512, S_tok], BF16)
        avd = S.dram("avd", [S_tok, 512], BF16)
        yT = S.dram("yT", [1024, S_tok], BF16)

        block = st.enter_context(nc.Block())

        def convert_bufs():
            return (S.ring("cvf", [128, 3584], F32, 3, dma=True), S.ring("cvb", [128, 3584], BF16, 3, dma=True))

        def convert_gen(names_layers, bufs, engs=("act", "dve", "pool")):
            CB = 3584
            fr, br = bufs
            ei = 0
            for (name, idx) in names_layers:
                src = IN[name].ap
                dst = Wb[name].ap
                for ix in idx:
                    src = src[ix]
                    dst = dst[ix]
                R_, C_ = src.shape
                for r0 in range(0, R_, 128):
                    for c0 in range(0, C_, CB):
                        cw = min(CB, C_ - c0)
                        fb = fr.next()
                        bb = br.next()
                        LD(fb.ap[:, 0:cw], src[r0:r0 + 128, c0:c0 + cw], fb, [IN[name]], [fb])
                        eng = engs[ei % len(engs)]
                        ei += 1
                        CP(eng, bb.ap[:, 0:cw], fb.ap[:, 0:cw], [fb], [bb])
                        LD(dst[r0:r0 + 128, c0:c0 + cw], bb.ap[:, 0:cw], bb, [bb], [Wb[name]], q="pool")
                        yield

        def convert(names_layers):
            with S.scope():
                for _ in convert_gen(names_layers, convert_bufs()):
                    pass

        def phase_A(L, hsrc):
            with S.scope():
                win = S.sbuf("win", [128, 8, DIN], BF16, dma=True)
                wv = Wb["w_in"].ap[L].rearrange("(k p) n -> p k n", p=128)
                LDS([(win.ap[:, :, c:c + 770], wv[:, :, c:c + 770]) for c in range(0, DIN, 770)],
                    win, [Wb["w_in"]], [win])
                prm = S.sbuf("prmA", [128, 1], F32, dma=True)
                gbc = S.sbuf("gbcA", [128, 1024], F32)
                bif = S.sbuf("bif", [128, 8], F32)
                wq4 = S.sbuf("wq4", [128, 4, 4], F32)
                wc31 = S.sbuf("wc31", [128, 2, 31], F32)
                cvec = S.sbuf("cvec", [128, 3, 2], F32)
                gq = S.sbuf("gq", [128, 64], F32)
                gk = S.sbuf("gk", [128, 64], F32)
                LDS([(gbc.ap[:], I["mix_norm_g"][L].partition_broadcast(128)),
                     (bif.ap[:, 0:4], I["b_igate"][L].partition_broadcast(128)),
                     (bif.ap[:, 4:8], I["b_fgate"][L].partition_broadcast(128)),
                     (gq.ap[:], I["a_q_norm_g"][L].partition_broadcast(128)),
                     (gk.ap[:], I["a_k_norm_g"][L].partition_broadcast(128))],
                    prm, [], [gbc, bif, gq, gk])
                LDS([(wq4.ap[:, t, :], I["m_qk_conv_w"][L][:, t * 128:(t + 1) * 128].rearrange("j p -> p j"))
                     for t in range(4)] +
                    [(wc31.ap[:, t, :], I["c_conv_w"][L][:, t * 128:(t + 1) * 128].rearrange("j p -> p j"))
                     for t in range(2)] +
                    [(cvec.ap[:, 0, :], I["c_conv_b"][L].rearrange("(t p) -> p t", p=128)),
                     (cvec.ap[:, 1, :], I["c_ln_g"][L].rearrange("(t p) -> p t", p=128)),
                     (cvec.ap[:, 2, :], I["c_ln_b"][L].rearrange("(t p) -> p t", p=128))],
                    prm, [], [wq4, wc31, cvec], slow=True)
                dq = S.sbuf("dq", [128, 16, 128], BF16)
                dc = S.sbuf("dc", [128, 62, 128], BF16)
                for c in range(4):
                    for j in range(4):
                        TS("pool", dq.ap[:, c * 4 + j, :], identf.ap[:], wq4.ap[:, c, j:j + 1], None, ALU.mult, None,
                           [identf, wq4], [dq])
                for c in range(2):
                    for j in range(31):
                        TS("pool", dc.ap[:, c * 31 + j, :], identf.ap[:], wc31.ap[:, c, j:j + 1], None, ALU.mult, None,
                           [identf, wc31], [dc])

                hin = S.ring("hinA", [128, 1024], F32, 6, dma=True)
                junk = S.ring("junkA", [128, 1024], BF16, 2)
                ssr = S.ring("ssA", [128, 4], F32, 2)
                abf = S.ring("abfA", [128, 1024], BF16, 2)
                aTr = S.ring("aTA", [128, 8, 512], BF16, 2)
                xqk = S.sbuf("xqk", [128, 4, 515], BF16)
                zbuf = S.sbuf("zbuf", [128, 2, 542], BF16)
                qko = S.ring("qko", [128, 512], BF16, 3, dma=True)
                ktmo = S.ring("ktmo", [128, 4, 256], BF16, 2, dma=True)
                sgr = S.ring("sgr", [128, 512], F32, 2)
                zc = S.sbuf("zc", [128, 2, 512], F32)
                zc2 = S.sbuf("zc2", [128, 2, 512], F32)
                mean_sb = S.sbuf("mean_sb", [128, 512], F32)
                var_sb = S.sbuf("var_sb", [128, 512], F32)
                dtmp = S.ring("dtmp", [128, 512], F32, 2)
                yco = S.ring("yco", [128, 512], BF16, 2, dma=True)
                gat = S.ring("gat", [128, 16], F32, 2)
                rvo = S.ring("rvo", [128, 4, 65], BF16, 2, dma=True)
                soo = S.ring("soo", [128, 256], BF16, 2, dma=True)
                sqj = S.ring("sqj", [128, 512], F32, 2)
                ssq = S.ring("ssq", [128, 16], F32, 2)
                qn1 = S.ring("qn1", [128, 512], F32, 2)
                qnb = S.ring("qnb", [128, 512], BF16, 2)
                qTo = S.ring("qTo", [128, 4, 512], BF16, 2, dma=True)
                kTo = S.ring("kTo", [128, 4, 512], BF16, 2, dma=True)
                vo = S.ring("vo", [128, 512], BF16, 2, dma=True)

                MSET("pool", xqk.ap[:, :, 0:3], 0.0, [xqk])
                MSET("pool", zbuf.ap[:, :, 0:30], 0.0, [zbuf])

                for g in range(NG):
                    t0g = g * 512
                    aT = aTr.next()
                    ss = ssr.next()
                    hbs = []
                    for j in range(4):
                        hb = hin.next()
                        hbs.append(hb)
                        r0 = t0g + j * 128
                        LD(hb.ap[:], hsrc.ap[r0:r0 + 128, :], hb, [hsrc], [hb])
                        jk = junk.next()
                        ACT(jk.ap[:], hb.ap[:], AF.Square, [hb], [jk, ss], accum_out=ss.ap[:, j:j + 1])
                    rsqrt_act(ss.ap[:], ss.ap[:], 1.0 / 1024, [ss], [ss])
                    for j in range(4):
                        ab = abf.next()
                        STT("dve", ab.ap[:], hbs[j].ap[:], ss.ap[:, j:j + 1], gbc.ap[:], ALU.mult, ALU.mult,
                            [hbs[j], ss, gbc], [ab])
                        pb = S.pbank()
                        pv = bview(pb).rearrange("p (k n) -> p k n", k=8)
                        TRN([(pv[:, k, :], ab.ap[:, k * 128:(k + 1) * 128]) for k in range(8)], [ab], [pb])
                        CP("act" if j % 2 == 0 else "dve", aT.ap[:, :, j * 128:(j + 1) * 128], pv, [pb], [aT])

                    def fm(col0):
                        pb = S.pbank()
                        MM([(pb.ap[:], win.ap[:, k, col0:col0 + 128], aT.ap[:, k, :], k == 0, k == 7)
                            for k in range(8)], [win, aT], [pb])
                        return pb

                    def tm(j, col0, n):
                        pb = S.pbank()
                        MM([(pb.ap[:, 0:n], aT.ap[:, k, j * 128:(j + 1) * 128], win.ap[:, k, col0:col0 + n],
                             k == 0, k == 7) for k in range(8)], [win, aT], [pb])
                        return pb

                    if g > 0:
                        CP("pool", xqk.ap[:, :, 0:3], xqk.ap[:, :, 512:515], [xqk], [xqk])
                    for c in range(4):
                        pb = fm(c * 128)
                        CP("act", xqk.ap[:, c, 3:515], pb.ap[:], [pb], [xqk])
                    kt = ktmo.next()
                    for c in range(4):
                        pb = S.pbank()
                        MM([(pb.ap[:], dq.ap[:, c * 4 + j, :], xqk.ap[:, c, j:j + 512], j == 0, j == 3)
                            for j in range(4)], [dq, xqk], [pb])
                        qo = qko.next()
                        ACT(qo.ap[:], pb.ap[:], AF.Silu, [pb], [qo])
                        dstT = (mqT if c < 2 else mkT)
                        LD(dstT.ap[(c % 2) * 128:(c % 2 + 1) * 128, t0g:t0g + 512], qo.ap[:], qo, [qo], [dstT], q="pool")
                        if c >= 2:
                            pb2 = S.pbank()
                            pv2 = bview(pb2).rearrange("p (k n) -> p k n", k=8)
                            TRN([(pv2[:, j, :], qo.ap[:, j * 128:(j + 1) * 128]) for j in range(4)], [qo], [pb2])
                            CP("dve", kt.ap[:, :, (c - 2) * 128:(c - 1) * 128], pv2[:, 0:4, :], [pb2], [kt])
                    LD(mktm.ap[t0g:t0g + 512, :].rearrange("(j p) c -> p j c", p=128), kt.ap[:], kt, [kt], [mktm], q="pool")

                    if g > 0:
                        CP("pool", zbuf.ap[:, :, 0:30], zbuf.ap[:, :, 512:542], [zbuf], [zbuf])
                    for c in range(2):
                        pa = fm(1032 + c * 128)
                        pg = fm(1288 + c * 128)
                        sg = sgr.next()
                        ACT(sg.ap[:], pg.ap[:], AF.Sigmoid, [pg], [sg])
                        TT("dve", zbuf.ap[:, c, 30:542], pa.ap[:], sg.ap[:], ALU.mult, [pa, sg], [zbuf])
                    for c in range(2):
                        pb = S.pbank()
                        MM([(pb.ap[:], dc.ap[:, c * 31 + j, :], zbuf.ap[:, c, j:j + 512], j == 0, j == 30)
                            for j in range(31)], [dc, zbuf], [pb])
                        ACT(zc.ap[:, c, :], pb.ap[:], AF.Identity, [pb, cvec], [zc], bias=cvec.ap[:, 0, c:c + 1])
                        ACT(zc2.ap[:, c, :], zc.ap[:, c, :], AF.Square, [zc], [zc2])
                    pm = S.pbank()
                    MM([(pm.ap[:], onesln.ap[:], zc.ap[:, c, :], c == 0, c == 1) for c in range(2)], [onesln, zc], [pm])
                    pv_ = S.pbank()
                    MM([(pv_.ap[:], onesln.ap[:], zc2.ap[:, c, :], c == 0, c == 1) for c in range(2)], [onesln, zc2], [pv_])
                    CP("act", mean_sb.ap[:], pm.ap[:], [pm], [mean_sb])
                    TT("dve", var_sb.ap[:], mean_sb.ap[:], mean_sb.ap[:], ALU.mult, [mean_sb], [var_sb])
                    TT("dve", var_sb.ap[:], pv_.ap[:], var_sb.ap[:], ALU.subtract, [pv_, var_sb], [var_sb])
                    rsqrt_act(var_sb.ap[:], var_sb.ap[:], 1.0, [var_sb], [var_sb])
                    for c in range(2):
                        dt_ = dtmp.next()
                        TT("dve", dt_.ap[:], zc.ap[:, c, :], mean_sb.ap[:], ALU.subtract, [zc, mean_sb], [dt_])
                        TT("dve", dt_.ap[:], dt_.ap[:], var_sb.ap[:], ALU.mult, [dt_, var_sb], [dt_])
                        yo = yco.next()
                        ACT(yo.ap[:], dt_.ap[:], AF.Silu, [dt_, cvec], [yo], scale=cvec.ap[:, 1, c:c + 1],
                            bias=cvec.ap[:, 2, c:c + 1])
                        LD(yT.ap[256 + c * 128:256 + (c + 1) * 128, t0g:t0g + 512], yo.ap[:], yo, [yo], [yT], q="pool")

                    qT_ = qTo.next()
                    kT_ = kTo.next()
                    for j in range(4):
                        t = g * 4 + j
                        r0 = t * 128
                        pvo = tm(j, 512, 512)
                        pif = tm(j, 1024, 8)
                        ga = gat.next()
                        TT("dve", ga.ap[:, 0:8], pif.ap[:, 0:8], bif.ap[:], ALU.add, [pif, bif], [ga])
                        ACT(ga.ap[:, 4:8], ga.ap[:, 4:8], AF.Exp, [ga], [ga], scale=-1.0)
                        ACT(ga.ap[:, 4:8], ga.ap[:, 4:8], AF.Ln, [ga], [ga], bias=oneb.ap[:, 0:1])
                        pc = S.pbank()
                        MM([(pc.ap[:, 0:4], triT.ap[:], ga.ap[:, 4:8], True, True),
                            (pc.ap[:, 4:8], selA.ap[:], ga.ap[:, 4:8], True, True),
                            (pc.ap[:, 8:12], selB.ap[:], ga.ap[:, 4:8], True, True)], [triT, selA, selB, ga], [pc])
                        ACT(A_tm.ap[:, t, :], pc.ap[:, 0:4], AF.Exp, [pc], [A_tm], scale=-1.0, bias=ln8b.ap[:, 0:1])
                        ACT(Gb.ap[:, 2 * t:2 * t + 2, :], pc.ap[:, 4:12].rearrange("p (a b) -> p a b", a=2), AF.Exp,
                            [pc], [Gb], scale=-1.0)
                        TT("dve", ga.ap[:, 8:12], ga.ap[:, 0:4], pc.ap[:, 0:4], ALU.add, [ga, pc], [ga])
                        ACT(ga.ap[:, 8:12], ga.ap[:, 8:12], AF.Exp, [ga], [ga])
                        rv = rvo.next()
                        TT("dve", rv.ap[:, :, 0:64], pvo.ap[:, 0:256].rearrange("p (h d) -> p h d", h=4),
                           ga.ap[:, 8:12].unsqueeze(2).to_broadcast([128, 4, 64]), ALU.mult, [pvo, ga], [rv])
                        CP("dve", rv.ap[:, :, 64:65], ga.ap[:, 8:12].unsqueeze(2), [ga], [rv])
                        LD(mrv.ap[r0:r0 + 128, :, :], rv.ap[:], rv, [rv], [mrv], q="pool")
                        so = soo.next()
                        ACT(so.ap[:], pvo.ap[:, 256:512], AF.Sigmoid, [pvo], [so])
                        LD(mso.ap[r0:r0 + 128, :], so.ap[:], so, [so], [mso], q="pool")
                        pq = tm(j, 1544, 512)
                        pk = tm(j, 2056, 512)
                        pvv = tm(j, 2568, 512)
                        sq_ = ssq.next()
                        for (pp_, off) in ((pq, 0), (pk, 8)):
                            sj = sqj.next()
                            ACT(sj.ap[:], pp_.ap[:], AF.Square, [pp_], [sj])
                            S.op("dve", (lambda sj=sj, off=off, sq_=sq_: lambda e: e.reduce_sum(
                                out=sq_.ap[:, off:off + 8], in_=sj.ap[:].rearrange("p (m d) -> p m d", m=8),
                                axis=AX.X))(), [sj], [sq_])
                        rsqrt_act(sq_.ap[:], sq_.ap[:], 1.0 / 64, [sq_], [sq_])
                        for (pp_, off, gg, dstT_) in ((pq, 0, gq, qT_), (pk, 8, gk, kT_)):
                            q1 = qn1.next()
                            TT("dve", q1.ap[:].rearrange("p (m d) -> p m d", m=8),
                               pp_.ap[:].rearrange("p (m d) -> p m d", m=8),
                               sq_.ap[:, off:off + 8].unsqueeze(2).to_broadcast([128, 8, 64]), ALU.mult,
                               [pp_, sq_], [q1])
                            qb = qnb.next()
                            TT("pool", qb.ap[:].rearrange("p (m d) -> p m d", m=8),
                               q1.ap[:].rearrange("p (m d) -> p m d", m=8),
                               gg.ap[:].unsqueeze(1).to_broadcast([128, 8, 64]), ALU.mult, [q1, gg], [qb])
                            pb = S.pbank()
                            pvw = bview(pb).rearrange("p (k n) -> p k n", k=8)
                            TRN([(pvw[:, m, :], qb.ap[:, m * 128:(m + 1) * 128]) for m in range(4)], [qb], [pb])
                            CP("act", dstT_.ap[:, :, j * 128:(j + 1) * 128], pvw[:, 0:4, :], [pb], [dstT_])
                        v_ = vo.next()
                        CP("act", v_.ap[:], pvv.ap[:], [pvv], [v_])
                        LD(avd.ap[r0:r0 + 128, :], v_.ap[:], v_, [v_], [avd], q="pool")
                    LD(aqT.ap[:, t0g:t0g + 512].rearrange("(m p) s -> p m s", p=128), qT_.ap[:], qT_, [qT_], [aqT], q="pool")
                    LD(akT.ap[:, t0g:t0g + 512].rearrange("(m p) s -> p m s", p=128), kT_.ap[:], kT_, [kT_], [akT], q="pool")

        def phase_B(L):
            with S.scope():
                prm = S.sbuf("prmB", [128, 1], F32, dma=True)
                gm = S.sbuf("gmB", [128, 256], F32)
                LDS([(gm.ap[:], I["m_out_norm_g"][L].partition_broadcast(128))], prm, [], [gm])
                qTh = S.sbuf("qTh", [64, S_tok], BF16, dma=True)
                kTh = S.sbuf("kTh", [64, S_tok], BF16, dma=True)
                ktm = S.sbuf("ktmB", [128, NT, 64], BF16, dma=True)
                rvbd = S.sbuf("rvbd", [128, NT, 130], BF16, dma=True)
                soh = S.sbuf("soh", [128, NT, 64], BF16, dma=True)
                gso = S.sbuf("gso", [128, NT, 64], F32)
                X = S.sbuf("Xst", [64, NCH, 65], F32)
                Cst = S.sbuf("Cst", [64, NCH + 1, 65], BF16)
                yTm = S.sbuf("yTm", [64, S_tok], BF16, dma=True)
                smt = S.ring("smt", [128, 128], BF16, 3)
                ndr = S.ring("ndr", [128, 65], F32, 3)
                sm = S.ring("smB", [128, 8], F32, 3)
                jk = S.ring("jkB", [128, 64], F32, 2)
                ybr = S.ring("ybB", [128, 64], BF16, 3)
                MSET("pool", rvbd.ap[:], 0.0, [rvbd])
                MSET("pool", Cst.ap[:, 0, :], 0.0, [Cst])
                for h in range(4):
                    LD(qTh.ap[:], mqT.ap[h * 64:(h + 1) * 64, :], qTh, [mqT], [qTh])
                    LD(kTh.ap[:], mkT.ap[h * 64:(h + 1) * 64, :], kTh, [mkT], [kTh])
                    LDS([(ktm.ap[:], mktm.ap[:, h * 64:(h + 1) * 64].rearrange("(i p) c -> p i c", p=128))],
                        ktm, [mktm], [ktm], slow=True)
                    mv = mrv.ap.rearrange("(i two p) h c -> two p i h c", two=2, p=64)
                    LDS([(rvbd.ap[0:64, :, 0:65], mv[0][:, :, h, :]),
                         (rvbd.ap[64:128, :, 65:130], mv[1][:, :, h, :])], rvbd, [mrv], [rvbd], slow=True)
                    LDS([(soh.ap[:], mso.ap[:, h * 64:(h + 1) * 64].rearrange("(i p) c -> p i c", p=128))],
                        soh, [mso], [soh], slow=True)
                    TT("pool", gso.ap[:], soh.ap[:], gm.ap[:, h * 64:(h + 1) * 64].unsqueeze(1).to_broadcast([128, NT, 64]),
                       ALU.mult, [soh, gm], [gso])
                    for i in range(NT):
                        pb = S.pbank()
                        MM([(pb.ap[0:64, 0:130], ktm.ap[:, i, :], rvbd.ap[:, i, :], True, True)], [ktm, rvbd], [pb])
                        ACT(X.ap[:, 2 * i, :], pb.ap[0:64, 0:65], AF.Copy, [pb, Gb], [X], scale=Gb.ap[0:64, 2 * i, h:h + 1])
                        ACT(X.ap[:, 2 * i + 1, :], pb.ap[0:64, 65:130], AF.Copy, [pb, Gb], [X],
                            scale=Gb.ap[0:64, 2 * i + 1, h:h + 1])
                    for c in range(1, NCH):
                        STT("dve", X.ap[:, c, :], X.ap[:, c - 1, :], Gb.ap[0:64, c, h:h + 1], X.ap[:, c, :],
                            ALU.mult, ALU.add, [X, Gb], [X])
                    CP("act", Cst.ap[:, 1:NCH + 1, :], X.ap[:, :, :], [X], [Cst])
                    for i in range(NT):
                        ts_ = slice(i * 128, (i + 1) * 128)
                        ps = S.pbank()
                        MM([(ps.ap[:, 0:128], kTh.ap[:, ts_], qTh.ap[:, ts_], True, True)], [kTh, qTh], [ps])
                        sm_ = smt.next()
                        TT("dve", sm_.ap[:], ps.ap[:, 0:128], triT.ap[:], ALU.mult, [ps, triT], [sm_])
                        po = S.pbank()
                        MM([(po.ap[:, 0:130], qTh.ap[:, ts_], Cst.ap[:, 2 * i:2 * i + 2, :].rearrange("p a b -> p (a b)"),
                             True, False),
                            (po.ap[:, 0:130], sm_.ap[:], rvbd.ap[:, i, :], False, True)], [qTh, Cst, sm_, rvbd], [po])
                        nd = ndr.next()
                        ACT(nd.ap[0:64, :], po.ap[0:64, 0:65], AF.Copy, [po, A_tm], [nd], scale=A_tm.ap[0:64, i, h:h + 1])
                        ACT(nd.ap[64:128, :], po.ap[64:128, 65:130], AF.Copy, [po, A_tm], [nd],
                            scale=A_tm.ap[64:128, i, h:h + 1])
                        s_ = sm.next()
                        ACT(s_.ap[:, 0:1], nd.ap[:, 64:65], AF.Abs, [nd], [s_])
                        TS("dve", s_.ap[:, 0:1], s_.ap[:, 0:1], 1.0, None, ALU.max, None, [s_], [s_])
                        S.op("dve", (lambda s_=s_: lambda e: e.reciprocal(out=s_.ap[:, 1:2], in_=s_.ap[:, 0:1]))(), [s_], [s_])
                        j_ = jk.next()
                        ACT(j_.ap[:], nd.ap[:, 0:64], AF.Square, [nd, s_], [j_, s_], scale=s_.ap[:, 1:2],
                            accum_out=s_.ap[:, 2:3])
                        rsqrt_act(s_.ap[:, 3:4], s_.ap[:, 2:3], 1.0 / 64, [s_], [s_])
                        TT("dve", s_.ap[:, 4:5], s_.ap[:, 3:4], s_.ap[:, 1:2], ALU.mult, [s_], [s_])
                        yb = ybr.next()
                        STT("dve", yb.ap[:], nd.ap[:, 0:64], s_.ap[:, 4:5], gso.ap[:, i, :], ALU.mult, ALU.mult,
                            [nd, s_, gso], [yb])
                        pt = S.pbank()
                        ptv = bview(pt)
                        TRN([(ptv[0:64, 0:128], yb.ap[:, :])], [yb], [pt])
                        CP("act", yTm.ap[:, ts_], ptv[0:64, 0:128], [pt], [yTm])
                    LD(yT.ap[h * 64:(h + 1) * 64, :], yTm.ap[:], yTm, [yTm], [yT], q="pool")

        def phase_C(L, bg=None, bg_every=4):
            lam_init = 0.8 - 0.6 * math.exp(-0.3 * L)
            with S.scope():
                prm = S.sbuf("prmC", [128, 1], F32, dma=True)
                lv = S.sbuf("lvC", [128, 4, 64], F32)
                gsub = S.sbuf("gsub", [128, 128], F32)
                lam = S.sbuf("lam", [128, 4], F32)
                ljk = S.sbuf("ljk", [128, 64], F32)
                LDS([(lv.ap[:, 0, :], I["a_lambda_q1"][L].partition_broadcast(128)),
                     (lv.ap[:, 1, :], I["a_lambda_k1"][L].partition_broadcast(128)),
                     (lv.ap[:, 2, :], I["a_lambda_q2"][L].partition_broadcast(128)),
                     (lv.ap[:, 3, :], I["a_lambda_k2"][L].partition_broadcast(128)),
                     (gsub.ap[:], I["a_subln_g"][L].partition_broadcast(128))], prm, [], [lv, gsub])
                MSET("dve", lam.ap[:], 0.0, [lam])
                S.op("dve", lambda e: e.tensor_tensor(out=ljk.ap[:], in0=lv.ap[:, 0, :], in1=lv.ap[:, 1, :], op=ALU.mult),
                     [lv], [ljk])
                S.op("dve", lambda e: e.reduce_sum(out=lam.ap[:, 0:1], in_=ljk.ap[:], axis=AX.X), [ljk], [lam])
                S.op("dve", lambda e: e.tensor_tensor(out=ljk.ap[:], in0=lv.ap[:, 2, :], in1=lv.ap[:, 3, :], op=ALU.mult),
                     [lv, lam], [ljk])
                S.op("dve", lambda e: e.reduce_sum(out=lam.ap[:, 1:2], in_=ljk.ap[:], axis=AX.X), [ljk], [lam])
                ACT(lam.ap[:, 0:2], lam.ap[:, 0:2], AF.Exp, [lam], [lam])
                TT("dve", lam.ap[:, 2:3], lam.ap[:, 0:1], lam.ap[:, 1:2], ALU.subtract, [lam], [lam])
                TS("dve", lam.ap[:, 3:4], lam.ap[:, 2:3], lam_init, None, ALU.add, None, [lam], [lam])
                TS("dve", gsub.ap[:], gsub.ap[:], 1.0 - lam_init, None, ALU.mult, None, [gsub], [gsub])

                KT = S.ring("KTC", [128, S_tok], BF16, 2, dma=True)
                VV = S.ring("VVC", [128, NT, 129], BF16, 2, dma=True)
                QT = S.ring("QTC", [128, 512], BF16, 3, dma=True)
                PT = S.ring("PTC", [128, 512], BF16, 6)
                yta = S.ring("ytaC", [128, 512], BF16, 2, dma=True)
                sm = S.ring("smC", [128, 8], F32, 4)
                t2r = S.ring("t2C", [128, 128], F32, 2)
                yar = S.ring("yaC", [128, 128], F32, 2)
                jkr = S.ring("jkC", [128, 128], BF16, 2)
                ybr = S.ring("ybC", [128, 128], BF16, 2)
                for vb in VV.bufs:
                    MSET("pool", vb.ap[:, :, 128:129], 1.0, [vb])
                accb = S.reserve(3)

                def acc(m, j):
                    i_ = m * 4 + j
                    return accb[i_ // 3], (i_ % 3) * 132

                accs_r = S.ring("accsC", [128, 1056], F32, 2)
                fin_sm = S.ring("finsm", [128, 16], F32, 3)
                o_r = S.ring("oC", [128, 8, 128], F32, 2)
                ya_r = S.ring("yaC2", [128, 4, 128], F32, 2)
                sq_r = S.ring("sqC", [128, 4, 128], F32, 2)
                yb_r = S.ring("ybC2", [128, 4, 128], BF16, 2)
                deferred = []

                def finalize(h, qg):
                    ac = accs_r.next()
                    s_ = fin_sm.next()
                    o = o_r.next()
                    ya = ya_r.next()
                    sq = sq_r.next()
                    yb = yb_r.next()
                    yt = yta.next()
                    for bi in range(3):
                        w = 396 if bi < 2 else 264
                        CP("dve", ac.ap[:, bi * 396:bi * 396 + w], accb[bi].ap[:, 0:w], [accb[bi]], [ac])
                    acv = ac.ap[:].rearrange("p (i c) -> p i c", c=132)
                    S.op("dve", lambda e: e.reciprocal(out=s_.ap[:, 0:8].unsqueeze(2), in_=acv[:, :, 128:129]), [ac], [s_])
                    TS("dve", s_.ap[:, 4:8], s_.ap[:, 4:8], lam.ap[:, 3:4], None, ALU.mult, None, [s_, lam], [s_])
                    TT("dve", o.ap[:], acv[:, :, 0:128], s_.ap[:, 0:8].unsqueeze(2).to_broadcast([128, 8, 128]), ALU.mult,
                       [ac, s_], [o])
                    TT("dve", ya.ap[:], o.ap[:, 0:4, :], o.ap[:, 4:8, :], ALU.subtract, [o], [ya])
                    TT("dve", sq.ap[:], ya.ap[:], ya.ap[:], ALU.mult, [ya], [sq])
                    S.op("dve", lambda e: e.reduce_sum(out=s_.ap[:, 8:12], in_=sq.ap[:], axis=AX.X), [sq, s_], [s_])

                    def F2():
                        rsqrt_act(s_.ap[:, 12:16], s_.ap[:, 8:12], 1.0 / 128, [s_], [s_])

                    def F3():
                        TT("dve", sq.ap[:], ya.ap[:], s_.ap[:, 12:16].unsqueeze(2).to_broadcast([128, 4, 128]), ALU.mult,
                           [ya, s_, sq], [sq])
                        TT("dve", yb.ap[:], sq.ap[:], gsub.ap[:].unsqueeze(1).to_broadcast([128, 4, 128]), ALU.mult,
                           [sq, gsub], [yb])
                        pt_ = S.pbank()
                        ptv = bview(pt_).rearrange("p (k n) -> p k n", k=8)
                        TRN([(ptv[:, j, :], yb.ap[:, j, :]) for j in range(4)], [yb], [pt_])
                        CP("dve", yt.ap[:].rearrange("p (j n) -> p j n", j=4), ptv[:, 0:4, :], [pt_], [yt])
                        LD(yT.ap[512 + h * 128:512 + (h + 1) * 128, qg * 512:(qg + 1) * 512], yt.ap[:], yt, [yt], [yT],
                           q="pool")
                    deferred.append([3, F2])
                    deferred.append([6, F3])

                def tick(flush=False):
                    for d_ in list(deferred):
                        d_[0] -= 1
                        if d_[0] <= 0 or flush:
                            deferred.remove(d_)
                            d_[1]()

                def stage1(h, qg, kb, kt, vv, qt):
                    jj = max(0, kb - 4 * qg)
                    c0_ = jj * 128
                    di = kb - 4 * qg + NT
                    pts = []
                    for m in range(2):
                        pb = S.pbank()
                        MM([(pb.ap[:, c0_:512], kt.ap[m * 64:(m + 1) * 64, kb * 128:(kb + 1) * 128],
                             qt.ap[m * 64:(m + 1) * 64, c0_:512], True, True)], [kt, qt], [pb])
                        pt = PT.next()
                        ACT(pt.ap[:, c0_:512], pb.ap[:, c0_:512], AF.Exp, [pb, abt], [pt], scale=0.125,
                            bias=abt.ap[:, h, di:di + 1])
                        if kb >= 4 * qg:
                            TT("pool", pt.ap[:, c0_:c0_ + 128], pt.ap[:, c0_:c0_ + 128], triU.ap[:], ALU.mult,
                               [pt, triU], [pt])
                        pts.append(pt)

                    def stage2():
                        items = []
                        for m in range(2):
                            for j in range(jj, 4):
                                bk, off = acc(m, j)
                                items.append((bk.ap[:, off:off + 129], pts[m].ap[:, j * 128:(j + 1) * 128],
                                              vv.ap[:, kb, :], kb == 0 and off == 0, kb == 4 * qg + j, True))
                        MM(items, pts + [vv], accb)
                        if kb == 4 * qg + 3:
                            finalize(h, qg)
                    return stage2

                pending = None
                ucount = 0
                for h in range(4):
                    kt = KT.next()
                    vv = VV.next()
                    LD(kt.ap[:], akT.ap[h * 128:(h + 1) * 128, :], kt, [akT], [kt])
                    LDS([(vv.ap[:, :, 0:128], avd.ap[:, h * 128:(h + 1) * 128].rearrange("(i p) c -> p i c", p=128))],
                        vv, [avd], [vv])
                    for qg in range(NG):
                        qt = QT.next()
                        LD(qt.ap[:], aqT.ap[h * 128:(h + 1) * 128, qg * 512:(qg + 1) * 512], qt, [aqT], [qt])
                        for kb in range(4 * qg + 4):
                            s2 = stage1(h, qg, kb, kt, vv, qt)
                            if pending is not None:
                                pending()
                            pending = s2
                            ucount += 1
                            tick()
                            if bg is not None and ucount % bg_every == 0:
                                next(bg, None)
                pending()
                tick(flush=True)
                S.release(accb)

        def phase_D(L, hsrc, hdst):
            moe = (L % 2 == 1)
            with S.scope():
                prm = S.sbuf("prmD", [128, 1], F32, dma=True)
                gff = S.sbuf("gffD", [128, 1024], F32)
                gpl = S.sbuf("gplD", [128, 1024], F32)
                LDS([(gff.ap[:], I["ffn_norm_g"][L].partition_broadcast(128)),
                     (gpl.ap[:], I["ple_norm_g"][L].partition_broadcast(128))], prm, [], [gff, gpl])
                wpp = S.sbuf("wppD", [128, 2, 1024], BF16, dma=True)
                LD(wpp.ap[:], Wb["w_ple_proj"].ap[L].rearrange("(k p) n -> p k n", p=128), wpp, [Wb["w_ple_proj"]], [wpp])
                if moe:
                    wrt = S.sbuf("wrtD", [128, 8, 8], F32, dma=True)
                    LDS([(wrt.ap[:], I["router_w"][0].rearrange("(k p) n -> p k n", p=128))], wrt, [], [wrt])
                wblk = S.ring("wblk", [128, 8, 512], BF16, 4, dma=True)
                wdblk = S.ring("wdblk", [128, 4, 512], BF16, 3, dma=True)
                hin = S.ring("hinD", [128, 1024], F32, 5, dma=True)
                hw = [S.sbuf("hw%d" % j, [128, 1024], F32, dma=True) for j in range(4)]
                yTg = S.ring("yTg", [128, 8, 512], BF16, 2, dma=True)
                junk = S.ring("junkD", [128, 1024], BF16, 2)
                ssr = S.ring("ssD", [128, 4], F32, 2)
                cbf = S.ring("cbfD", [128, 1024], BF16, 2)
                cT = S.sbuf("cTD", [128, 8, 512], BF16)
                FT = (FFE if moe else FFD) // 128
                hT = S.sbuf("hTD", [128, FT, 512], BF16)
                sgr = S.ring("sgD", [128, 512], F32, 3)
                pin = S.ring("pinD", [128, 256], F32, 4, dma=True)
                pbf = S.ring("pbfD", [128, 256], BF16, 2)
                pTt = S.sbuf("pTD", [128, 2, 512], BF16)
                gsb = S.ring("gsbD", [128, 512], F32, 2)
                if moe:
                    cf32 = S.ring("cf32", [128, 1024], F32, 2)
                    cTf = S.ring("cTf", [128, 8, 128], F32, 2)
                    rl = S.ring("rlD", [128, 8], F32, 2)
                    mx8 = S.ring("mx8", [128, 8], F32, 2)
                    rex = S.ring("rexD", [128, 8], F32, 2)
                    rsm = S.ring("rsmD", [128, 4], F32, 2)
                    comb = [S.sbuf("comb%d" % j, [128, 8], F32) for j in range(4)]

                def norm_T(gb_, dst, extra=None):
                    ss = ssr.next()
                    for j in range(4):
                        jk = junk.next()
                        ACT(jk.ap[:], hw[j].ap[:], AF.Square, [hw[j]], [jk, ss], accum_out=ss.ap[:, j:j + 1])
                    rsqrt_act(ss.ap[:], ss.ap[:], 1.0 / 1024, [ss], [ss])
                    for j in range(4):
                        cb = cbf.next()
                        STT("dve", cb.ap[:], hw[j].ap[:], ss.ap[:, j:j + 1], gb_.ap[:], ALU.mult, ALU.mult,
                            [hw[j], ss, gb_], [cb])
                        pb = S.pbank()
                        pv = bview(pb).rearrange("p (k n) -> p k n", k=8)
                        TRN([(pv[:, k, :], cb.ap[:, k * 128:(k + 1) * 128]) for k in range(8)], [cb], [pb])
                        CP("act" if j % 2 == 0 else "dve", dst.ap[:, :, j * 128:(j + 1) * 128], pv, [pb], [dst])
                        if extra is not None:
                            extra(j, ss)

                def ffn_expert(wg_ap, wu_ap, wd_ap, F_, scale_cols):
                    nfb = (F_ + 511) // 512
                    wgv = wg_ap.rearrange("(k p) n -> p k n", p=128)
                    wuv = wu_ap.rearrange("(k p) n -> p k n", p=128)
                    wdv = wd_ap.rearrange("(f p) n -> p f n", p=128)
                    for fb in range(nfb):
                        fw = min(512, F_ - fb * 512)
                        wg = wblk.next()
                        LD(wg.ap[:, :, 0:fw], wgv[:, :, fb * 512:fb * 512 + fw], wg, [WSRC], [wg])
                        wu = wblk.next()
                        LD(wu.ap[:, :, 0:fw], wuv[:, :, fb * 512:fb * 512 + fw], wu, [WSRC], [wu])
                        for ft in range(fw // 128):
                            f = fb * 4 + ft
                            pg = S.pbank()
                            MM([(pg.ap[:], wg.ap[:, k, ft * 128:(ft + 1) * 128], cT.ap[:, k, :], k == 0, k == 7)
                                for k in range(8)], [wg, cT], [pg])
                            pu = S.pbank()
                            MM([(pu.ap[:], wu.ap[:, k, ft * 128:(ft + 1) * 128], cT.ap[:, k, :], k == 0, k == 7)
                                for k in range(8)], [wu, cT], [pu])
                            sg = sgr.next()
                            ACT(sg.ap[:], pg.ap[:], AF.Silu, [pg], [sg])
                            TT("dve", hT.ap[:, f, :], pu.ap[:], sg.ap[:], ALU.mult, [pu, sg], [hT])
                    nft = F_ // 128
                    for half in range(2):
                        accs = [S.pbank() for _ in range(4)]
                        for fb in range(nfb):
                            nf = min(4, nft - fb * 4)
                            wd = wdblk.next()
                            LD(wd.ap[:, 0:nf, :], wdv[:, fb * 4:fb * 4 + nf, half * 512:(half + 1) * 512], wd, [WSRC], [wd])
                            items = []
                            for j in range(4):
                                for ft in range(nf):
                                    f = fb * 4 + ft
                                    items.append((accs[j].ap[:], hT.ap[:, f, j * 128:(j + 1) * 128], wd.ap[:, ft, :],
                                                  f == 0, f == nft - 1))
                            MM(items, [hT, wd], accs)
                        for j in range(4):
                            hs = hw[j].ap[:, half * 512:(half + 1) * 512]
                            if scale_cols is None:
                                TT("dve", hs, accs[j].ap[:], hs, ALU.add, [accs[j], hw[j]], [hw[j]])
                            else:
                                STT("dve", hs, accs[j].ap[:], scale_cols[j], hs, ALU.mult, ALU.add,
                                    [accs[j], hw[j]] + comb, [hw[j]])

                WSRC = Buf("wsrc_all")
                for g in range(NG):
                    t0g = g * 512
                    yg = yTg.next()
                    LD(yg.ap[:], yT.ap[:, t0g:t0g + 512].rearrange("(k p) s -> p k s", p=128), yg, [yT], [yg])
                    hbs = []
                    for j in range(4):
                        hb = hin.next()
                        hbs.append(hb)
                        LD(hb.ap[:], hsrc.ap[t0g + j * 128:t0g + (j + 1) * 128, :], hb, [hsrc], [hb])
                    wos = []
                    for half in range(2):
                        wo = wblk.next()
                        LD(wo.ap[:], Wb["w_out"].ap[L].rearrange("(k p) n -> p k n", p=128)[:, :, half * 512:(half + 1) * 512],
                           wo, [WSRC], [wo])
                        wos.append(wo)
                    for j in range(4):
                        for half in range(2):
                            pb = S.pbank()
                            MM([(pb.ap[:], yg.ap[:, k, j * 128:(j + 1) * 128], wos[half].ap[:, k, :], k == 0, k == 7)
                                for k in range(8)], [yg, wos[half]], [pb])
                            TT("dve", hw[j].ap[:, half * 512:(half + 1) * 512], pb.ap[:],
                               hbs[j].ap[:, half * 512:(half + 1) * 512], ALU.add, [pb, hbs[j]], [hw[j]])
                    if not moe:
                        norm_T(gff, cT)
                        ffn_expert(Wb["dense_w_gate"].ap[0], Wb["dense_w_up"].ap[0], Wb["dense_w_down"].ap[0], FFD, None)
                    else:
                        def router(j, ss):
                            cf = cf32.next()
                            STT("dve", cf.ap[:], hw[j].ap[:], ss.ap[:, j:j + 1], gff.ap[:], ALU.mult, ALU.mult,
                                [hw[j], ss, gff], [cf])
                            ct = cTf.next()
                            for kk in range(2):
                                pb = S.pbank()
                                pv = pb.ap[:].rearrange("p (k n) -> p k n", k=4)

                                def f(e, pv=pv, cf=cf, kk=kk):
                                    r = None
                                    for k in range(4):
                                        r = e.transpose(out=pv[:, k, :], in_=cf.ap[:, (kk * 4 + k) * 128:(kk * 4 + k + 1) * 128],
                                                        identity=identf.ap[:])
                                    return r
                                S.op("pe", f, [cf, identf], [pb])
                                CP("act", ct.ap[:, kk * 4:kk * 4 + 4, :], pv, [pb], [ct])
                            pl = S.pbank()
                            MM([(pl.ap[:, 0:8], ct.ap[:, k, :], wrt.ap[:, k, :], k == 0, k == 7) for k in range(8)],
                               [ct, wrt], [pl])
                            lg = rl.next()
                            CP("act", lg.ap[:], pl.ap[:, 0:8], [pl], [lg])
                            m8 = mx8.next()
                            S.op("dve", (lambda m8=m8, lg=lg: lambda e: e.max(out=m8.ap[:], in_=lg.ap[:]))(), [lg], [m8])
                            ex = rex.next()
                            r4 = rsm.next()
                            TS("dve", r4.ap[:, 0:1], m8.ap[:, 0:1], -1.0, None, ALU.mult, None, [m8], [r4])
                            ACT(ex.ap[:], lg.ap[:], AF.Exp, [lg, r4], [ex], bias=r4.ap[:, 0:1])
                            ACT(r4.ap[:, 1:2], m8.ap[:, 1:2], AF.Exp, [m8, r4], [r4], bias=r4.ap[:, 0:1])
                            TS("dve", r4.ap[:, 1:2], r4.ap[:, 1:2], 1.0, None, ALU.add, None, [r4], [r4])
                            S.op("dve", (lambda r4=r4: lambda e: e.reciprocal(out=r4.ap[:, 2:3], in_=r4.ap[:, 1:2]))(), [r4], [r4])
                            TS("dve", comb[j].ap[:], lg.ap[:], m8.ap[:, 1:2], None, ALU.is_ge, None, [lg, m8], [comb[j]])
                            TT("dve", comb[j].ap[:], comb[j].ap[:], ex.ap[:], ALU.mult, [ex, comb[j]], [comb[j]])
                            TS("dve", comb[j].ap[:], comb[j].ap[:], r4.ap[:, 2:3], None, ALU.mult, None, [r4, comb[j]], [comb[j]])
                        norm_T(gff, cT, router)
                        for ex_ in range(nexp):
                            ffn_expert(Wb["moe_w_gate"].ap[0][ex_], Wb["moe_w_up"].ap[0][ex_], Wb["moe_w_down"].ap[0][ex_],
                                       FFE, [comb[j].ap[:, ex_:ex_ + 1] for j in range(4)])
                    norm_T(gpl, cT)
                    for j in range(4):
                        pi_ = pin.next()
                        LD(pi_.ap[:], I["p"][L][t0g + j * 128:t0g + (j + 1) * 128, :], pi_, [], [pi_])
                        pb_ = pbf.next()
                        CP("pool", pb_.ap[:], pi_.ap[:], [pi_], [pb_])
                        pk_ = S.pbank()
                        pv = bview(pk_).rearrange("p (k n) -> p k n", k=8)
                        TRN([(pv[:, k, :], pb_.ap[:, k * 128:(k + 1) * 128]) for k in range(2)], [pb_], [pk_])
                        CP("act", pTt.ap[:, :, j * 128:(j + 1) * 128], pv[:, 0:2, :], [pk_], [pTt])
                    wgs = []
                    for half in range(2):
                        wo = wblk.next()
                        LD(wo.ap[:], Wb["w_ple_gate"].ap[L].rearrange("(k p) n -> p k n", p=128)[:, :, half * 512:(half + 1) * 512],
                           wo, [WSRC], [wo])
                        wgs.append(wo)
                    for j in range(4):
                        for half in range(2):
                            pg = S.pbank()
                            MM([(pg.ap[:], cT.ap[:, k, j * 128:(j + 1) * 128], wgs[half].ap[:, k, :], k == 0, k == 7)
                                for k in range(8)], [cT, wgs[half]], [pg])
                            pp2 = S.pbank()
                            MM([(pp2.ap[:], pTt.ap[:, k, j * 128:(j + 1) * 128], wpp.ap[:, k, half * 512:(half + 1) * 512],
                                 k == 0, k == 1) for k in range(2)], [pTt, wpp], [pp2])
                            gs = gsb.next()
                            ACT(gs.ap[:], pg.ap[:], AF.Sigmoid, [pg], [gs])
                            TT("dve", gs.ap[:], pp2.ap[:], gs.ap[:], ALU.mult, [pp2, gs], [gs])
                            hs = hw[j].ap[:, half * 512:(half + 1) * 512]
                            TT("dve", hs, hs, gs.ap[:], ALU.add, [gs, hw[j]], [hw[j]])
                        LD(hdst.ap[t0g + j * 128:t0g + (j + 1) * 128, :], hw[j].ap[:], hw[j], [hw[j]], [hdst], q="pool")

        conv_list = []
        conv_moe = []
        for L in layers:
            conv_list += [("w_in", (L,)), ("w_out", (L,)), ("w_ple_gate", (L,)), ("w_ple_proj", (L,))]
            if L % 2 == 0:
                conv_list += [("dense_w_gate", (0,)), ("dense_w_up", (0,)), ("dense_w_down", (0,))]
            else:
                for ex_ in range(nexp):
                    conv_moe += [("moe_w_gate", (0, ex_)), ("moe_w_up", (0, ex_)), ("moe_w_down", (0, ex_))]
        bg_ok = (len(layers) == 2 and "C" not in skip and "V" not in skip)
        if not bg_ok:
            conv_list += conv_moe
        if 'V' not in skip:
            convert(conv_list)
        hcur = IN["x"]
        for li, L in enumerate(layers):
            hnext = OUT if li == len(layers) - 1 else h1
            if "A" not in skip:
                phase_A(L, hcur)
            if "B" not in skip:
                phase_B(L)
            if "C" not in skip:
                if bg_ok and li == 0:
                    with S.scope():
                        n_chunks = nexp * (8 + 8 + 28)
                        n_units = 4 * sum(4 * q + 4 for q in range(NG))
                        gen = convert_gen(conv_moe, convert_bufs(), engs=("dve", "pool"))
                        phase_C(L, bg=gen, bg_every=max(1, n_units // (n_chunks + 8)))
                        for _ in gen:
                            pass
                else:
                    phase_C(L)
            if "yT" in taps and li == len(layers) - 1:
                break
            if "D" not in skip:
                phase_D(L, hcur, hnext)
            hcur = hnext
        if "yT" in taps:
            tp = tap_aps["yT"]
            with S.scope():
                tb = S.sbuf("tapb", [128, 8, S_tok], BF16, dma=True)
                LD(tb.ap[:], yT.ap.rearrange("(k p) s -> p k s", p=128), tb, [yT], [tb])
                LD(tp.rearrange("(k p) s -> p k s", p=128), tb.ap[:], tb, [tb], [OUT])
        S.barrier()
        S.emit(block)
    return nc


_CACHE = {}


def kernel(**inputs):
    x = np.asarray(inputs["x"], dtype=np.float32)
    B, S_tok, _ = x.shape
    p = np.asarray(inputs["p"], dtype=np.float32)
    key = S_tok
    if key not in _CACHE:
        _CACHE[key] = build(S_tok)
    nc = _CACHE[key]
    shared = {name: np.ascontiguousarray(np.asarray(inputs[name], dtype=np.float32)) for name, _ in PARAMS}
    ncores = 8
    active = [0, 1, 4, 5][:B] if B <= 4 else list(range(B))
    zeros = None
    in_maps = []
    for c in range(ncores):
        if c in active:
            b = active.index(c)
            m = dict(shared)
            m["x"] = np.ascontiguousarray(x[b])
            m["p"] = np.ascontiguousarray(p[:, b])
        else:
            if zeros is None:
                zeros = {name: np.zeros(shp, np.float32) for name, shp in PARAMS}
                zeros["x"] = np.zeros((S_tok, D), np.float32)
                zeros["p"] = np.zeros((2, S_tok, 256), np.float32)
            m = zeros
        in_maps.append(m)
    res = run_bass_kernel_spmd(nc, in_maps, core_ids=list(range(ncores)))
    out = np.stack([np.asarray(res.results[active[b]]["out"], dtype=np.float32) for b in range(B)], axis=0)
    return out.astype(np.float32)
```

```python
import contextlib
import math
import numpy as np
import concourse.bass as bass
import concourse.mybir as mybir
from concourse.bass_utils import run_bass_kernel_spmd

F32 = mybir.dt.float32
BF16 = mybir.dt.bfloat16
I32 = mybir.dt.int32
AF = mybir.ActivationFunctionType
ALU = mybir.AluOpType
AX = mybir.AxisListType

ENGS = ("pe", "act", "dve", "pool", "sp")
D = 1024
DIN = 3080
FFD = 2816
FFE = 3584
NEXP = 8
EPS = 1e-6
NDSEM = 56
NPSEM = 16


class Buf:
    def __init__(self, name, ap=None, dsem=None):
        self.name = name
        self.ap = ap
        self.w = None
        self.r = {}
        self.dsem = dsem


class Ring:
    def __init__(self, bufs):
        self.bufs = bufs
        self.i = -1

    def next(self):
        self.i = (self.i + 1) % len(self.bufs)
        return self.bufs[self.i]


class Sched:
    def __init__(self, nc, stack):
        self.nc = nc
        self.stack = stack
        self.stacks = [stack]
        self.sems = {}
        self.cnt = {}
        self.prog = {e: [] for e in ENGS}
        self.seen = {e: {} for e in ENGS}
        for e in ENGS:
            self.newsem("E_" + e)
        self.free_ds = [self.newsem("D_%d" % i) for i in range(NDSEM)]
        self.pool_sems = [self.newsem("P_%d" % i) for i in range(NPSEM)]
        self.pool_i = 0
        self.scope_ds = [[]]
        self.banks = []
        self.bank_i = -1
        self.reserved = set()

    def newsem(self, key):
        h = self.stack.enter_context(self.nc.semaphore(key))
        self.sems[key] = h
        self.cnt[key] = 0
        return key

    def sbuf(self, name, shape, dtype, dma=False):
        self.uid = getattr(self, "uid", 0) + 1
        name = "%s_u%d" % (name, self.uid)
        t = self.stacks[-1].enter_context(self.nc.sbuf_tensor(name, list(shape), dtype))
        b = Buf(name, t)
        if dma:
            b.dsem = self.free_ds.pop()
            self.scope_ds[-1].append(b.dsem)
        return b

    def ring(self, name, shape, dtype, n, dma=False):
        return Ring([self.sbuf("%s_%d" % (name, i), shape, dtype, dma) for i in range(n)])

    def dram(self, name, shape, dtype):
        t = self.nc.dram_tensor(name, list(shape), dtype, kind="Internal")
        return Buf(name, t.ap())

    def make_banks(self):
        for i in range(8):
            t = self.stack.enter_context(self.nc.psum_tensor("bank%d" % i, [128, 512], F32))
            self.banks.append(Buf("bank%d" % i, t))

    def pbank(self):
        for _ in range(8):
            self.bank_i = (self.bank_i + 1) % 8
            if self.bank_i not in self.reserved:
                return self.banks[self.bank_i]
        raise RuntimeError("no psum bank")

    def reserve(self, n):
        out = []
        for i in range(8):
            if i not in self.reserved and len(out) < n:
                self.reserved.add(i)
                out.append(self.banks[i])
        return out

    def release(self, banks):
        for b in banks:
            self.reserved.discard(self.banks.index(b))

    @contextlib.contextmanager
    def scope(self):
        st = contextlib.ExitStack()
        self.stacks.append(st)
        self.scope_ds.append([])
        try:
            yield
        finally:
            self.barrier()
            self.free_ds.extend(self.scope_ds.pop())
            self.stacks.pop()
            st.close()

    def _waits(self, eng, reads, writes):
        need = {}

        def add(tok):
            if tok is None:
                return
            k, v = tok
            if eng == "pe" and k == "E_pe":
                return
            if v > need.get(k, 0):
                need[k] = v

        for b in reads:
            add(b.w)
        for b in writes:
            add(b.w)
            for k, v in b.r.items():
                add((k, v))
        out = []
        for k, v in need.items():
            if k.startswith("D_"):
                v = self.cnt[k]
            if self.seen[eng].get(k, 0) >= v:
                continue
            self.seen[eng][k] = v
            out.append((k, v))
        return out

    def _commit(self, tok, reads, writes):
        k, v = tok
        for b in reads:
            if v > b.r.get(k, 0):
                b.r[k] = v
        for b in writes:
            b.w = tok
            b.r = {}

    def op(self, eng, fn, reads=(), writes=()):
        waits = self._waits(eng, reads, writes)
        k = "E_" + eng
        self.cnt[k] += 1
        tok = (k, self.cnt[k])
        self.prog[eng].append((waits, fn, k, 1, 1))
        self._commit(tok, reads, writes)
        return tok

    def raw(self, eng, fn, reads=()):
        waits = self._waits(eng, reads, ())
        self.prog[eng].append((waits, fn, None, -1, 0))

    def dma(self, fn, semb, reads=(), writes=(), n=1, q="sp"):
        waits = self._waits(q, reads, writes)
        if q == "pool":
            assert n == 1
            k = self.pool_sems[self.pool_i % NPSEM]
            self.pool_i += 1
            v0 = self.cnt[k]
            if v0 > 0 and self.seen[q].get(k, 0) < v0:
                self.seen[q][k] = v0
                waits = waits + [(k, v0)]
        else:
            k = semb.dsem
        self.cnt[k] += 16 * n
        tok = (k, self.cnt[k])
        self.prog[q].append((waits, fn, k, 16, n))
        self._commit(tok, reads, writes)
        return tok

    def barrier(self):
        for e in ENGS:
            out = []
            for k, v in self.cnt.items():
                if v == 0 or (e == "pe" and k == "E_pe"):
                    continue
                if self.seen[e].get(k, 0) >= v:
                    continue
                self.seen[e][k] = v
                out.append((k, v))
            if out:
                self.prog[e].append((out, None, None, 0, 0))

    def emit(self, block):
        def run(e):
            def body(engine):
                for waits, fn, k, inc, n in self.prog[e]:
                    for wk, wv in waits:
                        engine.wait_ge(self.sems[wk], wv)
                    if fn is None:
                        continue
                    r = fn(engine)
                    if inc == -1:
                        continue
                    if inc == 16:
                        assert len(r) == n, (len(r), n)
                        for ins in r:
                            ins.then_inc(self.sems[k], 16)
                    else:
                        r.then_inc(self.sems[k], 1)
            return body
        block.tensor(run("pe"))
        block.scalar(run("act"))
        block.vector(run("dve"))
        block.gpsimd(run("pool"))
        block.sync(run("sp"))


PARAMS = [
    ("mix_norm_g", (2, 1024)), ("w_in", (2, 1024, 3080)), ("b_igate", (2, 4)), ("b_fgate", (2, 4)),
    ("m_qk_conv_w", (2, 4, 512)), ("m_out_norm_g", (2, 256)), ("c_conv_w", (2, 31, 256)),
    ("c_conv_b", (2, 256)), ("c_ln_g", (2, 256)), ("c_ln_b", (2, 256)), ("a_q_norm_g", (2, 64)),
    ("a_k_norm_g", (2, 64)), ("a_lambda_q1", (2, 64)), ("a_lambda_k1", (2, 64)), ("a_lambda_q2", (2, 64)),
    ("a_lambda_k2", (2, 64)), ("a_subln_g", (2, 128)), ("w_out", (2, 1024, 1024)), ("ffn_norm_g", (2, 1024)),
    ("dense_w_gate", (1, 1024, 2816)), ("dense_w_up", (1, 1024, 2816)), ("dense_w_down", (1, 2816, 1024)),
    ("router_w", (1, 1024, 8)), ("moe_w_gate", (1, 8, 1024, 3584)), ("moe_w_up", (1, 8, 1024, 3584)),
    ("moe_w_down", (1, 8, 3584, 1024)), ("ple_norm_g", (2, 1024)), ("w_ple_gate", (2, 1024, 1024)),
    ("w_ple_proj", (2, 256, 1024)),
]


def build(S_tok, layers=(0, 1), taps=(), nexp=NEXP, skip=(), sparse=True):
    NT = S_tok // 128
    NG = S_tok // 512
    NCH = S_tok // 64
    nc = bass.Bass("TRN2", target_bir_lowering=False)
    I = {}
    I["x"] = nc.dram_tensor("x", [S_tok, D], F32, kind="ExternalInput").ap()
    I["p"] = nc.dram_tensor("p", [2, S_tok, 256], F32, kind="ExternalInput").ap()
    for name, shp in PARAMS:
        I[name] = nc.dram_tensor(name, list(shp), F32, kind="ExternalInput").ap()
    out_ap = nc.dram_tensor("out", [S_tok, D], F32, kind="ExternalOutput").ap()
    tap_aps = {}
    if "yT" in taps:
        tap_aps["yT"] = nc.dram_tensor("tap_yT", [1024, S_tok], BF16, kind="ExternalOutput").ap()

    with contextlib.ExitStack() as st:
        S = Sched(nc, st)
        S.make_banks()
        IN = {k: Buf("in_" + k, v) for k, v in I.items()}
        OUT = Buf("out", out_ap)

        def ACT(out, in_, func, R, W, **kw):
            S.op("act", lambda e: e.activation(out=out, in_=in_, func=func, **kw), R, W)

        def TT(eng, out, a, b, op, R, W):
            S.op(eng, lambda e: e.tensor_tensor(out=out, in0=a, in1=b, op=op), R, W)

        def TS(eng, out, a, s1, s2, op0, op1, R, W):
            if s2 is None:
                S.op(eng, lambda e: e.tensor_scalar(out=out, in0=a, scalar1=s1, scalar2=None, op0=op0), R, W)
            else:
                S.op(eng, lambda e: e.tensor_scalar(out=out, in0=a, scalar1=s1, scalar2=s2, op0=op0, op1=op1), R, W)

        def STT(eng, out, a, s, b, op0, op1, R, W):
            S.op(eng, lambda e: e.scalar_tensor_tensor(out=out, in0=a, scalar=s, in1=b, op0=op0, op1=op1), R, W)

        def CP(eng, out, in_, R, W):
            if eng == "act":
                S.op("act", lambda e: e.copy(out=out, in_=in_), R, W)
            else:
                S.op(eng, lambda e: e.tensor_copy(out=out, in_=in_), R, W)

        def MSET(eng, ap, val, W):
            S.op(eng, lambda e: e.memset(ap, val), (), W)

        def MM(items, R, W):
            def f(e):
                r = None
                for it in items:
                    (o, l, rh, s0, s1) = it[:5]
                    if len(it) > 5:
                        r = e.matmul(o, lhsT=l, rhs=rh, start=s0, stop=s1, skip_group_check=True)
                    else:
                        r = e.matmul(o, lhsT=l, rhs=rh, start=s0, stop=s1)
                return r
            S.op("pe", f, R, W)

        def TRN(items, R, W):
            def f(e):
                r = None
                for (o, i_) in items:
                    r = e.transpose(out=o, in_=i_, identity=ident.ap[0:i_.shape[0], 0:i_.shape[0]])
                return r
            S.op("pe", f, list(R) + [ident], W)

        def LD(out, in_, semb, R, W, q="sp"):
            S.dma(lambda e: [e.dma_start(out=out, in_=in_)], semb, R, W, 1, q)

        def LDS(pairs, semb, R, W, q="sp", slow=False):
            def f(e):
                if slow:
                    return [e.dma_start(out=o, in_=i_, allow_slow_non_contiguous=True) for (o, i_) in pairs]
                return [e.dma_start(out=o, in_=i_) for (o, i_) in pairs]
            S.dma(f, semb, R, W, len(pairs), q)

        def bview(bank):
            return bank.ap[:].bitcast(BF16)

        def rsqrt_act(out, in_, scale, R, W):
            ACT(out, in_, AF.Ln, R, W, scale=scale, bias=epsb.ap[0:in_.shape[0], 0:1])
            ACT(out, out, AF.Exp, W, W, scale=-0.5)

        ident = S.sbuf("ident", [128, 128], BF16)
        identf = S.sbuf("identf", [128, 128], F32)
        triT = S.sbuf("triT", [128, 128], F32)
        triTb = S.sbuf("triTb", [128, 128], BF16)
        triU = S.sbuf("triU", [128, 128], BF16)
        selA = S.sbuf("selA", [128, 128], F32)
        selB = S.sbuf("selB", [128, 128], F32)
        onesln = S.sbuf("onesln", [128, 128], F32)
        epsb = S.sbuf("epsb", [128, 1], F32)
        oneb = S.sbuf("oneb", [128, 1], F32)
        ln8b = S.sbuf("ln8b", [128, 1], F32)
        kcol_i = S.sbuf("kcol_i", [128, 1], I32)
        kcol = S.sbuf("kcol", [128, 1], F32)
        NDD = NT + 4
        abt = S.sbuf("abt", [128, 4, NDD], F32)
        A_tm = S.sbuf("A_tm", [128, NT, 4], F32)
        Gb = S.sbuf("Gb", [128, NCH, 4], F32)

        def mk_consts(e):
            e.memset(identf.ap[:], 1.0)
            e.affine_select(out=identf.ap[:], in_=identf.ap[:], compare_op=ALU.is_ge, fill=0.0,
                            base=0, pattern=[[-1, 128]], channel_multiplier=1)
            e.affine_select(out=identf.ap[:], in_=identf.ap[:], compare_op=ALU.is_ge, fill=0.0,
                            base=0, pattern=[[1, 128]], channel_multiplier=-1)
            e.memset(triT.ap[:], 1.0)
            e.affine_select(out=triT.ap[:], in_=triT.ap[:], compare_op=ALU.is_ge, fill=0.0,
                            base=0, pattern=[[1, 128]], channel_multiplier=-1)
            e.memset(triT.ap[0:64, 64:128], 0.0)
            e.memset(selA.ap[:], 0.0)
            e.memset(selA.ap[0:64, :], 1.0)
            e.memset(selB.ap[:], 0.0)
            e.memset(selB.ap[64:128, :], 1.0)
            e.memset(onesln.ap[:], 1.0 / 256.0)
            e.memset(epsb.ap[:], EPS)
            e.memset(oneb.ap[:], 1.0)
            e.memset(ln8b.ap[:], -math.log(8.0))
            return e.iota(kcol_i.ap[:], pattern=[[0, 1]], base=0, channel_multiplier=1)
        S.op("pool", mk_consts, (), [identf, triT, selA, selB, onesln, epsb, oneb, ln8b, kcol_i])
        CP("pool", ident.ap[:], identf.ap[:], [identf], [ident])
        CP("pool", triTb.ap[:], triT.ap[:], [triT], [triTb])
        CP("pool", kcol.ap[:], kcol_i.ap[:], [kcol_i], [kcol])

        def mk_triU(e):
            e.memset(triU.ap[:], 1.0)
            return e.affine_select(out=triU.ap[:], in_=triU.ap[:], compare_op=ALU.is_ge, fill=0.0,
                                   base=0, pattern=[[1, 128]], channel_multiplier=-1)
        S.op("pool", mk_triU, (), [triU])
        for h in range(4):
            slope = 2.0 ** (-8.0 * (h + 1) / 4)
            for di in range(NDD):
                dd = di - NT
                TS("pool", abt.ap[:, h, di:di + 1], kcol.ap[:], slope, slope * (128.0 * dd - 256.0),
                   ALU.mult, ALU.add, [kcol], [abt])

        Wb = {}
        Wb["w_in"] = S.dram("wb_in", [2, 1024, DIN], BF16)
        Wb["w_out"] = S.dram("wb_out", [2, 1024, 1024], BF16)
        Wb["dense_w_gate"] = S.dram("wb_dg", [1, 1024, FFD], BF16)
        Wb["dense_w_up"] = S.dram("wb_du", [1, 1024, FFD], BF16)
        Wb["dense_w_down"] = S.dram("wb_dd", [1, FFD, 1024], BF16)
        if sparse:
            Wb["moe_w_gate"] = S.dram("wb_mg", [8 * 7 * 128, 8 * 512], BF16)
            Wb["moe_w_up"] = S.dram("wb_mu", [8 * 7 * 128, 8 * 512], BF16)
            Wb["moe_w_down"] = S.dram("wb_md", [8 * 2 * 7 * 128, 4 * 512], BF16)
        else:
            Wb["moe_w_gate"] = S.dram("wb_mg", [1, 8, 1024, FFE], BF16)
            Wb["moe_w_up"] = S.dram("wb_mu", [1, 8, 1024, FFE], BF16)
            Wb["moe_w_down"] = S.dram("wb_md", [1, 8, FFE, 1024], BF16)
        Wb["w_ple_gate"] = S.dram("wb_pg", [2, 1024, 1024], BF16)
        Wb["w_ple_proj"] = S.dram("wb_pp", [2, 256, 1024], BF16)
        h1 = S.dram("h1", [S_tok, D], F32)
        mqT = S.dram("mqT", [256, S_tok], BF16)
        mkT = S.dram("mkT", [256, S_tok], BF16)
        mktm = S.dram("mktm", [S_tok, 256], BF16)
        mrv = S.dram("mrv", [S_tok, 4, 65], BF16)
        mso = S.dram("mso", [S_tok, 256], BF16)
        aqT = S.dram("aqT", [512, S_tok], BF16)
        akT = S.dram("akT", [512, S_tok], BF16)
        avd = S.dram("avd", [S_tok, 512], BF16)
        yT = S.dram("yT", [1024, S_tok], BF16)

        block = st.enter_context(nc.Block())

        def convert_bufs():
            return (S.ring("cvf", [128, 3584], F32, 3, dma=True), S.ring("cvb", [128, 3584], BF16, 3, dma=True))

        def convert_gen(names_layers, bufs, engs=("act", "dve", "pool")):
            CB = 3584
            fr, br = bufs
            ei = 0
            for (name, idx) in names_layers:
                src = IN[name].ap
                dst = Wb[name].ap
                for ix in idx:
                    src = src[ix]
                    if not (sparse and name.startswith("moe_")):
                        dst = dst[ix]
                R_, C_ = src.shape
                for r0 in range(0, R_, 128):
                    for c0 in range(0, C_, CB):
                        cw = min(CB, C_ - c0)
                        fb = fr.next()
                        bb = br.next()
                        LD(fb.ap[:, 0:cw], src[r0:r0 + 128, c0:c0 + cw], fb, [IN[name]], [fb])
                        eng = engs[ei % len(engs)]
                        ei += 1
                        CP(eng, bb.ap[:, 0:cw], fb.ap[:, 0:cw], [fb], [bb])
                        if sparse and name in ("moe_w_gate", "moe_w_up"):
                            dv = Wb[name].ap.rearrange("(e fb p) (k c) -> e k p fb c", e=8, fb=7, p=128, k=8)[idx[1]][r0 // 128]
                            LD(dv, bb.ap[:, 0:3584].rearrange("p (fb c) -> p fb c", fb=7), bb, [bb], [Wb[name]], q="pool")
                        elif sparse and name == "moe_w_down":
                            f_ = r0 // 128
                            dv = Wb[name].ap.rearrange("(e h fb p) (ft c) -> e fb ft p h c", e=8, h=2, fb=7, p=128, ft=4)[idx[1]][f_ // 4][f_ % 4]
                            LD(dv, bb.ap[:, 0:1024].rearrange("p (h c) -> p h c", h=2), bb, [bb], [Wb[name]], q="pool")
                        else:
                            LD(dst[r0:r0 + 128, c0:c0 + cw], bb.ap[:, 0:cw], bb, [bb], [Wb[name]], q="pool")
                        yield

        def convert(names_layers):
            with S.scope():
                for _ in convert_gen(names_layers, convert_bufs()):
                    pass

        def phase_A(L, hsrc):
            with S.scope():
                win = S.sbuf("win", [128, 8, DIN], BF16, dma=True)
                wv = Wb["w_in"].ap[L].rearrange("(k p) n -> p k n", p=128)
                LDS([(win.ap[:, :, c:c + 770], wv[:, :, c:c + 770]) for c in range(0, DIN, 770)],
                    win, [Wb["w_in"]], [win])
                prm = S.sbuf("prmA", [128, 1], F32, dma=True)
                gbc = S.sbuf("gbcA", [128, 1024], F32)
                bif = S.sbuf("bif", [128, 8], F32)
                wq4 = S.sbuf("wq4", [128, 4, 4], F32)
                wc31 = S.sbuf("wc31", [128, 2, 31], F32)
                cvec = S.sbuf("cvec", [128, 3, 2], F32)
                gq = S.sbuf("gq", [128, 64], F32)
                gk = S.sbuf("gk", [128, 64], F32)
                LDS([(gbc.ap[:], I["mix_norm_g"][L].partition_broadcast(128)),
                     (bif.ap[:, 0:4], I["b_igate"][L].partition_broadcast(128)),
                     (bif.ap[:, 4:8], I["b_fgate"][L].partition_broadcast(128)),
                     (gq.ap[:], I["a_q_norm_g"][L].partition_broadcast(128)),
                     (gk.ap[:], I["a_k_norm_g"][L].partition_broadcast(128))],
                    prm, [], [gbc, bif, gq, gk])
                LDS([(wq4.ap[:, t, :], I["m_qk_conv_w"][L][:, t * 128:(t + 1) * 128].rearrange("j p -> p j"))
                     for t in range(4)] +
                    [(wc31.ap[:, t, :], I["c_conv_w"][L][:, t * 128:(t + 1) * 128].rearrange("j p -> p j"))
                     for t in range(2)] +
                    [(cvec.ap[:, 0, :], I["c_conv_b"][L].rearrange("(t p) -> p t", p=128)),
                     (cvec.ap[:, 1, :], I["c_ln_g"][L].rearrange("(t p) -> p t", p=128)),
                     (cvec.ap[:, 2, :], I["c_ln_b"][L].rearrange("(t p) -> p t", p=128))],
                    prm, [], [wq4, wc31, cvec], slow=True)
                dq = S.sbuf("dq", [128, 16, 128], BF16)
                dc = S.sbuf("dc", [128, 62, 128], BF16)
                for c in range(4):
                    for j in range(4):
                        TS("pool", dq.ap[:, c * 4 + j, :], identf.ap[:], wq4.ap[:, c, j:j + 1], None, ALU.mult, None,
                           [identf, wq4], [dq])
                for c in range(2):
                    for j in range(31):
                        TS("pool", dc.ap[:, c * 31 + j, :], identf.ap[:], wc31.ap[:, c, j:j + 1], None, ALU.mult, None,
                           [identf, wc31], [dc])

                hin = S.ring("hinA", [128, 1024], F32, 6, dma=True)
                junk = S.ring("junkA", [128, 1024], BF16, 2)
                ssr = S.ring("ssA", [128, 4], F32, 2)
                abf = S.ring("abfA", [128, 1024], BF16, 2)
                aTr = S.ring("aTA", [128, 8, 512], BF16, 2)
                xqk = S.sbuf("xqk", [128, 4, 515], BF16)
                zbuf = S.sbuf("zbuf", [128, 2, 542], BF16)
                qko = S.ring("qko", [128, 512], BF16, 3, dma=True)
                ktmo = S.ring("ktmo", [128, 4, 256], BF16, 2, dma=True)
                sgr = S.ring("sgr", [128, 512], F32, 2)
                zc = S.sbuf("zc", [128, 2, 512], F32)
                zc2 = S.sbuf("zc2", [128, 2, 512], F32)
                mean_sb = S.sbuf("mean_sb", [128, 512], F32)
                var_sb = S.sbuf("var_sb", [128, 512], F32)
                dtmp = S.ring("dtmp", [128, 512], F32, 2)
                yco = S.ring("yco", [128, 512], BF16, 2, dma=True)
                gat = S.ring("gat", [128, 16], F32, 2)
                rvo = S.ring("rvo", [128, 4, 65], BF16, 2, dma=True)
                soo = S.ring("soo", [128, 256], BF16, 2, dma=True)
                sqj = S.ring("sqj", [128, 512], F32, 2)
                ssq = S.ring("ssq", [128, 16], F32, 2)
                qn1 = S.ring("qn1", [128, 512], F32, 2)
                qnb = S.ring("qnb", [128, 512], BF16, 2)
                qTo = S.ring("qTo", [128, 4, 512], BF16, 2, dma=True)
                kTo = S.ring("kTo", [128, 4, 512], BF16, 2, dma=True)
                vo = S.ring("vo", [128, 512], BF16, 2, dma=True)

                MSET("pool", xqk.ap[:, :, 0:3], 0.0, [xqk])
                MSET("pool", zbuf.ap[:, :, 0:30], 0.0, [zbuf])

                for g in range(NG):
                    t0g = g * 512
                    aT = aTr.next()
                    ss = ssr.next()
                    hbs = []
                    for j in range(4):
                        hb = hin.next()
                        hbs.append(hb)
                        r0 = t0g + j * 128
                        LD(hb.ap[:], hsrc.ap[r0:r0 + 128, :], hb, [hsrc], [hb])
                        jk = junk.next()
                        ACT(jk.ap[:], hb.ap[:], AF.Square, [hb], [jk, ss], accum_out=ss.ap[:, j:j + 1])
                    rsqrt_act(ss.ap[:], ss.ap[:], 1.0 / 1024, [ss], [ss])
                    for j in range(4):
                        ab = abf.next()
                        STT("dve", ab.ap[:], hbs[j].ap[:], ss.ap[:, j:j + 1], gbc.ap[:], ALU.mult, ALU.mult,
                            [hbs[j], ss, gbc], [ab])
                        pb = S.pbank()
                        pv = bview(pb).rearrange("p (k n) -> p k n", k=8)
                        TRN([(pv[:, k, :], ab.ap[:, k * 128:(k + 1) * 128]) for k in range(8)], [ab], [pb])
                        CP("act" if j % 2 == 0 else "dve", aT.ap[:, :, j * 128:(j + 1) * 128], pv, [pb], [aT])

                    def fm(col0):
                        pb = S.pbank()
                        MM([(pb.ap[:], win.ap[:, k, col0:col0 + 128], aT.ap[:, k, :], k == 0, k == 7)
                            for k in range(8)], [win, aT], [pb])
                        return pb

                    def tm(j, col0, n):
                        pb = S.pbank()
                        MM([(pb.ap[:, 0:n], aT.ap[:, k, j * 128:(j + 1) * 128], win.ap[:, k, col0:col0 + n],
                             k == 0, k == 7) for k in range(8)], [win, aT], [pb])
                        return pb

                    if g > 0:
                        CP("pool", xqk.ap[:, :, 0:3], xqk.ap[:, :, 512:515], [xqk], [xqk])
                    for c in range(4):
                        pb = fm(c * 128)
                        CP("act", xqk.ap[:, c, 3:515], pb.ap[:], [pb], [xqk])
                    kt = ktmo.next()
                    for c in range(4):
                        pb = S.pbank()
                        MM([(pb.ap[:], dq.ap[:, c * 4 + j, :], xqk.ap[:, c, j:j + 512], j == 0, j == 3)
                            for j in range(4)], [dq, xqk], [pb])
                        qo = qko.next()
                        ACT(qo.ap[:], pb.ap[:], AF.Silu, [pb], [qo])
                        dstT = (mqT if c < 2 else mkT)
                        LD(dstT.ap[(c % 2) * 128:(c % 2 + 1) * 128, t0g:t0g + 512], qo.ap[:], qo, [qo], [dstT], q="pool")
                        if c >= 2:
                            pb2 = S.pbank()
                            pv2 = bview(pb2).rearrange("p (k n) -> p k n", k=8)
                            TRN([(pv2[:, j, :], qo.ap[:, j * 128:(j + 1) * 128]) for j in range(4)], [qo], [pb2])
                            CP("dve", kt.ap[:, :, (c - 2) * 128:(c - 1) * 128], pv2[:, 0:4, :], [pb2], [kt])
                    LD(mktm.ap[t0g:t0g + 512, :].rearrange("(j p) c -> p j c", p=128), kt.ap[:], kt, [kt], [mktm], q="pool")

                    if g > 0:
                        CP("pool", zbuf.ap[:, :, 0:30], zbuf.ap[:, :, 512:542], [zbuf], [zbuf])
                    for c in range(2):
                        pa = fm(1032 + c * 128)
                        pg = fm(1288 + c * 128)
                        sg = sgr.next()
                        ACT(sg.ap[:], pg.ap[:], AF.Sigmoid, [pg], [sg])
                        TT("dve", zbuf.ap[:, c, 30:542], pa.ap[:], sg.ap[:], ALU.mult, [pa, sg], [zbuf])
                    for c in range(2):
                        pb = S.pbank()
                        MM([(pb.ap[:], dc.ap[:, c * 31 + j, :], zbuf.ap[:, c, j:j + 512], j == 0, j == 30)
                            for j in range(31)], [dc, zbuf], [pb])
                        ACT(zc.ap[:, c, :], pb.ap[:], AF.Identity, [pb, cvec], [zc], bias=cvec.ap[:, 0, c:c + 1])
                        ACT(zc2.ap[:, c, :], zc.ap[:, c, :], AF.Square, [zc], [zc2])
                    pm = S.pbank()
                    MM([(pm.ap[:], onesln.ap[:], zc.ap[:, c, :], c == 0, c == 1) for c in range(2)], [onesln, zc], [pm])
                    pv_ = S.pbank()
                    MM([(pv_.ap[:], onesln.ap[:], zc2.ap[:, c, :], c == 0, c == 1) for c in range(2)], [onesln, zc2], [pv_])
                    CP("act", mean_sb.ap[:], pm.ap[:], [pm], [mean_sb])
                    TT("dve", var_sb.ap[:], mean_sb.ap[:], mean_sb.ap[:], ALU.mult, [mean_sb], [var_sb])
                    TT("dve", var_sb.ap[:], pv_.ap[:], var_sb.ap[:], ALU.subtract, [pv_, var_sb], [var_sb])
                    rsqrt_act(var_sb.ap[:], var_sb.ap[:], 1.0, [var_sb], [var_sb])
                    for c in range(2):
                        dt_ = dtmp.next()
                        TT("dve", dt_.ap[:], zc.ap[:, c, :], mean_sb.ap[:], ALU.subtract, [zc, mean_sb], [dt_])
                        TT("dve", dt_.ap[:], dt_.ap[:], var_sb.ap[:], ALU.mult, [dt_, var_sb], [dt_])
                        yo = yco.next()
                        ACT(yo.ap[:], dt_.ap[:], AF.Silu, [dt_, cvec], [yo], scale=cvec.ap[:, 1, c:c + 1],
                            bias=cvec.ap[:, 2, c:c + 1])
                        LD(yT.ap[256 + c * 128:256 + (c + 1) * 128, t0g:t0g + 512], yo.ap[:], yo, [yo], [yT], q="pool")

                    qT_ = qTo.next()
                    kT_ = kTo.next()
                    for j in range(4):
                        t = g * 4 + j
                        r0 = t * 128
                        pvo = tm(j, 512, 512)
                        pif = tm(j, 1024, 8)
                        ga = gat.next()
                        TT("dve", ga.ap[:, 0:8], pif.ap[:, 0:8], bif.ap[:], ALU.add, [pif, bif], [ga])
                        ACT(ga.ap[:, 4:8], ga.ap[:, 4:8], AF.Exp, [ga], [ga], scale=-1.0)
                        ACT(ga.ap[:, 4:8], ga.ap[:, 4:8], AF.Ln, [ga], [ga], bias=oneb.ap[:, 0:1])
                        pc = S.pbank()
                        MM([(pc.ap[:, 0:4], triT.ap[:], ga.ap[:, 4:8], True, True),
                            (pc.ap[:, 4:8], selA.ap[:], ga.ap[:, 4:8], True, True),
                            (pc.ap[:, 8:12], selB.ap[:], ga.ap[:, 4:8], True, True)], [triT, selA, selB, ga], [pc])
                        ACT(A_tm.ap[:, t, :], pc.ap[:, 0:4], AF.Exp, [pc], [A_tm], scale=-1.0, bias=ln8b.ap[:, 0:1])
                        ACT(Gb.ap[:, 2 * t:2 * t + 2, :], pc.ap[:, 4:12].rearrange("p (a b) -> p a b", a=2), AF.Exp,
                            [pc], [Gb], scale=-1.0)
                        TT("dve", ga.ap[:, 8:12], ga.ap[:, 0:4], pc.ap[:, 0:4], ALU.add, [ga, pc], [ga])
                        ACT(ga.ap[:, 8:12], ga.ap[:, 8:12], AF.Exp, [ga], [ga])
                        rv = rvo.next()
                        TT("dve", rv.ap[:, :, 0:64], pvo.ap[:, 0:256].rearrange("p (h d) -> p h d", h=4),
                           ga.ap[:, 8:12].unsqueeze(2).to_broadcast([128, 4, 64]), ALU.mult, [pvo, ga], [rv])
                        CP("dve", rv.ap[:, :, 64:65], ga.ap[:, 8:12].unsqueeze(2), [ga], [rv])
                        LD(mrv.ap[r0:r0 + 128, :, :], rv.ap[:], rv, [rv], [mrv], q="pool")
                        so = soo.next()
                        ACT(so.ap[:], pvo.ap[:, 256:512], AF.Sigmoid, [pvo], [so])
                        LD(mso.ap[r0:r0 + 128, :], so.ap[:], so, [so], [mso], q="pool")
                        pq = tm(j, 1544, 512)
                        pk = tm(j, 2056, 512)
                        pvv = tm(j, 2568, 512)
                        sq_ = ssq.next()
                        for (pp_, off) in ((pq, 0), (pk, 8)):
                            sj = sqj.next()
                            ACT(sj.ap[:], pp_.ap[:], AF.Square, [pp_], [sj])
                            S.op("dve", (lambda sj=sj, off=off, sq_=sq_: lambda e: e.reduce_sum(
                                out=sq_.ap[:, off:off + 8], in_=sj.ap[:].rearrange("p (m d) -> p m d", m=8),
                                axis=AX.X))(), [sj], [sq_])
                        rsqrt_act(sq_.ap[:], sq_.ap[:], 1.0 / 64, [sq_], [sq_])
                        for (pp_, off, gg, dstT_) in ((pq, 0, gq, qT_), (pk, 8, gk, kT_)):
                            q1 = qn1.next()
                            TT("dve", q1.ap[:].rearrange("p (m d) -> p m d", m=8),
                               pp_.ap[:].rearrange("p (m d) -> p m d", m=8),
                               sq_.ap[:, off:off + 8].unsqueeze(2).to_broadcast([128, 8, 64]), ALU.mult,
                               [pp_, sq_], [q1])
                            qb = qnb.next()
                            TT("pool", qb.ap[:].rearrange("p (m d) -> p m d", m=8),
                               q1.ap[:].rearrange("p (m d) -> p m d", m=8),
                               gg.ap[:].unsqueeze(1).to_broadcast([128, 8, 64]), ALU.mult, [q1, gg], [qb])
                            pb = S.pbank()
                            pvw = bview(pb).rearrange("p (k n) -> p k n", k=8)
                            TRN([(pvw[:, m, :], qb.ap[:, m * 128:(m + 1) * 128]) for m in range(4)], [qb], [pb])
                            CP("act", dstT_.ap[:, :, j * 128:(j + 1) * 128], pvw[:, 0:4, :], [pb], [dstT_])
                        v_ = vo.next()
                        CP("act", v_.ap[:], pvv.ap[:], [pvv], [v_])
                        LD(avd.ap[r0:r0 + 128, :], v_.ap[:], v_, [v_], [avd], q="pool")
                    LD(aqT.ap[:, t0g:t0g + 512].rearrange("(m p) s -> p m s", p=128), qT_.ap[:], qT_, [qT_], [aqT], q="pool")
                    LD(akT.ap[:, t0g:t0g + 512].rearrange("(m p) s -> p m s", p=128), kT_.ap[:], kT_, [kT_], [akT], q="pool")

        def phase_B(L):
            with S.scope():
                prm = S.sbuf("prmB", [128, 1], F32, dma=True)
                gm = S.sbuf("gmB", [128, 256], F32)
                LDS([(gm.ap[:], I["m_out_norm_g"][L].partition_broadcast(128))], prm, [], [gm])
                qTh = S.sbuf("qTh", [64, S_tok], BF16, dma=True)
                kTh = S.sbuf("kTh", [64, S_tok], BF16, dma=True)
                ktm = S.sbuf("ktmB", [128, NT, 64], BF16, dma=True)
                rvbd = S.sbuf("rvbd", [128, NT, 130], BF16, dma=True)
                soh = S.sbuf("soh", [128, NT, 64], BF16, dma=True)
                gso = S.sbuf("gso", [128, NT, 64], F32)
                X = S.sbuf("Xst", [64, NCH, 65], F32)
                Cst = S.sbuf("Cst", [64, NCH + 1, 65], BF16)
                yTm = S.sbuf("yTm", [64, S_tok], BF16, dma=True)
                smt = S.ring("smt", [128, 128], BF16, 3)
                ndr = S.ring("ndr", [128, 65], F32, 3)
                sm = S.ring("smB", [128, 8], F32, 3)
                jk = S.ring("jkB", [128, 64], F32, 2)
                ybr = S.ring("ybB", [128, 64], BF16, 3)
                MSET("pool", rvbd.ap[:], 0.0, [rvbd])
                MSET("pool", Cst.ap[:, 0, :], 0.0, [Cst])
                for h in range(4):
                    LD(qTh.ap[:], mqT.ap[h * 64:(h + 1) * 64, :], qTh, [mqT], [qTh])
                    LD(kTh.ap[:], mkT.ap[h * 64:(h + 1) * 64, :], kTh, [mkT], [kTh])
                    LDS([(ktm.ap[:], mktm.ap[:, h * 64:(h + 1) * 64].rearrange("(i p) c -> p i c", p=128))],
                        ktm, [mktm], [ktm], slow=True)
                    mv = mrv.ap.rearrange("(i two p) h c -> two p i h c", two=2, p=64)
                    LDS([(rvbd.ap[0:64, :, 0:65], mv[0][:, :, h, :]),
                         (rvbd.ap[64:128, :, 65:130], mv[1][:, :, h, :])], rvbd, [mrv], [rvbd], slow=True)
                    LDS([(soh.ap[:], mso.ap[:, h * 64:(h + 1) * 64].rearrange("(i p) c -> p i c", p=128))],
                        soh, [mso], [soh], slow=True)
                    TT("pool", gso.ap[:], soh.ap[:], gm.ap[:, h * 64:(h + 1) * 64].unsqueeze(1).to_broadcast([128, NT, 64]),
                       ALU.mult, [soh, gm], [gso])
                    for i in range(NT):
                        pb = S.pbank()
                        MM([(pb.ap[0:64, 0:130], ktm.ap[:, i, :], rvbd.ap[:, i, :], True, True)], [ktm, rvbd], [pb])
                        ACT(X.ap[:, 2 * i, :], pb.ap[0:64, 0:65], AF.Copy, [pb, Gb], [X], scale=Gb.ap[0:64, 2 * i, h:h + 1])
                        ACT(X.ap[:, 2 * i + 1, :], pb.ap[0:64, 65:130], AF.Copy, [pb, Gb], [X],
                            scale=Gb.ap[0:64, 2 * i + 1, h:h + 1])
                    for c in range(1, NCH):
                        STT("dve", X.ap[:, c, :], X.ap[:, c - 1, :], Gb.ap[0:64, c, h:h + 1], X.ap[:, c, :],
                            ALU.mult, ALU.add, [X, Gb], [X])
                    CP("act", Cst.ap[:, 1:NCH + 1, :], X.ap[:, :, :], [X], [Cst])
                    for i in range(NT):
                        ts_ = slice(i * 128, (i + 1) * 128)
                        ps = S.pbank()
                        MM([(ps.ap[:, 0:128], kTh.ap[:, ts_], qTh.ap[:, ts_], True, True)], [kTh, qTh], [ps])
                        sm_ = smt.next()
                        TT("dve", sm_.ap[:], ps.ap[:, 0:128], triT.ap[:], ALU.mult, [ps, triT], [sm_])
                        po = S.pbank()
                        MM([(po.ap[:, 0:130], qTh.ap[:, ts_], Cst.ap[:, 2 * i:2 * i + 2, :].rearrange("p a b -> p (a b)"),
                             True, False),
                            (po.ap[:, 0:130], sm_.ap[:], rvbd.ap[:, i, :], False, True)], [qTh, Cst, sm_, rvbd], [po])
                        nd = ndr.next()
                        ACT(nd.ap[0:64, :], po.ap[0:64, 0:65], AF.Copy, [po, A_tm], [nd], scale=A_tm.ap[0:64, i, h:h + 1])
                        ACT(nd.ap[64:128, :], po.ap[64:128, 65:130], AF.Copy, [po, A_tm], [nd],
                            scale=A_tm.ap[64:128, i, h:h + 1])
                        s_ = sm.next()
                        ACT(s_.ap[:, 0:1], nd.ap[:, 64:65], AF.Abs, [nd], [s_])
                        TS("dve", s_.ap[:, 0:1], s_.ap[:, 0:1], 1.0, None, ALU.max, None, [s_], [s_])
                        S.op("dve", (lambda s_=s_: lambda e: e.reciprocal(out=s_.ap[:, 1:2], in_=s_.ap[:, 0:1]))(), [s_], [s_])
                        j_ = jk.next()
                        ACT(j_.ap[:], nd.ap[:, 0:64], AF.Square, [nd, s_], [j_, s_], scale=s_.ap[:, 1:2],
                            accum_out=s_.ap[:, 2:3])
                        rsqrt_act(s_.ap[:, 3:4], s_.ap[:, 2:3], 1.0 / 64, [s_], [s_])
                        TT("dve", s_.ap[:, 4:5], s_.ap[:, 3:4], s_.ap[:, 1:2], ALU.mult, [s_], [s_])
                        yb = ybr.next()
                        STT("dve", yb.ap[:], nd.ap[:, 0:64], s_.ap[:, 4:5], gso.ap[:, i, :], ALU.mult, ALU.mult,
                            [nd, s_, gso], [yb])
                        pt = S.pbank()
                        ptv = bview(pt)
                        TRN([(ptv[0:64, 0:128], yb.ap[:, :])], [yb], [pt])
                        CP("act", yTm.ap[:, ts_], ptv[0:64, 0:128], [pt], [yTm])
                    LD(yT.ap[h * 64:(h + 1) * 64, :], yTm.ap[:], yTm, [yTm], [yT], q="pool")

        def phase_C(L, bg=None, bg_every=4):
            lam_init = 0.8 - 0.6 * math.exp(-0.3 * L)
            with S.scope():
                prm = S.sbuf("prmC", [128, 1], F32, dma=True)
                lv = S.sbuf("lvC", [128, 4, 64], F32)
                gsub = S.sbuf("gsub", [128, 128], F32)
                lam = S.sbuf("lam", [128, 4], F32)
                ljk = S.sbuf("ljk", [128, 64], F32)
                LDS([(lv.ap[:, 0, :], I["a_lambda_q1"][L].partition_broadcast(128)),
                     (lv.ap[:, 1, :], I["a_lambda_k1"][L].partition_broadcast(128)),
                     (lv.ap[:, 2, :], I["a_lambda_q2"][L].partition_broadcast(128)),
                     (lv.ap[:, 3, :], I["a_lambda_k2"][L].partition_broadcast(128)),
                     (gsub.ap[:], I["a_subln_g"][L].partition_broadcast(128))], prm, [], [lv, gsub])
                MSET("dve", lam.ap[:], 0.0, [lam])
                S.op("dve", lambda e: e.tensor_tensor(out=ljk.ap[:], in0=lv.ap[:, 0, :], in1=lv.ap[:, 1, :], op=ALU.mult),
                     [lv], [ljk])
                S.op("dve", lambda e: e.reduce_sum(out=lam.ap[:, 0:1], in_=ljk.ap[:], axis=AX.X), [ljk], [lam])
                S.op("dve", lambda e: e.tensor_tensor(out=ljk.ap[:], in0=lv.ap[:, 2, :], in1=lv.ap[:, 3, :], op=ALU.mult),
                     [lv, lam], [ljk])
                S.op("dve", lambda e: e.reduce_sum(out=lam.ap[:, 1:2], in_=ljk.ap[:], axis=AX.X), [ljk], [lam])
                ACT(lam.ap[:, 0:2], lam.ap[:, 0:2], AF.Exp, [lam], [lam])
                TT("dve", lam.ap[:, 2:3], lam.ap[:, 0:1], lam.ap[:, 1:2], ALU.subtract, [lam], [lam])
                TS("dve", lam.ap[:, 3:4], lam.ap[:, 2:3], lam_init, None, ALU.add, None, [lam], [lam])
                TS("dve", gsub.ap[:], gsub.ap[:], 1.0 - lam_init, None, ALU.mult, None, [gsub], [gsub])

                KT = S.ring("KTC", [128, S_tok], BF16, 2, dma=True)
                VV = S.ring("VVC", [128, NT, 129], BF16, 2, dma=True)
                QT = S.ring("QTC", [128, 512], BF16, 3, dma=True)
                PT = S.ring("PTC", [128, 512], BF16, 6)
                yta = S.ring("ytaC", [128, 512], BF16, 2, dma=True)
                sm = S.ring("smC", [128, 8], F32, 4)
                t2r = S.ring("t2C", [128, 128], F32, 2)
                yar = S.ring("yaC", [128, 128], F32, 2)
                jkr = S.ring("jkC", [128, 128], BF16, 2)
                ybr = S.ring("ybC", [128, 128], BF16, 2)
                for vb in VV.bufs:
                    MSET("pool", vb.ap[:, :, 128:129], 1.0, [vb])
                accb = S.reserve(3)

                def acc(m, j):
                    i_ = m * 4 + j
                    return accb[i_ // 3], (i_ % 3) * 132

                accs_r = S.ring("accsC", [128, 1056], F32, 2)
                fin_sm = S.ring("finsm", [128, 16], F32, 3)
                o_r = S.ring("oC", [128, 8, 128], F32, 2)
                ya_r = S.ring("yaC2", [128, 4, 128], F32, 2)
                sq_r = S.ring("sqC", [128, 4, 128], F32, 2)
                yb_r = S.ring("ybC2", [128, 4, 128], BF16, 2)
                deferred = []

                def finalize(h, qg):
                    ac = accs_r.next()
                    s_ = fin_sm.next()
                    o = o_r.next()
                    ya = ya_r.next()
                    sq = sq_r.next()
                    yb = yb_r.next()
                    yt = yta.next()
                    for bi in range(3):
                        w = 396 if bi < 2 else 264
                        CP("dve", ac.ap[:, bi * 396:bi * 396 + w], accb[bi].ap[:, 0:w], [accb[bi]], [ac])
                    acv = ac.ap[:].rearrange("p (i c) -> p i c", c=132)
                    S.op("dve", lambda e: e.reciprocal(out=s_.ap[:, 0:8].unsqueeze(2), in_=acv[:, :, 128:129]), [ac], [s_])
                    TS("dve", s_.ap[:, 4:8], s_.ap[:, 4:8], lam.ap[:, 3:4], None, ALU.mult, None, [s_, lam], [s_])
                    TT("dve", o.ap[:], acv[:, :, 0:128], s_.ap[:, 0:8].unsqueeze(2).to_broadcast([128, 8, 128]), ALU.mult,
                       [ac, s_], [o])
                    TT("dve", ya.ap[:], o.ap[:, 0:4, :], o.ap[:, 4:8, :], ALU.subtract, [o], [ya])
                    TT("dve", sq.ap[:], ya.ap[:], ya.ap[:], ALU.mult, [ya], [sq])
                    S.op("dve", lambda e: e.reduce_sum(out=s_.ap[:, 8:12], in_=sq.ap[:], axis=AX.X), [sq, s_], [s_])

                    def F2():
                        rsqrt_act(s_.ap[:, 12:16], s_.ap[:, 8:12], 1.0 / 128, [s_], [s_])

                    def F3():
                        TT("dve", sq.ap[:], ya.ap[:], s_.ap[:, 12:16].unsqueeze(2).to_broadcast([128, 4, 128]), ALU.mult,
                           [ya, s_, sq], [sq])
                        TT("dve", yb.ap[:], sq.ap[:], gsub.ap[:].unsqueeze(1).to_broadcast([128, 4, 128]), ALU.mult,
                           [sq, gsub], [yb])
                        pt_ = S.pbank()
                        ptv = bview(pt_).rearrange("p (k n) -> p k n", k=8)
                        TRN([(ptv[:, j, :], yb.ap[:, j, :]) for j in range(4)], [yb], [pt_])
                        CP("dve", yt.ap[:].rearrange("p (j n) -> p j n", j=4), ptv[:, 0:4, :], [pt_], [yt])
                        LD(yT.ap[512 + h * 128:512 + (h + 1) * 128, qg * 512:(qg + 1) * 512], yt.ap[:], yt, [yt], [yT],
                           q="pool")
                    deferred.append([3, F2])
                    deferred.append([6, F3])

                def tick(flush=False):
                    for d_ in list(deferred):
                        d_[0] -= 1
                        if d_[0] <= 0 or flush:
                            deferred.remove(d_)
                            d_[1]()

                def stage1(h, qg, kb, kt, vv, qt):
                    jj = max(0, kb - 4 * qg)
                    c0_ = jj * 128
                    di = kb - 4 * qg + NT
                    pts = []
                    for m in range(2):
                        pb = S.pbank()
                        MM([(pb.ap[:, c0_:512], kt.ap[m * 64:(m + 1) * 64, kb * 128:(kb + 1) * 128],
                             qt.ap[m * 64:(m + 1) * 64, c0_:512], True, True)], [kt, qt], [pb])
                        pt = PT.next()
                        ACT(pt.ap[:, c0_:512], pb.ap[:, c0_:512], AF.Exp, [pb, abt], [pt], scale=0.125,
                            bias=abt.ap[:, h, di:di + 1])
                        if kb >= 4 * qg:
                            TT("pool", pt.ap[:, c0_:c0_ + 128], pt.ap[:, c0_:c0_ + 128], triU.ap[:], ALU.mult,
                               [pt, triU], [pt])
                        pts.append(pt)

                    def stage2():
                        items = []
                        for m in range(2):
                            for j in range(jj, 4):
                                bk, off = acc(m, j)
                                items.append((bk.ap[:, off:off + 129], pts[m].ap[:, j * 128:(j + 1) * 128],
                                              vv.ap[:, kb, :], kb == 0 and off == 0, kb == 4 * qg + j, True))
                        MM(items, pts + [vv], accb)
                        if kb == 4 * qg + 3:
                            finalize(h, qg)
                    return stage2

                pending = None
                ucount = 0
                for h in range(4):
                    kt = KT.next()
                    vv = VV.next()
                    LD(kt.ap[:], akT.ap[h * 128:(h + 1) * 128, :], kt, [akT], [kt])
                    LDS([(vv.ap[:, :, 0:128], avd.ap[:, h * 128:(h + 1) * 128].rearrange("(i p) c -> p i c", p=128))],
                        vv, [avd], [vv])
                    for qg in range(NG):
                        qt = QT.next()
                        LD(qt.ap[:], aqT.ap[h * 128:(h + 1) * 128, qg * 512:(qg + 1) * 512], qt, [aqT], [qt])
                        for kb in range(4 * qg + 4):
                            s2 = stage1(h, qg, kb, kt, vv, qt)
                            if pending is not None:
                                pending()
                            pending = s2
                            ucount += 1
                            tick()
                            if bg is not None and ucount % bg_every == 0:
                                next(bg, None)
                pending()
                tick(flush=True)
                S.release(accb)

        def phase_D(L, hsrc, hdst):
            moe = (L % 2 == 1)
            with S.scope():
                prm = S.sbuf("prmD", [128, 1], F32, dma=True)
                gff = S.sbuf("gffD", [128, 1024], F32)
                gpl = S.sbuf("gplD", [128, 1024], F32)
                LDS([(gff.ap[:], I["ffn_norm_g"][L].partition_broadcast(128)),
                     (gpl.ap[:], I["ple_norm_g"][L].partition_broadcast(128))], prm, [], [gff, gpl])
                wpp = S.sbuf("wppD", [128, 2, 1024], BF16, dma=True)
                LD(wpp.ap[:], Wb["w_ple_proj"].ap[L].rearrange("(k p) n -> p k n", p=128), wpp, [Wb["w_ple_proj"]], [wpp])
                if moe:
                    wrt = S.sbuf("wrtD", [128, 8, 8], F32, dma=True)
                    LDS([(wrt.ap[:], I["router_w"][0].rearrange("(k p) n -> p k n", p=128))], wrt, [], [wrt])
                wblk = S.ring("wblk", [128, 8, 512], BF16, 4, dma=True)
                wdblk = S.ring("wdblk", [128, 4, 512], BF16, 3, dma=True)
                hin = S.ring("hinD", [128, 1024], F32, 5, dma=True)
                hw = [S.sbuf("hw%d" % j, [128, 1024], F32, dma=True) for j in range(4)]
                yTg = S.ring("yTg", [128, 8, 512], BF16, 2, dma=True)
                junk = S.ring("junkD", [128, 1024], BF16, 2)
                ssr = S.ring("ssD", [128, 4], F32, 2)
                cbf = S.ring("cbfD", [128, 1024], BF16, 2)
                cT = S.sbuf("cTD", [128, 8, 512], BF16)
                FT = (FFE if moe else FFD) // 128
                hT = S.sbuf("hTD", [128, FT, 512], BF16)
                sgr = S.ring("sgD", [128, 512], F32, 3)
                pin = S.ring("pinD", [128, 256], F32, 4, dma=True)
                pbf = S.ring("pbfD", [128, 256], BF16, 2)
                pTt = S.sbuf("pTD", [128, 2, 512], BF16)
                gsb = S.ring("gsbD", [128, 512], F32, 2)
                if moe:
                    cf32 = S.ring("cf32", [128, 1024], F32, 2)
                    cTf = S.ring("cTf", [128, 8, 128], F32, 2)
                    rl = S.ring("rlD", [128, 8], F32, 2)
                    mx8 = S.ring("mx8", [128, 8], F32, 2)
                    rex = S.ring("rexD", [128, 8], F32, 2)
                    rsm = S.ring("rsmD", [128, 4], F32, 2)
                    comb = [S.sbuf("comb%d" % j, [128, 8], F32) for j in range(4)]

                def norm_T(gb_, dst, extra=None):
                    ss = ssr.next()
                    for j in range(4):
                        jk = junk.next()
                        ACT(jk.ap[:], hw[j].ap[:], AF.Square, [hw[j]], [jk, ss], accum_out=ss.ap[:, j:j + 1])
                    rsqrt_act(ss.ap[:], ss.ap[:], 1.0 / 1024, [ss], [ss])
                    for j in range(4):
                        cb = cbf.next()
                        STT("dve", cb.ap[:], hw[j].ap[:], ss.ap[:, j:j + 1], gb_.ap[:], ALU.mult, ALU.mult,
                            [hw[j], ss, gb_], [cb])
                        pb = S.pbank()
                        pv = bview(pb).rearrange("p (k n) -> p k n", k=8)
                        TRN([(pv[:, k, :], cb.ap[:, k * 128:(k + 1) * 128]) for k in range(8)], [cb], [pb])
                        CP("act" if j % 2 == 0 else "dve", dst.ap[:, :, j * 128:(j + 1) * 128], pv, [pb], [dst])
                        if extra is not None:
                            extra(j, ss)

                def ffn_expert(wg_ap, wu_ap, wd_ap, F_, scale_cols):
                    nfb = (F_ + 511) // 512
                    wgv = wg_ap.rearrange("(k p) n -> p k n", p=128)
                    wuv = wu_ap.rearrange("(k p) n -> p k n", p=128)
                    wdv = wd_ap.rearrange("(f p) n -> p f n", p=128)
                    for fb in range(nfb):
                        fw = min(512, F_ - fb * 512)
                        wg = wblk.next()
                        LD(wg.ap[:, :, 0:fw], wgv[:, :, fb * 512:fb * 512 + fw], wg, [WSRC], [wg])
                        wu = wblk.next()
                        LD(wu.ap[:, :, 0:fw], wuv[:, :, fb * 512:fb * 512 + fw], wu, [WSRC], [wu])
                        for ft in range(fw // 128):
                            f = fb * 4 + ft
                            pg = S.pbank()
                            MM([(pg.ap[:], wg.ap[:, k, ft * 128:(ft + 1) * 128], cT.ap[:, k, :], k == 0, k == 7)
                                for k in range(8)], [wg, cT], [pg])
                            pu = S.pbank()
                            MM([(pu.ap[:], wu.ap[:, k, ft * 128:(ft + 1) * 128], cT.ap[:, k, :], k == 0, k == 7)
                                for k in range(8)], [wu, cT], [pu])
                            sg = sgr.next()
                            ACT(sg.ap[:], pg.ap[:], AF.Silu, [pg], [sg])
                            TT("dve", hT.ap[:, f, :], pu.ap[:], sg.ap[:], ALU.mult, [pu, sg], [hT])
                    nft = F_ // 128
                    for half in range(2):
                        accs = [S.pbank() for _ in range(4)]
                        for fb in range(nfb):
                            nf = min(4, nft - fb * 4)
                            wd = wdblk.next()
                            LD(wd.ap[:, 0:nf, :], wdv[:, fb * 4:fb * 4 + nf, half * 512:(half + 1) * 512], wd, [WSRC], [wd])
                            items = []
                            for j in range(4):
                                for ft in range(nf):
                                    f = fb * 4 + ft
                                    items.append((accs[j].ap[:], hT.ap[:, f, j * 128:(j + 1) * 128], wd.ap[:, ft, :],
                                                  f == 0, f == nft - 1))
                            MM(items, [hT, wd], accs)
                        for j in range(4):
                            hs = hw[j].ap[:, half * 512:(half + 1) * 512]
                            if scale_cols is None:
                                TT("dve", hs, accs[j].ap[:], hs, ALU.add, [accs[j], hw[j]], [hw[j]])
                            else:
                                STT("dve", hs, accs[j].ap[:], scale_cols[j], hs, ALU.mult, ALU.add,
                                    [accs[j], hw[j]] + comb, [hw[j]])

                WSRC = Buf("wsrc_all")
                for g in range(NG):
                    t0g = g * 512
                    yg = yTg.next()
                    LD(yg.ap[:], yT.ap[:, t0g:t0g + 512].rearrange("(k p) s -> p k s", p=128), yg, [yT], [yg])
                    hbs = []
                    for j in range(4):
                        hb = hin.next()
                        hbs.append(hb)
                        LD(hb.ap[:], hsrc.ap[t0g + j * 128:t0g + (j + 1) * 128, :], hb, [hsrc], [hb])
                    wos = []
                    for half in range(2):
                        wo = wblk.next()
                        LD(wo.ap[:], Wb["w_out"].ap[L].rearrange("(k p) n -> p k n", p=128)[:, :, half * 512:(half + 1) * 512],
                           wo, [WSRC], [wo])
                        wos.append(wo)
                    for j in range(4):
                        for half in range(2):
                            pb = S.pbank()
                            MM([(pb.ap[:], yg.ap[:, k, j * 128:(j + 1) * 128], wos[half].ap[:, k, :], k == 0, k == 7)
                                for k in range(8)], [yg, wos[half]], [pb])
                            TT("dve", hw[j].ap[:, half * 512:(half + 1) * 512], pb.ap[:],
                               hbs[j].ap[:, half * 512:(half + 1) * 512], ALU.add, [pb, hbs[j]], [hw[j]])
                    if not moe:
                        norm_T(gff, cT)
                        ffn_expert(Wb["dense_w_gate"].ap[0], Wb["dense_w_up"].ap[0], Wb["dense_w_down"].ap[0], FFD, None)
                    else:
                        def router(j, ss):
                            cf = cf32.next()
                            STT("dve", cf.ap[:], hw[j].ap[:], ss.ap[:, j:j + 1], gff.ap[:], ALU.mult, ALU.mult,
                                [hw[j], ss, gff], [cf])
                            ct = cTf.next()
                            for kk in range(2):
                                pb = S.pbank()
                                pv = pb.ap[:].rearrange("p (k n) -> p k n", k=4)

                                def f(e, pv=pv, cf=cf, kk=kk):
                                    r = None
                                    for k in range(4):
                                        r = e.transpose(out=pv[:, k, :], in_=cf.ap[:, (kk * 4 + k) * 128:(kk * 4 + k + 1) * 128],
                                                        identity=identf.ap[:])
                                    return r
                                S.op("pe", f, [cf, identf], [pb])
                                CP("act", ct.ap[:, kk * 4:kk * 4 + 4, :], pv, [pb], [ct])
                            pl = S.pbank()
                            MM([(pl.ap[:, 0:8], ct.ap[:, k, :], wrt.ap[:, k, :], k == 0, k == 7) for k in range(8)],
                               [ct, wrt], [pl])
                            lg = rl.next()
                            CP("act", lg.ap[:], pl.ap[:, 0:8], [pl], [lg])
                            m8 = mx8.next()
                            S.op("dve", (lambda m8=m8, lg=lg: lambda e: e.max(out=m8.ap[:], in_=lg.ap[:]))(), [lg], [m8])
                            ex = rex.next()
                            r4 = rsm.next()
                            TS("dve", r4.ap[:, 0:1], m8.ap[:, 0:1], -1.0, None, ALU.mult, None, [m8], [r4])
                            ACT(ex.ap[:], lg.ap[:], AF.Exp, [lg, r4], [ex], bias=r4.ap[:, 0:1])
                            ACT(r4.ap[:, 1:2], m8.ap[:, 1:2], AF.Exp, [m8, r4], [r4], bias=r4.ap[:, 0:1])
                            TS("dve", r4.ap[:, 1:2], r4.ap[:, 1:2], 1.0, None, ALU.add, None, [r4], [r4])
                            S.op("dve", (lambda r4=r4: lambda e: e.reciprocal(out=r4.ap[:, 2:3], in_=r4.ap[:, 1:2]))(), [r4], [r4])
                            TS("dve", comb[j].ap[:], lg.ap[:], m8.ap[:, 1:2], None, ALU.is_ge, None, [lg, m8], [comb[j]])
                            TT("dve", comb[j].ap[:], comb[j].ap[:], ex.ap[:], ALU.mult, [ex, comb[j]], [comb[j]])
                            TS("dve", comb[j].ap[:], comb[j].ap[:], r4.ap[:, 2:3], None, ALU.mult, None, [r4, comb[j]], [comb[j]])
                        norm_T(gff, cT, router)
                        for ex_ in range(nexp):
                            ffn_expert(Wb["moe_w_gate"].ap[0][ex_], Wb["moe_w_up"].ap[0][ex_], Wb["moe_w_down"].ap[0][ex_],
                                       FFE, [comb[j].ap[:, ex_:ex_ + 1] for j in range(4)])
                    norm_T(gpl, cT)
                    for j in range(4):
                        pi_ = pin.next()
                        LD(pi_.ap[:], I["p"][L][t0g + j * 128:t0g + (j + 1) * 128, :], pi_, [], [pi_])
                        pb_ = pbf.next()
                        CP("pool", pb_.ap[:], pi_.ap[:], [pi_], [pb_])
                        pk_ = S.pbank()
                        pv = bview(pk_).rearrange("p (k n) -> p k n", k=8)
                        TRN([(pv[:, k, :], pb_.ap[:, k * 128:(k + 1) * 128]) for k in range(2)], [pb_], [pk_])
                        CP("act", pTt.ap[:, :, j * 128:(j + 1) * 128], pv[:, 0:2, :], [pk_], [pTt])
                    wgs = []
                    for half in range(2):
                        wo = wblk.next()
                        LD(wo.ap[:], Wb["w_ple_gate"].ap[L].rearrange("(k p) n -> p k n", p=128)[:, :, half * 512:(half + 1) * 512],
                           wo, [WSRC], [wo])
                        wgs.append(wo)
                    for j in range(4):
                        for half in range(2):
                            pg = S.pbank()
                            MM([(pg.ap[:], cT.ap[:, k, j * 128:(j + 1) * 128], wgs[half].ap[:, k, :], k == 0, k == 7)
                                for k in range(8)], [cT, wgs[half]], [pg])
                            pp2 = S.pbank()
                            MM([(pp2.ap[:], pTt.ap[:, k, j * 128:(j + 1) * 128], wpp.ap[:, k, half * 512:(half + 1) * 512],
                                 k == 0, k == 1) for k in range(2)], [pTt, wpp], [pp2])
                            gs = gsb.next()
                            ACT(gs.ap[:], pg.ap[:], AF.Sigmoid, [pg], [gs])
                            TT("dve", gs.ap[:], pp2.ap[:], gs.ap[:], ALU.mult, [pp2, gs], [gs])
                            hs = hw[j].ap[:, half * 512:(half + 1) * 512]
                            TT("dve", hs, hs, gs.ap[:], ALU.add, [gs, hw[j]], [hw[j]])
                        LD(hdst.ap[t0g + j * 128:t0g + (j + 1) * 128, :], hw[j].ap[:], hw[j], [hw[j]], [hdst], q="pool")


        def phase_D_moe(L, hsrc, hdst):
            NTILE = 2 * NG + 8
            NSLOT = NTILE * 512
            hmid = S.dram("hmid", [S_tok, D], F32)
            csd = S.dram("csd", [S_tok, D], BF16)
            xsd = S.dram("xsd", [NSLOT, D], BF16)
            ysd = S.dram("ysd", [NSLOT, D], F32)
            WSRC = Buf("wsrc_all2")
            with S.scope():
                E1 = S.sbuf("E1t", [128, NT, 8], F32)
                E2 = S.sbuf("E2t", [128, NT, 8], F32)
                POS = S.sbuf("POSt", [128, NT, 8], F32)
                GT = S.sbuf("GTt", [128, NT, 2], F32)
                carry = S.sbuf("carry", [128, 8], F32)
                DSTi = S.sbuf("DSTi", [128, NT, 2], I32)
                IGi = S.sbuf("IGi", [128, NTILE, 7], I32)
                IDi = S.sbuf("IDi", [128, NTILE, 14], I32)
                triS = S.sbuf("triS", [128, 128], F32)
                ones128 = S.sbuf("ones128", [128, 128], F32)

                def mk(e):
                    e.memset(triS.ap[:], 1.0)
                    e.affine_select(out=triS.ap[:], in_=triS.ap[:], compare_op=ALU.is_ge, fill=0.0,
                                    base=-1, pattern=[[1, 128]], channel_multiplier=-1)
                    e.memset(carry.ap[:], 0.0)
                    return e.memset(ones128.ap[:], 1.0)
                S.op("pool", mk, (), [triS, carry, ones128])

                with S.scope():
                    prm = S.sbuf("prmM", [128, 1], F32, dma=True)
                    gff = S.sbuf("gffM", [128, 1024], F32)
                    wrt = S.sbuf("wrtM", [128, 8, 8], F32)
                    LDS([(gff.ap[:], I["ffn_norm_g"][L].partition_broadcast(128)),
                         (wrt.ap[:], I["router_w"][0].rearrange("(k p) n -> p k n", p=128))], prm, [], [gff, wrt])
                    wblk = S.ring("wblkM", [128, 8, 512], BF16, 2, dma=True)
                    hin = S.ring("hinM", [128, 1024], F32, 5, dma=True)
                    hw = S.ring("hwM", [128, 1024], F32, 6, dma=True)
                    yTg = S.ring("yTgM", [128, 8, 512], BF16, 2, dma=True)
                    junk = S.ring("junkM", [128, 1024], BF16, 2)
                    ssr = S.ring("ssM", [128, 4], F32, 4)
                    cbf = S.ring("cbfM", [128, 1024], BF16, 3, dma=True)
                    cf32 = S.ring("cf32M", [128, 1024], F32, 2)
                    cTf = S.ring("cTfM", [128, 8, 128], F32, 2)
                    rl = S.ring("rlM", [128, 8], F32, 3)
                    mx8 = S.ring("mx8M", [128, 8], F32, 3)
                    rsm = S.ring("rsmM", [128, 4], F32, 3)
                    selr = S.ring("selM", [128, 8], F32, 3)
                    wos = []
                    for half in range(2):
                        wo = wblk.next()
                        LD(wo.ap[:], Wb["w_out"].ap[L].rearrange("(k p) n -> p k n", p=128)[:, :, half * 512:(half + 1) * 512],
                           wo, [WSRC], [wo])
                        wos.append(wo)
                    for g in range(NG):
                        t0g = g * 512
                        yg = yTg.next()
                        LD(yg.ap[:], yT.ap[:, t0g:t0g + 512].rearrange("(k p) s -> p k s", p=128), yg, [yT], [yg])
                        for j in range(4):
                            t = g * 4 + j
                            r0 = t * 128
                            hb = hin.next()
                            LD(hb.ap[:], hsrc.ap[r0:r0 + 128, :], hb, [hsrc], [hb])
                            hwj = hw.next()
                            for half in range(2):
                                pb = S.pbank()
                                MM([(pb.ap[:], yg.ap[:, k, j * 128:(j + 1) * 128], wos[half].ap[:, k, :], k == 0, k == 7)
                                    for k in range(8)], [yg, wos[half]], [pb])
                                TT("dve", hwj.ap[:, half * 512:(half + 1) * 512], pb.ap[:],
                                   hb.ap[:, half * 512:(half + 1) * 512], ALU.add, [pb, hb], [hwj])
                            LD(hmid.ap[r0:r0 + 128, :], hwj.ap[:], hwj, [hwj], [hmid], q="pool")
                            ss = ssr.next()
                            jk = junk.next()
                            ACT(jk.ap[:], hwj.ap[:], AF.Square, [hwj], [jk, ss], accum_out=ss.ap[:, 0:1])
                            rsqrt_act(ss.ap[:, 0:1], ss.ap[:, 0:1], 1.0 / 1024, [ss], [ss])
                            cb = cbf.next()
                            STT("dve", cb.ap[:], hwj.ap[:], ss.ap[:, 0:1], gff.ap[:], ALU.mult, ALU.mult, [hwj, ss, gff], [cb])
                            LD(csd.ap[r0:r0 + 128, :], cb.ap[:], cb, [cb], [csd], q="pool")
                            cf = cf32.next()
                            STT("dve", cf.ap[:], hwj.ap[:], ss.ap[:, 0:1], gff.ap[:], ALU.mult, ALU.mult, [hwj, ss, gff], [cf])
                            ct = cTf.next()
                            for kk in range(2):
                                pb = S.pbank()
                                pv = pb.ap[:].rearrange("p (k n) -> p k n", k=4)

                                def f(e, pv=pv, cf=cf, kk=kk):
                                    r = None
                                    for k in range(4):
                                        r = e.transpose(out=pv[:, k, :], in_=cf.ap[:, (kk * 4 + k) * 128:(kk * 4 + k + 1) * 128],
                                                        identity=identf.ap[:])
                                    return r
                                S.op("pe", f, [cf, identf], [pb])
                                CP("act", ct.ap[:, kk * 4:kk * 4 + 4, :], pv, [pb], [ct])
                            pl = S.pbank()
                            MM([(pl.ap[:, 0:8], ct.ap[:, k, :], wrt.ap[:, k, :], k == 0, k == 7) for k in range(8)],
                               [ct, wrt], [pl])
                            lg = rl.next()
                            CP("act", lg.ap[:], pl.ap[:, 0:8], [pl], [lg])
                            m8 = mx8.next()
                            S.op("dve", (lambda m8=m8, lg=lg: lambda e: e.max(out=m8.ap[:], in_=lg.ap[:]))(), [lg], [m8])
                            TS("dve", E1.ap[:, t, :], lg.ap[:], m8.ap[:, 0:1], None, ALU.is_equal, None, [lg, m8], [E1])
                            TS("dve", E2.ap[:, t, :], lg.ap[:], m8.ap[:, 1:2], None, ALU.is_equal, None, [lg, m8], [E2])
                            sel = selr.next()
                            TT("dve", sel.ap[:], E1.ap[:, t, :], E2.ap[:, t, :], ALU.add, [E1, E2], [sel])
                            r4 = rsm.next()
                            TS("dve", r4.ap[:, 0:1], m8.ap[:, 0:1], -1.0, None, ALU.mult, None, [m8], [r4])
                            ACT(r4.ap[:, 1:2], m8.ap[:, 1:2], AF.Exp, [m8, r4], [r4], bias=r4.ap[:, 0:1])
                            TS("dve", r4.ap[:, 1:2], r4.ap[:, 1:2], 1.0, None, ALU.add, None, [r4], [r4])
                            S.op("dve", (lambda r4=r4, t=t: lambda e: e.reciprocal(out=GT.ap[:, t, 0:1], in_=r4.ap[:, 1:2]))(),
                                 [r4], [GT])
                            TS("dve", GT.ap[:, t, 1:2], GT.ap[:, t, 0:1], -1.0, 1.0, ALU.mult, ALU.add, [GT], [GT])
                            pp = S.pbank()
                            MM([(pp.ap[:, 0:8], triS.ap[:], sel.ap[:], True, True),
                                (pp.ap[:, 8:16], ones128.ap[:], sel.ap[:], True, True)], [triS, ones128, sel], [pp])
                            TT("dve", POS.ap[:, t, :], pp.ap[:, 0:8], carry.ap[:], ALU.add, [pp, carry], [POS])
                            TT("dve", carry.ap[:], pp.ap[:, 8:16], carry.ap[:], ALU.add, [pp, carry], [carry])

                with S.scope():
                    ci = S.sbuf("ciM", [128, 8], I32)
                    ntf = S.sbuf("ntfM", [128, 8], F32)
                    cum = S.sbuf("cumM", [128, 8], F32)
                    base = S.sbuf("baseM", [128, 8], F32)
                    iot_i = S.sbuf("iotiM", [128, NTILE], I32)
                    iot = S.sbuf("iotM", [128, NTILE], F32)
                    eid = S.sbuf("eidM", [128, NTILE], F32)
                    tmpe = S.sbuf("tmpeM", [128, NTILE], F32)
                    pb_i = S.sbuf("pbiM", [128, 14], I32)
                    pbf_ = S.sbuf("pbfM", [128, 14], F32)
                    igf = S.sbuf("igfM", [128, NTILE, 14], F32)
                    tmp3 = S.sbuf("tmp3M", [128, NT, 8], F32)
                    tmp4 = S.sbuf("tmp4M", [128, NT, 8], F32)
                    dstf = S.sbuf("dstfM", [128, NT, 2], F32)
                    TS("dve", ntf.ap[:], carry.ap[:], 511.0, None, ALU.add, None, [carry], [ntf])
                    CP("dve", ci.ap[:], ntf.ap[:], [ntf], [ci])
                    S.op("dve", lambda e: e.tensor_single_scalar(out=ci.ap[:], in_=ci.ap[:], scalar=9, op=ALU.arith_shift_right),
                         [ci], [ci])
                    CP("dve", ntf.ap[:], ci.ap[:], [ci], [ntf])
                    CP("dve", cum.ap[:, 0:1], ntf.ap[:, 0:1], [ntf], [cum])
                    for e_ in range(1, 8):
                        TT("dve", cum.ap[:, e_:e_ + 1], cum.ap[:, e_ - 1:e_], ntf.ap[:, e_:e_ + 1], ALU.add, [cum, ntf], [cum])
                    TT("dve", base.ap[:], cum.ap[:], ntf.ap[:], ALU.subtract, [cum, ntf], [base])
                    TS("dve", base.ap[:], base.ap[:], 512.0, None, ALU.mult, None, [base], [base])

                    def mk2(e):
                        e.iota(iot_i.ap[:], pattern=[[1, NTILE]], base=0, channel_multiplier=0)
                        return e.iota(pb_i.ap[:], pattern=[[128, 14]], base=0, channel_multiplier=1)
                    S.op("pool", mk2, (), [iot_i, pb_i])
                    CP("dve", iot.ap[:], iot_i.ap[:], [iot_i], [iot])
                    CP("dve", pbf_.ap[:], pb_i.ap[:], [pb_i], [pbf_])
                    MSET("dve", eid.ap[:], 0.0, [eid])
                    for e_ in range(8):
                        TS("dve", tmpe.ap[:], iot.ap[:], cum.ap[:, e_:e_ + 1], None, ALU.is_ge, None, [iot, cum], [tmpe])
                        TT("dve", eid.ap[:], eid.ap[:], tmpe.ap[:], ALU.add, [eid, tmpe], [eid])
                    TS("dve", eid.ap[:], eid.ap[:], 7.0, None, ALU.min, None, [eid], [eid])
                    TS("dve", tmpe.ap[:], eid.ap[:], 896.0, None, ALU.mult, None, [eid], [tmpe])
                    TT("dve", igf.ap[:, :, 0:7], tmpe.ap[:].unsqueeze(2).to_broadcast([128, NTILE, 7]),
                       pbf_.ap[:, 0:7].unsqueeze(1).to_broadcast([128, NTILE, 7]), ALU.add, [tmpe, pbf_], [igf])
                    CP("dve", IGi.ap[:], igf.ap[:, :, 0:7], [igf], [IGi])
                    TS("dve", tmpe.ap[:], eid.ap[:], 1792.0, None, ALU.mult, None, [eid, igf], [tmpe])
                    TT("dve", igf.ap[:], tmpe.ap[:].unsqueeze(2).to_broadcast([128, NTILE, 14]),
                       pbf_.ap[:].unsqueeze(1).to_broadcast([128, NTILE, 14]), ALU.add, [tmpe, pbf_, IGi], [igf])
                    CP("dve", IDi.ap[:], igf.ap[:], [igf], [IDi])
                    TT("dve", tmp3.ap[:], POS.ap[:], base.ap[:].unsqueeze(1).to_broadcast([128, NT, 8]), ALU.add,
                       [POS, base], [tmp3])
                    for (k_, Ek) in ((0, E1), (1, E2)):
                        TT("dve", tmp4.ap[:], tmp3.ap[:], Ek.ap[:], ALU.mult, [tmp3, Ek], [tmp4])
                        S.op("dve", (lambda k_=k_: lambda e: e.reduce_sum(out=dstf.ap[:, :, k_:k_ + 1], in_=tmp4.ap[:], axis=AX.X))(),
                             [tmp4, dstf], [dstf])
                    CP("dve", DSTi.ap[:], dstf.ap[:], [dstf], [DSTi])

                with S.scope():
                    cbt = S.ring("cbtM", [128, 1024], BF16, 4, dma=True)
                    for t in range(NT):
                        cb = cbt.next()
                        LD(cb.ap[:], csd.ap[t * 128:(t + 1) * 128, :], cb, [csd], [cb])
                        for k_ in range(2):
                            S.dma((lambda cb=cb, t=t, k_=k_: lambda e: [e.indirect_dma_start(
                                out=xsd.ap[:, :], out_offset=bass.IndirectOffsetOnAxis(ap=DSTi.ap[:, t, k_:k_ + 1], axis=0),
                                in_=cb.ap[:, :], in_offset=None)])(),
                                cb, [cb, DSTi], [xsd], q="pool")

                with S.scope():
                    wblk = S.ring("wblkE", [128, 8, 512], BF16, 4, dma=True)
                    wdblk = S.ring("wdblkE", [128, 4, 512], BF16, 4, dma=True)
                    xtr = S.ring("xtE", [128, 4, 1024], BF16, 2, dma=True)
                    cTr = S.ring("cTE", [128, 8, 512], BF16, 2)
                    hT = S.sbuf("hTE", [128, 28, 512], BF16)
                    sgr = S.ring("sgE", [128, 512], F32, 3)
                    yor = S.ring("yoE", [128, 1024], F32, 8, dma=True)

                    def gather(dst_ap, srcbuf, idx_ap, semb, nrows):
                        S.dma(lambda e: [e.indirect_dma_start(
                            out=dst_ap, out_offset=None, in_=srcbuf.ap[:, :],
                            in_offset=bass.IndirectOffsetOnAxis(ap=idx_ap, axis=0))], semb, [WSRC, IGi, IDi], [semb], q="pool")

                    for i in range(NTILE):
                        xt = xtr.next()
                        LD(xt.ap[:], xsd.ap[i * 512:(i + 1) * 512, :].rearrange("(j p) d -> p j d", p=128), xt, [xsd], [xt])
                        cT = cTr.next()
                        for j in range(4):
                            pb = S.pbank()
                            pv = bview(pb).rearrange("p (k n) -> p k n", k=8)
                            TRN([(pv[:, k, :], xt.ap[:, j, k * 128:(k + 1) * 128]) for k in range(8)], [xt], [pb])
                            CP("act" if j % 2 == 0 else "dve", cT.ap[:, :, j * 128:(j + 1) * 128], pv, [pb], [cT])
                        for fb in range(7):
                            wg = wblk.next()
                            gather(wg.ap[:].rearrange("p k c -> p (k c)"), Wb["moe_w_gate"], IGi.ap[:, i, fb:fb + 1], wg, 8 * 7 * 128)
                            wu = wblk.next()
                            gather(wu.ap[:].rearrange("p k c -> p (k c)"), Wb["moe_w_up"], IGi.ap[:, i, fb:fb + 1], wu, 8 * 7 * 128)
                            for ft in range(4):
                                f = fb * 4 + ft
                                pg = S.pbank()
                                MM([(pg.ap[:], wg.ap[:, k, ft * 128:(ft + 1) * 128], cT.ap[:, k, :], k == 0, k == 7)
                                    for k in range(8)], [wg, cT], [pg])
                                pu = S.pbank()
                                MM([(pu.ap[:], wu.ap[:, k, ft * 128:(ft + 1) * 128], cT.ap[:, k, :], k == 0, k == 7)
                                    for k in range(8)], [wu, cT], [pu])
                                sg = sgr.next()
                                ACT(sg.ap[:], pg.ap[:], AF.Silu, [pg], [sg])
                                TT("dve", hT.ap[:, f, :], pu.ap[:], sg.ap[:], ALU.mult, [pu, sg], [hT])
                        yos = [yor.next() for _ in range(4)]
                        for half in range(2):
                            accs = [S.pbank() for _ in range(4)]
                            for fb in range(7):
                                wd = wdblk.next()
                                gather(wd.ap[:].rearrange("p k c -> p (k c)"), Wb["moe_w_down"],
                                       IDi.ap[:, i, half * 7 + fb:half * 7 + fb + 1], wd, 8 * 2 * 7 * 128)
                                items = []
                                for j in range(4):
                                    for ft in range(4):
                                        f = fb * 4 + ft
                                        items.append((accs[j].ap[:], hT.ap[:, f, j * 128:(j + 1) * 128], wd.ap[:, ft, :],
                                                      f == 0, f == 27))
                                MM(items, [hT, wd], accs)
                            for j in range(4):
                                CP("act" if j % 2 == 0 else "dve", yos[j].ap[:, half * 512:(half + 1) * 512], accs[j].ap[:],
                                   [accs[j]], [yos[j]])
                        for j in range(4):
                            r0 = i * 512 + j * 128
                            LD(ysd.ap[r0:r0 + 128, :], yos[j].ap[:], yos[j], [yos[j]], [ysd], q="sp")

                with S.scope():
                    prm = S.sbuf("prmP", [128, 1], F32, dma=True)
                    gpl = S.sbuf("gplP", [128, 1024], F32)
                    LDS([(gpl.ap[:], I["ple_norm_g"][L].partition_broadcast(128))], prm, [], [gpl])
                    wpp = S.sbuf("wppP", [128, 2, 1024], BF16, dma=True)
                    LD(wpp.ap[:], Wb["w_ple_proj"].ap[L].rearrange("(k p) n -> p k n", p=128), wpp, [Wb["w_ple_proj"]], [wpp])
                    wgs = []
                    wgr = S.ring("wgP", [128, 8, 512], BF16, 2, dma=True)
                    for half in range(2):
                        wo = wgr.next()
                        LD(wo.ap[:], Wb["w_ple_gate"].ap[L].rearrange("(k p) n -> p k n", p=128)[:, :, half * 512:(half + 1) * 512],
                           wo, [WSRC], [wo])
                        wgs.append(wo)
                    hw = S.ring("hwP", [128, 1024], F32, 8, dma=True)
                    y12 = S.ring("y12P", [128, 1024], F32, 6, dma=True)
                    junk = S.ring("junkP", [128, 1024], BF16, 2)
                    ssr = S.ring("ssP", [128, 4], F32, 3)
                    cbf = S.ring("cbfP", [128, 1024], BF16, 2)
                    cTr = S.ring("cTP", [128, 8, 512], BF16, 2)
                    pin = S.ring("pinP", [128, 256], F32, 4, dma=True)
                    pbf = S.ring("pbfP", [128, 256], BF16, 2)
                    pTr = S.ring("pTP", [128, 2, 512], BF16, 2)
                    gsb = S.ring("gsbP", [128, 512], F32, 3)
                    for g in range(NG):
                        t0g = g * 512
                        hws = []
                        cT = cTr.next()
                        pTt = pTr.next()
                        ss = ssr.next()
                        for j in range(4):
                            t = g * 4 + j
                            r0 = t * 128
                            hwj = hw.next()
                            hws.append(hwj)
                            LD(hwj.ap[:], hmid.ap[r0:r0 + 128, :], hwj, [hmid], [hwj])
                            for k_ in range(2):
                                yk = y12.next()
                                S.dma((lambda yk=yk, t=t, k_=k_: lambda e: [e.indirect_dma_start(
                                    out=yk.ap[:, :], out_offset=None, in_=ysd.ap[:, :],
                                    in_offset=bass.IndirectOffsetOnAxis(ap=DSTi.ap[:, t, k_:k_ + 1], axis=0))])(), yk, [ysd, DSTi], [yk], q="pool")
                                STT("dve", hwj.ap[:], yk.ap[:], GT.ap[:, t, k_:k_ + 1], hwj.ap[:], ALU.mult, ALU.add,
                                    [yk, GT, hwj], [hwj])
                            jk = junk.next()
                            ACT(jk.ap[:], hwj.ap[:], AF.Square, [hwj], [jk, ss], accum_out=ss.ap[:, j:j + 1])
                        rsqrt_act(ss.ap[:], ss.ap[:], 1.0 / 1024, [ss], [ss])
                        for j in range(4):
                            cb = cbf.next()
                            STT("dve", cb.ap[:], hws[j].ap[:], ss.ap[:, j:j + 1], gpl.ap[:], ALU.mult, ALU.mult,
                                [hws[j], ss, gpl], [cb])
                            pb = S.pbank()
                            pv = bview(pb).rearrange("p (k n) -> p k n", k=8)
                            TRN([(pv[:, k, :], cb.ap[:, k * 128:(k + 1) * 128]) for k in range(8)], [cb], [pb])
                            CP("act" if j % 2 == 0 else "dve", cT.ap[:, :, j * 128:(j + 1) * 128], pv, [pb], [cT])
                            pi_ = pin.next()
                            LD(pi_.ap[:], I["p"][L][t0g + j * 128:t0g + (j + 1) * 128, :], pi_, [], [pi_])
                            pb_ = pbf.next()
                            CP("pool", pb_.ap[:], pi_.ap[:], [pi_], [pb_])
                            pk_ = S.pbank()
                            pv2 = bview(pk_).rearrange("p (k n) -> p k n", k=8)
                            TRN([(pv2[:, k, :], pb_.ap[:, k * 128:(k + 1) * 128]) for k in range(2)], [pb_], [pk_])
                            CP("act", pTt.ap[:, :, j * 128:(j + 1) * 128], pv2[:, 0:2, :], [pk_], [pTt])
                        for j in range(4):
                            for half in range(2):
                                pg = S.pbank()
                                MM([(pg.ap[:], cT.ap[:, k, j * 128:(j + 1) * 128], wgs[half].ap[:, k, :], k == 0, k == 7)
                                    for k in range(8)], [cT, wgs[half]], [pg])
                                pp2 = S.pbank()
                                MM([(pp2.ap[:], pTt.ap[:, k, j * 128:(j + 1) * 128], wpp.ap[:, k, half * 512:(half + 1) * 512],
                                     k == 0, k == 1) for k in range(2)], [pTt, wpp], [pp2])
                                gs = gsb.next()
                                ACT(gs.ap[:], pg.ap[:], AF.Sigmoid, [pg], [gs])
                                TT("dve", gs.ap[:], pp2.ap[:], gs.ap[:], ALU.mult, [pp2, gs], [gs])
                                hs = hws[j].ap[:, half * 512:(half + 1) * 512]
                                TT("dve", hs, hs, gs.ap[:], ALU.add, [gs, hws[j]], [hws[j]])
                            LD(hdst.ap[t0g + j * 128:t0g + (j + 1) * 128, :], hws[j].ap[:], hws[j], [hws[j]], [hdst], q="sp")

        conv_list = []
        conv_moe = []
        for L in layers:
            conv_list += [("w_in", (L,)), ("w_out", (L,)), ("w_ple_gate", (L,)), ("w_ple_proj", (L,))]
            if L % 2 == 0:
                conv_list += [("dense_w_gate", (0,)), ("dense_w_up", (0,)), ("dense_w_down", (0,))]
            else:
                for ex_ in range(nexp):
                    conv_moe += [("moe_w_gate", (0, ex_)), ("moe_w_up", (0, ex_)), ("moe_w_down", (0, ex_))]
        bg_ok = (len(layers) == 2 and "C" not in skip and "V" not in skip)
        if not bg_ok:
            conv_list += conv_moe
        if 'V' not in skip:
            convert(conv_list)
        hcur = IN["x"]
        for li, L in enumerate(layers):
            hnext = OUT if li == len(layers) - 1 else h1
            if "A" not in skip:
                phase_A(L, hcur)
            if "B" not in skip:
                phase_B(L)
            if "C" not in skip:
                if bg_ok and li == 0:
                    with S.scope():
                        n_chunks = nexp * (8 + 8 + 28)
                        n_units = 4 * sum(4 * q + 4 for q in range(NG))
                        gen = convert_gen(conv_moe, convert_bufs(), engs=("dve", "pool"))
                        phase_C(L, bg=gen, bg_every=max(1, n_units // (n_chunks + 8)))
                        for _ in gen:
                            pass
                else:
                    phase_C(L)
            if "yT" in taps and li == len(layers) - 1:
                break
            if "D" not in skip:
                if sparse and L % 2 == 1:
                    phase_D_moe(L, hcur, hnext)
                else:
                    phase_D(L, hcur, hnext)
            hcur = hnext
        if "yT" in taps:
            tp = tap_aps["yT"]
            with S.scope():
                tb = S.sbuf("tapb", [128, 8, S_tok], BF16, dma=True)
                LD(tb.ap[:], yT.ap.rearrange("(k p) s -> p k s", p=128), tb, [yT], [tb])
                LD(tp.rearrange("(k p) s -> p k s", p=128), tb.ap[:], tb, [tb], [OUT])
        S.barrier()
        S.emit(block)
    return nc


_CACHE = {}


def kernel(**inputs):
    x = np.asarray(inputs["x"], dtype=np.float32)
    B, S_tok, _ = x.shape
    p = np.asarray(inputs["p"], dtype=np.float32)
    key = S_tok
    if key not in _CACHE:
        _CACHE[key] = build(S_tok)
    nc = _CACHE[key]
    shared = {name: np.ascontiguousarray(np.asarray(inputs[name], dtype=np.float32)) for name, _ in PARAMS}
    ncores = 8
    active = [0, 1, 4, 5][:B] if B <= 4 else list(range(B))
    zeros = None
    in_maps = []
    for c in range(ncores):
        if c in active:
            b = active.index(c)
            m = dict(shared)
            m["x"] = np.ascontiguousarray(x[b])
            m["p"] = np.ascontiguousarray(p[:, b])
        else:
            if zeros is None:
                zeros = {name: np.zeros(shp, np.float32) for name, shp in PARAMS}
                zeros["x"] = np.zeros((S_tok, D), np.float32)
                zeros["p"] = np.zeros((2, S_tok, 256), np.float32)
            m = zeros
        in_maps.append(m)
    res = run_bass_kernel_spmd(nc, in_maps, core_ids=list(range(ncores)))
    out = np.stack([np.asarray(res.results[active[b]]["out"], dtype=np.float32) for b in range(B)], axis=0)
    return out.astype(np.float32)
```

```python
import contextlib
import math
import numpy as np
import concourse.bass as bass
import concourse.mybir as mybir
from concourse.bass_utils import run_bass_kernel_spmd

F32 = mybir.dt.float32
BF16 = mybir.dt.bfloat16
I32 = mybir.dt.int32
AF = mybir.ActivationFunctionType
ALU = mybir.AluOpType
AX = mybir.AxisListType

ENGS = ("pe", "act", "dve", "pool", "sp")
D = 1024
DIN = 3080
FFD = 2816
FFE = 3584
NEXP = 8
EPS = 1e-6
NDSEM = 56
NPSEM = 16


class Buf:
    def __init__(self, name, ap=None, dsem=None):
        self.name = name
        self.ap = ap
        self.w = None
        self.r = {}
        self.dsem = dsem


class Ring:
    def __init__(self, bufs):
        self.bufs = bufs
        self.i = -1

    def next(self):
        self.i = (self.i + 1) % len(self.bufs)
        return self.bufs[self.i]


class Sched:
    def __init__(self, nc, stack):
        self.nc = nc
        self.stack = stack
        self.stacks = [stack]
        self.sems = {}
        self.cnt = {}
        self.prog = {e: [] for e in ENGS}
        self.seen = {e: {} for e in ENGS}
        for e in ENGS:
            self.newsem("E_" + e)
        self.free_ds = [self.newsem("D_%d" % i) for i in range(NDSEM)]
        self.pool_sems = [self.newsem("P_%d" % i) for i in range(NPSEM)]
        self.pool_i = 0
        self.scope_ds = [[]]
        self.banks = []
        self.bank_i = -1
        self.reserved = set()

    def newsem(self, key):
        h = self.stack.enter_context(self.nc.semaphore(key))
        self.sems[key] = h
        self.cnt[key] = 0
        return key

    def sbuf(self, name, shape, dtype, dma=False):
        self.uid = getattr(self, "uid", 0) + 1
        name = "%s_u%d" % (name, self.uid)
        t = self.stacks[-1].enter_context(self.nc.sbuf_tensor(name, list(shape), dtype))
        b = Buf(name, t)
        if dma:
            b.dsem = self.free_ds.pop()
            self.scope_ds[-1].append(b.dsem)
        return b

    def ring(self, name, shape, dtype, n, dma=False):
        return Ring([self.sbuf("%s_%d" % (name, i), shape, dtype, dma) for i in range(n)])

    def dram(self, name, shape, dtype):
        t = self.nc.dram_tensor(name, list(shape), dtype, kind="Internal")
        return Buf(name, t.ap())

    def make_banks(self):
        for i in range(8):
            t = self.stack.enter_context(self.nc.psum_tensor("bank%d" % i, [128, 512], F32))
            self.banks.append(Buf("bank%d" % i, t))

    def pbank(self):
        for _ in range(8):
            self.bank_i = (self.bank_i + 1) % 8
            if self.bank_i not in self.reserved:
                return self.banks[self.bank_i]
        raise RuntimeError("no psum bank")

    def reserve(self, n):
        out = []
        for i in range(8):
            if i not in self.reserved and len(out) < n:
                self.reserved.add(i)
                out.append(self.banks[i])
        return out

    def release(self, banks):
        for b in banks:
            self.reserved.discard(self.banks.index(b))

    @contextlib.contextmanager
    def scope(self):
        st = contextlib.ExitStack()
        self.stacks.append(st)
        self.scope_ds.append([])
        try:
            yield
        finally:
            self.barrier()
            self.free_ds.extend(self.scope_ds.pop())
            self.stacks.pop()
            st.close()

    def _waits(self, eng, reads, writes):
        need = {}

        def add(tok):
            if tok is None:
                return
            k, v = tok
            if eng == "pe" and k == "E_pe":
                return
            if v > need.get(k, 0):
                need[k] = v

        for b in reads:
            add(b.w)
        for b in writes:
            add(b.w)
            for k, v in b.r.items():
                add((k, v))
        out = []
        for k, v in need.items():
            if k.startswith("D_"):
                v = self.cnt[k]
            if self.seen[eng].get(k, 0) >= v:
                continue
            self.seen[eng][k] = v
            out.append((k, v))
        return out

    def _commit(self, tok, reads, writes):
        k, v = tok
        for b in reads:
            if v > b.r.get(k, 0):
                b.r[k] = v
        for b in writes:
            b.w = tok
            b.r = {}

    def op(self, eng, fn, reads=(), writes=()):
        waits = self._waits(eng, reads, writes)
        k = "E_" + eng
        self.cnt[k] += 1
        tok = (k, self.cnt[k])
        self.prog[eng].append((waits, fn, k, 1, 1))
        self._commit(tok, reads, writes)
        return tok

    def raw(self, eng, fn, reads=()):
        waits = self._waits(eng, reads, ())
        self.prog[eng].append((waits, fn, None, -1, 0))

    def dma(self, fn, semb, reads=(), writes=(), n=1, q="sp"):
        waits = self._waits(q, reads, writes)
        if q == "pool":
            assert n == 1
            k = self.pool_sems[self.pool_i % NPSEM]
            self.pool_i += 1
            v0 = self.cnt[k]
            if v0 > 0 and self.seen[q].get(k, 0) < v0:
                self.seen[q][k] = v0
                waits = waits + [(k, v0)]
        else:
            k = semb.dsem
        self.cnt[k] += 16 * n
        tok = (k, self.cnt[k])
        self.prog[q].append((waits, fn, k, 16, n))
        self._commit(tok, reads, writes)
        return tok

    def barrier(self):
        for e in ENGS:
            out = []
            for k, v in self.cnt.items():
                if v == 0 or (e == "pe" and k == "E_pe"):
                    continue
                if self.seen[e].get(k, 0) >= v:
                    continue
                self.seen[e][k] = v
                out.append((k, v))
            if out:
                self.prog[e].append((out, None, None, 0, 0))

    def emit(self, block):
        def run(e):
            def body(engine):
                for waits, fn, k, inc, n in self.prog[e]:
                    for wk, wv in waits:
                        engine.wait_ge(self.sems[wk], wv)
                    if fn is None:
                        continue
                    r = fn(engine)
                    if inc == -1:
                        continue
                    if inc == 16:
                        assert len(r) == n, (len(r), n)
                        for ins in r:
                            ins.then_inc(self.sems[k], 16)
                    else:
                        r.then_inc(self.sems[k], 1)
            return body
        block.tensor(run("pe"))
        block.scalar(run("act"))
        block.vector(run("dve"))
        block.gpsimd(run("pool"))
        block.sync(run("sp"))


PARAMS = [
    ("mix_norm_g", (2, 1024)), ("w_in", (2, 1024, 3080)), ("b_igate", (2, 4)), ("b_fgate", (2, 4)),
    ("m_qk_conv_w", (2, 4, 512)), ("m_out_norm_g", (2, 256)), ("c_conv_w", (2, 31, 256)),
    ("c_conv_b", (2, 256)), ("c_ln_g", (2, 256)), ("c_ln_b", (2, 256)), ("a_q_norm_g", (2, 64)),
    ("a_k_norm_g", (2, 64)), ("a_lambda_q1", (2, 64)), ("a_lambda_k1", (2, 64)), ("a_lambda_q2", (2, 64)),
    ("a_lambda_k2", (2, 64)), ("a_subln_g", (2, 128)), ("w_out", (2, 1024, 1024)), ("ffn_norm_g", (2, 1024)),
    ("dense_w_gate", (1, 1024, 2816)), ("dense_w_up", (1, 1024, 2816)), ("dense_w_down", (1, 2816, 1024)),
    ("router_w", (1, 1024, 8)), ("moe_w_gate", (1, 8, 1024, 3584)), ("moe_w_up", (1, 8, 1024, 3584)),
    ("moe_w_down", (1, 8, 3584, 1024)), ("ple_norm_g", (2, 1024)), ("w_ple_gate", (2, 1024, 1024)),
    ("w_ple_proj", (2, 256, 1024)),
]


def build(S_tok, layers=(0, 1), taps=(), nexp=NEXP, skip=(), sparse=True):
    NT = S_tok // 128
    NG = S_tok // 512
    NCH = S_tok // 64
    nc = bass.Bass("TRN2", target_bir_lowering=False)
    I = {}
    I["x"] = nc.dram_tensor("x", [S_tok, D], F32, kind="ExternalInput").ap()
    I["p"] = nc.dram_tensor("p", [2, S_tok, 256], F32, kind="ExternalInput").ap()
    for name, shp in PARAMS:
        I[name] = nc.dram_tensor(name, list(shp), F32, kind="ExternalInput").ap()
    out_ap = nc.dram_tensor("out", [S_tok, D], F32, kind="ExternalOutput").ap()
    tap_aps = {}
    if "yT" in taps:
        tap_aps["yT"] = nc.dram_tensor("tap_yT", [1024, S_tok], BF16, kind="ExternalOutput").ap()

    with contextlib.ExitStack() as st:
        S = Sched(nc, st)
        S.make_banks()
        IN = {k: Buf("in_" + k, v) for k, v in I.items()}
        OUT = Buf("out", out_ap)

        def ACT(out, in_, func, R, W, **kw):
            S.op("act", lambda e: e.activation(out=out, in_=in_, func=func, **kw), R, W)

        def TT(eng, out, a, b, op, R, W):
            S.op(eng, lambda e: e.tensor_tensor(out=out, in0=a, in1=b, op=op), R, W)

        def TS(eng, out, a, s1, s2, op0, op1, R, W):
            if s2 is None:
                S.op(eng, lambda e: e.tensor_scalar(out=out, in0=a, scalar1=s1, scalar2=None, op0=op0), R, W)
            else:
                S.op(eng, lambda e: e.tensor_scalar(out=out, in0=a, scalar1=s1, scalar2=s2, op0=op0, op1=op1), R, W)

        def STT(eng, out, a, s, b, op0, op1, R, W):
            S.op(eng, lambda e: e.scalar_tensor_tensor(out=out, in0=a, scalar=s, in1=b, op0=op0, op1=op1), R, W)

        def CP(eng, out, in_, R, W):
            if eng == "act":
                S.op("act", lambda e: e.copy(out=out, in_=in_), R, W)
            else:
                S.op(eng, lambda e: e.tensor_copy(out=out, in_=in_), R, W)

        def MSET(eng, ap, val, W):
            S.op(eng, lambda e: e.memset(ap, val), (), W)

        def MM(items, R, W):
            def f(e):
                r = None
                for it in items:
                    (o, l, rh, s0, s1) = it[:5]
                    if len(it) > 5:
                        r = e.matmul(o, lhsT=l, rhs=rh, start=s0, stop=s1, skip_group_check=True)
                    else:
                        r = e.matmul(o, lhsT=l, rhs=rh, start=s0, stop=s1)
                return r
            S.op("pe", f, R, W)

        def TRN(items, R, W):
            def f(e):
                r = None
                for (o, i_) in items:
                    r = e.transpose(out=o, in_=i_, identity=ident.ap[0:i_.shape[0], 0:i_.shape[0]])
                return r
            S.op("pe", f, list(R) + [ident], W)

        def LD(out, in_, semb, R, W, q="sp"):
            S.dma(lambda e: [e.dma_start(out=out, in_=in_)], semb, R, W, 1, q)

        def LDS(pairs, semb, R, W, q="sp", slow=False):
            def f(e):
                if slow:
                    return [e.dma_start(out=o, in_=i_, allow_slow_non_contiguous=True) for (o, i_) in pairs]
                return [e.dma_start(out=o, in_=i_) for (o, i_) in pairs]
            S.dma(f, semb, R, W, len(pairs), q)

        def bview(bank):
            return bank.ap[:].bitcast(BF16)

        def rsqrt_act(out, in_, scale, R, W):
            ACT(out, in_, AF.Ln, R, W, scale=scale, bias=epsb.ap[0:in_.shape[0], 0:1])
            ACT(out, out, AF.Exp, W, W, scale=-0.5)

        ident = S.sbuf("ident", [128, 128], BF16)
        identf = S.sbuf("identf", [128, 128], F32)
        triT = S.sbuf("triT", [128, 128], F32)
        triTb = S.sbuf("triTb", [128, 128], BF16)
        triU = S.sbuf("triU", [128, 128], BF16)
        selA = S.sbuf("selA", [128, 128], F32)
        selB = S.sbuf("selB", [128, 128], F32)
        onesln = S.sbuf("onesln", [128, 128], F32)
        epsb = S.sbuf("epsb", [128, 1], F32)
        oneb = S.sbuf("oneb", [128, 1], F32)
        ln8b = S.sbuf("ln8b", [128, 1], F32)
        kcol_i = S.sbuf("kcol_i", [128, 1], I32)
        kcol = S.sbuf("kcol", [128, 1], F32)
        NDD = NT + 4
        abt = S.sbuf("abt", [128, 4, NDD], F32)
        A_tm = S.sbuf("A_tm", [128, NT, 4], F32)
        Gb = S.sbuf("Gb", [128, NCH, 4], F32)

        def mk_consts(e):
            e.memset(identf.ap[:], 1.0)
            e.affine_select(out=identf.ap[:], in_=identf.ap[:], compare_op=ALU.is_ge, fill=0.0,
                            base=0, pattern=[[-1, 128]], channel_multiplier=1)
            e.affine_select(out=identf.ap[:], in_=identf.ap[:], compare_op=ALU.is_ge, fill=0.0,
                            base=0, pattern=[[1, 128]], channel_multiplier=-1)
            e.memset(triT.ap[:], 1.0)
            e.affine_select(out=triT.ap[:], in_=triT.ap[:], compare_op=ALU.is_ge, fill=0.0,
                            base=0, pattern=[[1, 128]], channel_multiplier=-1)
            e.memset(triT.ap[0:64, 64:128], 0.0)
            e.memset(selA.ap[:], 0.0)
            e.memset(selA.ap[0:64, :], 1.0)
            e.memset(selB.ap[:], 0.0)
            e.memset(selB.ap[64:128, :], 1.0)
            e.memset(onesln.ap[:], 1.0 / 256.0)
            e.memset(epsb.ap[:], EPS)
            e.memset(oneb.ap[:], 1.0)
            e.memset(ln8b.ap[:], -math.log(8.0))
            return e.iota(kcol_i.ap[:], pattern=[[0, 1]], base=0, channel_multiplier=1)
        S.op("pool", mk_consts, (), [identf, triT, selA, selB, onesln, epsb, oneb, ln8b, kcol_i])
        CP("pool", ident.ap[:], identf.ap[:], [identf], [ident])
        CP("pool", triTb.ap[:], triT.ap[:], [triT], [triTb])
        CP("pool", kcol.ap[:], kcol_i.ap[:], [kcol_i], [kcol])

        def mk_triU(e):
            e.memset(triU.ap[:], 1.0)
            return e.affine_select(out=triU.ap[:], in_=triU.ap[:], compare_op=ALU.is_ge, fill=0.0,
                                   base=0, pattern=[[1, 128]], channel_multiplier=-1)
        S.op("pool", mk_triU, (), [triU])
        for h in range(4):
            slope = 2.0 ** (-8.0 * (h + 1) / 4)
            for di in range(NDD):
                dd = di - NT
                TS("pool", abt.ap[:, h, di:di + 1], kcol.ap[:], slope, slope * (128.0 * dd - 256.0),
                   ALU.mult, ALU.add, [kcol], [abt])

        Wb = {}
        Wb["w_in"] = S.dram("wb_in", [2, 1024, DIN], BF16)
        Wb["w_out"] = S.dram("wb_out", [2, 1024, 1024], BF16)
        Wb["dense_w_gate"] = S.dram("wb_dg", [1, 1024, FFD], BF16)
        Wb["dense_w_up"] = S.dram("wb_du", [1, 1024, FFD], BF16)
        Wb["dense_w_down"] = S.dram("wb_dd", [1, FFD, 1024], BF16)
        if sparse:
            Wb["moe_w_gate"] = S.dram("wb_mg", [8 * 7 * 128, 8 * 512], BF16)
            Wb["moe_w_up"] = S.dram("wb_mu", [8 * 7 * 128, 8 * 512], BF16)
            Wb["moe_w_down"] = S.dram("wb_md", [8 * 2 * 7 * 128, 4 * 512], BF16)
        else:
            Wb["moe_w_gate"] = S.dram("wb_mg", [1, 8, 1024, FFE], BF16)
            Wb["moe_w_up"] = S.dram("wb_mu", [1, 8, 1024, FFE], BF16)
            Wb["moe_w_down"] = S.dram("wb_md", [1, 8, FFE, 1024], BF16)
        Wb["w_ple_gate"] = S.dram("wb_pg", [2, 1024, 1024], BF16)
        Wb["w_ple_proj"] = S.dram("wb_pp", [2, 256, 1024], BF16)
        h1 = S.dram("h1", [S_tok, D], F32)
        mqT = S.dram("mqT", [256, S_tok], BF16)
        mkT = S.dram("mkT", [256, S_tok], BF16)
        mktm = S.dram("mktm", [S_tok, 256], BF16)
        mrv = S.dram("mrv", [S_tok, 4, 65], BF16)
        mso = S.dram("mso", [S_tok, 256], BF16)
        aqT = S.dram("aqT", [512, S_tok], BF16)
        akT = S.dram("akT", [512, S_tok], BF16)
        avd = S.dram("avd", [S_tok, 512], BF16)
        yT = S.dram("yT", [1024, S_tok], BF16)

        block = st.enter_context(nc.Block())

        def convert_bufs():
            return (S.ring("cvf", [128, 3584], F32, 3, dma=True), S.ring("cvb", [128, 3584], BF16, 3, dma=True))

        def convert_gen(names_layers, bufs, engs=("act", "dve", "pool")):
            CB = 3584
            fr, br = bufs
            ei = 0
            for (name, idx) in names_layers:
                src = IN[name].ap
                dst = Wb[name].ap
                for ix in idx:
                    src = src[ix]
                    if not (sparse and name.startswith("moe_")):
                        dst = dst[ix]
                R_, C_ = src.shape
                for r0 in range(0, R_, 128):
                    for c0 in range(0, C_, CB):
                        cw = min(CB, C_ - c0)
                        fb = fr.next()
                        bb = br.next()
                        LD(fb.ap[:, 0:cw], src[r0:r0 + 128, c0:c0 + cw], fb, [IN[name]], [fb])
                        eng = engs[ei % len(engs)]
                        ei += 1
                        CP(eng, bb.ap[:, 0:cw], fb.ap[:, 0:cw], [fb], [bb])
                        if sparse and name in ("moe_w_gate", "moe_w_up"):
                            dv = Wb[name].ap.rearrange("(e fb p) (k c) -> e k p fb c", e=8, fb=7, p=128, k=8)[idx[1]][r0 // 128]
                            LD(dv, bb.ap[:, 0:3584].rearrange("p (fb c) -> p fb c", fb=7), bb, [bb], [Wb[name]], q="pool")
                        elif sparse and name == "moe_w_down":
                            f_ = r0 // 128
                            dv = Wb[name].ap.rearrange("(e h fb p) (ft c) -> e fb ft p h c", e=8, h=2, fb=7, p=128, ft=4)[idx[1]][f_ // 4][f_ % 4]
                            LD(dv, bb.ap[:, 0:1024].rearrange("p (h c) -> p h c", h=2), bb, [bb], [Wb[name]], q="pool")
                        else:
                            LD(dst[r0:r0 + 128, c0:c0 + cw], bb.ap[:, 0:cw], bb, [bb], [Wb[name]], q="pool")
                        yield

        def convert(names_layers):
            with S.scope():
                for _ in convert_gen(names_layers, convert_bufs()):
                    pass

        def phase_A(L, hsrc):
            with S.scope():
                win = S.sbuf("win", [128, 8, DIN], BF16, dma=True)
                wv = Wb["w_in"].ap[L].rearrange("(k p) n -> p k n", p=128)
                LDS([(win.ap[:, :, c:c + 770], wv[:, :, c:c + 770]) for c in range(0, DIN, 770)],
                    win, [Wb["w_in"]], [win])
                prm = S.sbuf("prmA", [128, 1], F32, dma=True)
                gbc = S.sbuf("gbcA", [128, 1024], F32)
                bif = S.sbuf("bif", [128, 8], F32)
                wq4 = S.sbuf("wq4", [128, 4, 4], F32)
                wc31 = S.sbuf("wc31", [128, 2, 31], F32)
                cvec = S.sbuf("cvec", [128, 3, 2], F32)
                gq = S.sbuf("gq", [128, 64], F32)
                gk = S.sbuf("gk", [128, 64], F32)
                LDS([(gbc.ap[:], I["mix_norm_g"][L].partition_broadcast(128)),
                     (bif.ap[:, 0:4], I["b_igate"][L].partition_broadcast(128)),
                     (bif.ap[:, 4:8], I["b_fgate"][L].partition_broadcast(128)),
                     (gq.ap[:], I["a_q_norm_g"][L].partition_broadcast(128)),
                     (gk.ap[:], I["a_k_norm_g"][L].partition_broadcast(128))],
                    prm, [], [gbc, bif, gq, gk])
                LDS([(wq4.ap[:, t, :], I["m_qk_conv_w"][L][:, t * 128:(t + 1) * 128].rearrange("j p -> p j"))
                     for t in range(4)] +
                    [(wc31.ap[:, t, :], I["c_conv_w"][L][:, t * 128:(t + 1) * 128].rearrange("j p -> p j"))
                     for t in range(2)] +
                    [(cvec.ap[:, 0, :], I["c_conv_b"][L].rearrange("(t p) -> p t", p=128)),
                     (cvec.ap[:, 1, :], I["c_ln_g"][L].rearrange("(t p) -> p t", p=128)),
                     (cvec.ap[:, 2, :], I["c_ln_b"][L].rearrange("(t p) -> p t", p=128))],
                    prm, [], [wq4, wc31, cvec], slow=True)
                dq = S.sbuf("dq", [128, 16, 128], BF16)
                dc = S.sbuf("dc", [128, 62, 128], BF16)
                for c in range(4):
                    for j in range(4):
                        TS("pool", dq.ap[:, c * 4 + j, :], identf.ap[:], wq4.ap[:, c, j:j + 1], None, ALU.mult, None,
                           [identf, wq4], [dq])
                for c in range(2):
                    for j in range(31):
                        TS("pool", dc.ap[:, c * 31 + j, :], identf.ap[:], wc31.ap[:, c, j:j + 1], None, ALU.mult, None,
                           [identf, wc31], [dc])

                hin = S.ring("hinA", [128, 1024], F32, 6, dma=True)
                junk = S.ring("junkA", [128, 1024], BF16, 2)
                ssr = S.ring("ssA", [128, 4], F32, 2)
                abf = S.ring("abfA", [128, 1024], BF16, 2)
                aTr = S.ring("aTA", [128, 8, 512], BF16, 2)
                xqk = S.sbuf("xqk", [128, 4, 515], BF16)
                zbuf = S.sbuf("zbuf", [128, 2, 542], BF16)
                qko = S.ring("qko", [128, 512], BF16, 3, dma=True)
                ktmo = S.ring("ktmo", [128, 4, 256], BF16, 2, dma=True)
                sgr = S.ring("sgr", [128, 512], F32, 2)
                zc = S.sbuf("zc", [128, 2, 512], F32)
                zc2 = S.sbuf("zc2", [128, 2, 512], F32)
                mean_sb = S.sbuf("mean_sb", [128, 512], F32)
                var_sb = S.sbuf("var_sb", [128, 512], F32)
                dtmp = S.ring("dtmp", [128, 512], F32, 2)
                yco = S.ring("yco", [128, 512], BF16, 2, dma=True)
                gat = S.ring("gat", [128, 16], F32, 3)
                rvo = S.ring("rvo", [128, 4, 65], BF16, 2, dma=True)
                soo = S.ring("soo", [128, 256], BF16, 2, dma=True)
                sqj = S.ring("sqj", [128, 512], F32, 2)
                ssq = S.ring("ssq", [128, 16], F32, 3)
                qn1 = S.ring("qn1", [128, 512], F32, 2)
                qnb = S.ring("qnb", [128, 512], BF16, 4)
                qTo = S.ring("qTo", [128, 4, 512], BF16, 2, dma=True)
                kTo = S.ring("kTo", [128, 4, 512], BF16, 2, dma=True)
                vo = S.ring("vo", [128, 512], BF16, 2, dma=True)

                MSET("pool", xqk.ap[:, :, 0:3], 0.0, [xqk])
                MSET("pool", zbuf.ap[:, :, 0:30], 0.0, [zbuf])

                for g in range(NG):
                    t0g = g * 512
                    aT = aTr.next()
                    ss = ssr.next()
                    hbs = []
                    for j in range(4):
                        hb = hin.next()
                        hbs.append(hb)
                        r0 = t0g + j * 128
                        LD(hb.ap[:], hsrc.ap[r0:r0 + 128, :], hb, [hsrc], [hb])
                        jk = junk.next()
                        ACT(jk.ap[:], hb.ap[:], AF.Square, [hb], [jk, ss], accum_out=ss.ap[:, j:j + 1])
                    rsqrt_act(ss.ap[:], ss.ap[:], 1.0 / 1024, [ss], [ss])
                    for j in range(4):
                        ab = abf.next()
                        STT("dve", ab.ap[:], hbs[j].ap[:], ss.ap[:, j:j + 1], gbc.ap[:], ALU.mult, ALU.mult,
                            [hbs[j], ss, gbc], [ab])
                        pb = S.pbank()
                        pv = bview(pb).rearrange("p (k n) -> p k n", k=8)
                        TRN([(pv[:, k, :], ab.ap[:, k * 128:(k + 1) * 128]) for k in range(8)], [ab], [pb])
                        CP("act" if j % 2 == 0 else "dve", aT.ap[:, :, j * 128:(j + 1) * 128], pv, [pb], [aT])

                    def fm(col0):
                        pb = S.pbank()
                        MM([(pb.ap[:], win.ap[:, k, col0:col0 + 128], aT.ap[:, k, :], k == 0, k == 7)
                            for k in range(8)], [win, aT], [pb])
                        return pb

                    def tm(j, col0, n):
                        pb = S.pbank()
                        MM([(pb.ap[:, 0:n], aT.ap[:, k, j * 128:(j + 1) * 128], win.ap[:, k, col0:col0 + n],
                             k == 0, k == 7) for k in range(8)], [win, aT], [pb])
                        return pb

                    def fm_qk_a():
                        if g > 0:
                            CP("pool", xqk.ap[:, :, 0:3], xqk.ap[:, :, 512:515], [xqk], [xqk])
                        for c in range(4):
                            pb = fm(c * 128)
                            CP("act", xqk.ap[:, c, 3:515], pb.ap[:], [pb], [xqk])

                    def fm_qk_b():
                        kt = ktmo.next()
                        for c in range(4):
                            pb = S.pbank()
                            MM([(pb.ap[:], dq.ap[:, c * 4 + j, :], xqk.ap[:, c, j:j + 512], j == 0, j == 3)
                                for j in range(4)], [dq, xqk], [pb])
                            qo = qko.next()
                            ACT(qo.ap[:], pb.ap[:], AF.Silu, [pb], [qo])
                            dstT = (mqT if c < 2 else mkT)
                            LD(dstT.ap[(c % 2) * 128:(c % 2 + 1) * 128, t0g:t0g + 512], qo.ap[:], qo, [qo], [dstT], q="pool")
                            if c >= 2:
                                pb2 = S.pbank()
                                pv2 = bview(pb2).rearrange("p (k n) -> p k n", k=8)
                                TRN([(pv2[:, j, :], qo.ap[:, j * 128:(j + 1) * 128]) for j in range(4)], [qo], [pb2])
                                CP("dve", kt.ap[:, :, (c - 2) * 128:(c - 1) * 128], pv2[:, 0:4, :], [pb2], [kt])
                        LD(mktm.ap[t0g:t0g + 512, :].rearrange("(j p) c -> p j c", p=128), kt.ap[:], kt, [kt], [mktm], q="pool")

                    def fm_cf_a():
                        if g > 0:
                            CP("pool", zbuf.ap[:, :, 0:30], zbuf.ap[:, :, 512:542], [zbuf], [zbuf])
                        for c in range(2):
                            pa = fm(1032 + c * 128)
                            pg = fm(1288 + c * 128)
                            sg = sgr.next()
                            ACT(sg.ap[:], pg.ap[:], AF.Sigmoid, [pg], [sg])
                            TT("dve", zbuf.ap[:, c, 30:542], pa.ap[:], sg.ap[:], ALU.mult, [pa, sg], [zbuf])

                    def fm_cf_b():
                        for c in range(2):
                            pb = S.pbank()
                            MM([(pb.ap[:], dc.ap[:, c * 31 + j, :], zbuf.ap[:, c, j:j + 512], j == 0, j == 30)
                                for j in range(31)], [dc, zbuf], [pb])
                            ACT(zc.ap[:, c, :], pb.ap[:], AF.Identity, [pb, cvec], [zc], bias=cvec.ap[:, 0, c:c + 1])
                            ACT(zc2.ap[:, c, :], zc.ap[:, c, :], AF.Square, [zc], [zc2])
                        pm = S.pbank()
                        MM([(pm.ap[:], onesln.ap[:], zc.ap[:, c, :], c == 0, c == 1) for c in range(2)], [onesln, zc], [pm])
                        pv_ = S.pbank()
                        MM([(pv_.ap[:], onesln.ap[:], zc2.ap[:, c, :], c == 0, c == 1) for c in range(2)], [onesln, zc2], [pv_])
                        CP("act", mean_sb.ap[:], pm.ap[:], [pm], [mean_sb])
                        TT("dve", var_sb.ap[:], mean_sb.ap[:], mean_sb.ap[:], ALU.mult, [mean_sb], [var_sb])
                        TT("dve", var_sb.ap[:], pv_.ap[:], var_sb.ap[:], ALU.subtract, [pv_, var_sb], [var_sb])
                        rsqrt_act(var_sb.ap[:], var_sb.ap[:], 1.0, [var_sb], [var_sb])
                        for c in range(2):
                            dt_ = dtmp.next()
                            TT("dve", dt_.ap[:], zc.ap[:, c, :], mean_sb.ap[:], ALU.subtract, [zc, mean_sb], [dt_])
                            TT("dve", dt_.ap[:], dt_.ap[:], var_sb.ap[:], ALU.mult, [dt_, var_sb], [dt_])
                            yo = yco.next()
                            ACT(yo.ap[:], dt_.ap[:], AF.Silu, [dt_, cvec], [yo], scale=cvec.ap[:, 1, c:c + 1],
                                bias=cvec.ap[:, 2, c:c + 1])
                            LD(yT.ap[256 + c * 128:256 + (c + 1) * 128, t0g:t0g + 512], yo.ap[:], yo, [yo], [yT], q="pool")

                    qT_ = qTo.next()
                    kT_ = kTo.next()

                    def tile_a(j):
                        t = g * 4 + j
                        r0 = t * 128
                        pvo = tm(j, 512, 512)
                        pif = tm(j, 1024, 8)
                        ga = gat.next()
                        TT("dve", ga.ap[:, 0:8], pif.ap[:, 0:8], bif.ap[:], ALU.add, [pif, bif], [ga])
                        ACT(ga.ap[:, 4:8], ga.ap[:, 4:8], AF.Exp, [ga], [ga], scale=-1.0)
                        ACT(ga.ap[:, 4:8], ga.ap[:, 4:8], AF.Ln, [ga], [ga], bias=oneb.ap[:, 0:1])
                        pq = tm(j, 1544, 512)
                        pk = tm(j, 2056, 512)
                        pvv = tm(j, 2568, 512)
                        v_ = vo.next()
                        CP("act", v_.ap[:], pvv.ap[:], [pvv], [v_])
                        LD(avd.ap[r0:r0 + 128, :], v_.ap[:], v_, [v_], [avd], q="pool")
                        sq_ = ssq.next()
                        for (pp_, off) in ((pq, 0), (pk, 8)):
                            sj = sqj.next()
                            ACT(sj.ap[:], pp_.ap[:], AF.Square, [pp_], [sj])
                            S.op("dve", (lambda sj=sj, off=off, sq_=sq_: lambda e: e.reduce_sum(
                                out=sq_.ap[:, off:off + 8], in_=sj.ap[:].rearrange("p (m d) -> p m d", m=8),
                                axis=AX.X))(), [sj], [sq_])
                        rsqrt_act(sq_.ap[:], sq_.ap[:], 1.0 / 64, [sq_], [sq_])
                        live = [pvo, pq, pk]
                        for bk in live:
                            S.reserved.add(S.banks.index(bk))
                        return (pvo, ga, pq, pk, sq_, live)

                    def tile_b2(j, st_):
                        (pvo, ga, pq, pk, sq_, live) = st_
                        t = g * 4 + j
                        r0 = t * 128
                        pc = S.pbank()
                        MM([(pc.ap[:, 0:4], triT.ap[:], ga.ap[:, 4:8], True, True),
                            (pc.ap[:, 4:8], selA.ap[:], ga.ap[:, 4:8], True, True),
                            (pc.ap[:, 8:12], selB.ap[:], ga.ap[:, 4:8], True, True)], [triT, selA, selB, ga], [pc])
                        ACT(A_tm.ap[:, t, :], pc.ap[:, 0:4], AF.Exp, [pc], [A_tm], scale=-1.0, bias=ln8b.ap[:, 0:1])
                        ACT(Gb.ap[:, 2 * t:2 * t + 2, :], pc.ap[:, 4:12].rearrange("p (a b) -> p a b", a=2), AF.Exp,
                            [pc], [Gb], scale=-1.0)
                        TT("dve", ga.ap[:, 8:12], ga.ap[:, 0:4], pc.ap[:, 0:4], ALU.add, [ga, pc], [ga])
                        ACT(ga.ap[:, 8:12], ga.ap[:, 8:12], AF.Exp, [ga], [ga])
                        rv = rvo.next()
                        TT("dve", rv.ap[:, :, 0:64], pvo.ap[:, 0:256].rearrange("p (h d) -> p h d", h=4),
                           ga.ap[:, 8:12].unsqueeze(2).to_broadcast([128, 4, 64]), ALU.mult, [pvo, ga], [rv])
                        CP("dve", rv.ap[:, :, 64:65], ga.ap[:, 8:12].unsqueeze(2), [ga], [rv])
                        LD(mrv.ap[r0:r0 + 128, :, :], rv.ap[:], rv, [rv], [mrv], q="pool")
                        so = soo.next()
                        ACT(so.ap[:], pvo.ap[:, 256:512], AF.Sigmoid, [pvo], [so])
                        LD(mso.ap[r0:r0 + 128, :], so.ap[:], so, [so], [mso], q="pool")
                        qbs = []
                        for (pp_, off, gg, dstT_) in ((pq, 0, gq, qT_), (pk, 8, gk, kT_)):
                            q1 = qn1.next()
                            TT("dve", q1.ap[:].rearrange("p (m d) -> p m d", m=8),
                               pp_.ap[:].rearrange("p (m d) -> p m d", m=8),
                               sq_.ap[:, off:off + 8].unsqueeze(2).to_broadcast([128, 8, 64]), ALU.mult,
                               [pp_, sq_], [q1])
                            qb = qnb.next()
                            TT("pool", qb.ap[:].rearrange("p (m d) -> p m d", m=8),
                               q1.ap[:].rearrange("p (m d) -> p m d", m=8),
                               gg.ap[:].unsqueeze(1).to_broadcast([128, 8, 64]), ALU.mult, [q1, gg], [qb])
                            qbs.append((qb, dstT_))
                        for bk in live:
                            S.reserved.discard(S.banks.index(bk))
                        return qbs

                    def tile_c(j, qbs):
                        for (qb, dstT_) in qbs:
                            pb = S.pbank()
                            pvw = bview(pb).rearrange("p (k n) -> p k n", k=8)
                            TRN([(pvw[:, m, :], qb.ap[:, m * 128:(m + 1) * 128]) for m in range(4)], [qb], [pb])
                            CP("act", dstT_.ap[:, :, j * 128:(j + 1) * 128], pvw[:, 0:4, :], [pb], [dstT_])

                    fm_qk_a()
                    st0 = tile_a(0)
                    fm_qk_b()
                    q0 = tile_b2(0, st0)
                    st1 = tile_a(1)
                    tile_c(0, q0)
                    fm_cf_a()
                    q1_ = tile_b2(1, st1)
                    st2 = tile_a(2)
                    tile_c(1, q1_)
                    fm_cf_b()
                    q2 = tile_b2(2, st2)
                    st3 = tile_a(3)
                    tile_c(2, q2)
                    q3 = tile_b2(3, st3)
                    tile_c(3, q3)
                    LD(aqT.ap[:, t0g:t0g + 512].rearrange("(m p) s -> p m s", p=128), qT_.ap[:], qT_, [qT_], [aqT], q="pool")
                    LD(akT.ap[:, t0g:t0g + 512].rearrange("(m p) s -> p m s", p=128), kT_.ap[:], kT_, [kT_], [akT], q="pool")

        def phase_B(L):
            with S.scope():
                prm = S.sbuf("prmB", [128, 1], F32, dma=True)
                gm = S.sbuf("gmB", [128, 256], F32)
                LDS([(gm.ap[:], I["m_out_norm_g"][L].partition_broadcast(128))], prm, [], [gm])
                qTh = S.sbuf("qTh", [64, S_tok], BF16, dma=True)
                kTh = S.sbuf("kTh", [64, S_tok], BF16, dma=True)
                ktm = S.sbuf("ktmB", [128, NT, 64], BF16, dma=True)
                rvbd = S.sbuf("rvbd", [128, NT, 130], BF16, dma=True)
                soh = S.sbuf("soh", [128, NT, 64], BF16, dma=True)
                gso = S.sbuf("gso", [128, NT, 64], F32)
                X = S.sbuf("Xst", [64, NCH, 65], F32)
                Cst = S.sbuf("Cst", [64, NCH + 1, 65], BF16)
                yTm = S.sbuf("yTm", [64, S_tok], BF16, dma=True)
                smt = S.ring("smt", [128, 128], BF16, 8)
                ndr = S.ring("ndr", [128, 65], F32, 8)
                sm = S.ring("smB", [128, 8], F32, 8)
                jk = S.ring("jkB", [128, 64], F32, 4)
                ybr = S.ring("ybB", [128, 64], BF16, 8)
                MSET("pool", rvbd.ap[:], 0.0, [rvbd])
                MSET("pool", Cst.ap[:, 0, :], 0.0, [Cst])
                for h in range(4):
                    LD(qTh.ap[:], mqT.ap[h * 64:(h + 1) * 64, :], qTh, [mqT], [qTh])
                    LD(kTh.ap[:], mkT.ap[h * 64:(h + 1) * 64, :], kTh, [mkT], [kTh])
                    LDS([(ktm.ap[:], mktm.ap[:, h * 64:(h + 1) * 64].rearrange("(i p) c -> p i c", p=128))],
                        ktm, [mktm], [ktm], slow=True)
                    mv = mrv.ap.rearrange("(i two p) h c -> two p i h c", two=2, p=64)
                    LDS([(rvbd.ap[0:64, :, 0:65], mv[0][:, :, h, :]),
                         (rvbd.ap[64:128, :, 65:130], mv[1][:, :, h, :])], rvbd, [mrv], [rvbd], slow=True)
                    LDS([(soh.ap[:], mso.ap[:, h * 64:(h + 1) * 64].rearrange("(i p) c -> p i c", p=128))],
                        soh, [mso], [soh], slow=True)
                    TT("pool", gso.ap[:], soh.ap[:], gm.ap[:, h * 64:(h + 1) * 64].unsqueeze(1).to_broadcast([128, NT, 64]),
                       ALU.mult, [soh, gm], [gso])
                    for i in range(NT):
                        pb = S.pbank()
                        MM([(pb.ap[0:64, 0:130], ktm.ap[:, i, :], rvbd.ap[:, i, :], True, True)], [ktm, rvbd], [pb])
                        ACT(X.ap[:, 2 * i, :], pb.ap[0:64, 0:65], AF.Copy, [pb, Gb], [X], scale=Gb.ap[0:64, 2 * i, h:h + 1])
                        ACT(X.ap[:, 2 * i + 1, :], pb.ap[0:64, 65:130], AF.Copy, [pb, Gb], [X],
                            scale=Gb.ap[0:64, 2 * i + 1, h:h + 1])
                    for c in range(1, NCH):
                        STT("dve", X.ap[:, c, :], X.ap[:, c - 1, :], Gb.ap[0:64, c, h:h + 1], X.ap[:, c, :],
                            ALU.mult, ALU.add, [X, Gb], [X])
                    CP("act", Cst.ap[:, 1:NCH + 1, :], X.ap[:, :, :], [X], [Cst])
                    W = 4
                    for i0 in range(0, NT, W):
                        tiles = list(range(i0, min(NT, i0 + W)))
                        pss, sms, pos, nds, sss, ybs = {}, {}, {}, {}, {}, {}
                        for i in tiles:
                            ts_ = slice(i * 128, (i + 1) * 128)
                            pss[i] = S.pbank()
                            MM([(pss[i].ap[:, 0:128], kTh.ap[:, ts_], qTh.ap[:, ts_], True, True)], [kTh, qTh], [pss[i]])
                        for i in tiles:
                            sms[i] = smt.next()
                            TT("dve", sms[i].ap[:], pss[i].ap[:, 0:128], triT.ap[:], ALU.mult, [pss[i], triT], [sms[i]])
                        for i in tiles:
                            ts_ = slice(i * 128, (i + 1) * 128)
                            pos[i] = S.pbank()
                            MM([(pos[i].ap[:, 0:130], qTh.ap[:, ts_],
                                 Cst.ap[:, 2 * i:2 * i + 2, :].rearrange("p a b -> p (a b)"), True, False),
                                (pos[i].ap[:, 0:130], sms[i].ap[:], rvbd.ap[:, i, :], False, True)],
                               [qTh, Cst, sms[i], rvbd], [pos[i]])
                        for i in tiles:
                            nd = nds[i] = ndr.next()
                            ACT(nd.ap[0:64, :], pos[i].ap[0:64, 0:65], AF.Copy, [pos[i], A_tm], [nd],
                                scale=A_tm.ap[0:64, i, h:h + 1])
                            ACT(nd.ap[64:128, :], pos[i].ap[64:128, 65:130], AF.Copy, [pos[i], A_tm], [nd],
                                scale=A_tm.ap[64:128, i, h:h + 1])
                        for i in tiles:
                            s_ = sss[i] = sm.next()
                            ACT(s_.ap[:, 0:1], nds[i].ap[:, 64:65], AF.Abs, [nds[i]], [s_])
                        for i in tiles:
                            s_ = sss[i]
                            TS("dve", s_.ap[:, 0:1], s_.ap[:, 0:1], 1.0, None, ALU.max, None, [s_], [s_])
                            S.op("dve", (lambda s_=s_: lambda e: e.reciprocal(out=s_.ap[:, 1:2], in_=s_.ap[:, 0:1]))(), [s_], [s_])
                        for i in tiles:
                            s_ = sss[i]
                            j_ = jk.next()
                            ACT(j_.ap[:], nds[i].ap[:, 0:64], AF.Square, [nds[i], s_], [j_, s_], scale=s_.ap[:, 1:2],
                                accum_out=s_.ap[:, 2:3])
                        for i in tiles:
                            s_ = sss[i]
                            ACT(s_.ap[:, 3:4], s_.ap[:, 2:3], AF.Ln, [s_], [s_], scale=1.0 / 64, bias=epsb.ap[:, 0:1])
                        for i in tiles:
                            s_ = sss[i]
                            ACT(s_.ap[:, 3:4], s_.ap[:, 3:4], AF.Exp, [s_], [s_], scale=-0.5)
                        for i in tiles:
                            s_ = sss[i]
                            TT("dve", s_.ap[:, 4:5], s_.ap[:, 3:4], s_.ap[:, 1:2], ALU.mult, [s_], [s_])
                            yb = ybs[i] = ybr.next()
                            STT("dve", yb.ap[:], nds[i].ap[:, 0:64], s_.ap[:, 4:5], gso.ap[:, i, :], ALU.mult, ALU.mult,
                                [nds[i], s_, gso], [yb])
                        for i in tiles:
                            ts_ = slice(i * 128, (i + 1) * 128)
                            pt = S.pbank()
                            ptv = bview(pt)
                            TRN([(ptv[0:64, 0:128], ybs[i].ap[:, :])], [ybs[i]], [pt])
                            CP("act", yTm.ap[:, ts_], ptv[0:64, 0:128], [pt], [yTm])
                    LD(yT.ap[h * 64:(h + 1) * 64, :], yTm.ap[:], yTm, [yTm], [yT], q="pool")

        def phase_C(L, bg=None, bg_every=4):
            lam_init = 0.8 - 0.6 * math.exp(-0.3 * L)
            with S.scope():
                prm = S.sbuf("prmC", [128, 1], F32, dma=True)
                lv = S.sbuf("lvC", [128, 4, 64], F32)
                gsub = S.sbuf("gsub", [128, 128], F32)
                lam = S.sbuf("lam", [128, 4], F32)
                ljk = S.sbuf("ljk", [128, 64], F32)
                LDS([(lv.ap[:, 0, :], I["a_lambda_q1"][L].partition_broadcast(128)),
                     (lv.ap[:, 1, :], I["a_lambda_k1"][L].partition_broadcast(128)),
                     (lv.ap[:, 2, :], I["a_lambda_q2"][L].partition_broadcast(128)),
                     (lv.ap[:, 3, :], I["a_lambda_k2"][L].partition_broadcast(128)),
                     (gsub.ap[:], I["a_subln_g"][L].partition_broadcast(128))], prm, [], [lv, gsub])
                MSET("dve", lam.ap[:], 0.0, [lam])
                S.op("dve", lambda e: e.tensor_tensor(out=ljk.ap[:], in0=lv.ap[:, 0, :], in1=lv.ap[:, 1, :], op=ALU.mult),
                     [lv], [ljk])
                S.op("dve", lambda e: e.reduce_sum(out=lam.ap[:, 0:1], in_=ljk.ap[:], axis=AX.X), [ljk], [lam])
                S.op("dve", lambda e: e.tensor_tensor(out=ljk.ap[:], in0=lv.ap[:, 2, :], in1=lv.ap[:, 3, :], op=ALU.mult),
                     [lv, lam], [ljk])
                S.op("dve", lambda e: e.reduce_sum(out=lam.ap[:, 1:2], in_=ljk.ap[:], axis=AX.X), [ljk], [lam])
                ACT(lam.ap[:, 0:2], lam.ap[:, 0:2], AF.Exp, [lam], [lam])
                TT("dve", lam.ap[:, 2:3], lam.ap[:, 0:1], lam.ap[:, 1:2], ALU.subtract, [lam], [lam])
                TS("dve", lam.ap[:, 3:4], lam.ap[:, 2:3], lam_init, None, ALU.add, None, [lam], [lam])
                TS("dve", gsub.ap[:], gsub.ap[:], 1.0 - lam_init, None, ALU.mult, None, [gsub], [gsub])

                KT = S.ring("KTC", [128, S_tok], BF16, 2, dma=True)
                VV = S.ring("VVC", [128, NT, 129], BF16, 2, dma=True)
                QT = S.ring("QTC", [128, 512], BF16, 3, dma=True)
                PT = S.ring("PTC", [128, 512], BF16, 6)
                yta = S.ring("ytaC", [128, 512], BF16, 2, dma=True)
                sm = S.ring("smC", [128, 8], F32, 4)
                t2r = S.ring("t2C", [128, 128], F32, 2)
                yar = S.ring("yaC", [128, 128], F32, 2)
                jkr = S.ring("jkC", [128, 128], BF16, 2)
                ybr = S.ring("ybC", [128, 128], BF16, 2)
                for vb in VV.bufs:
                    MSET("pool", vb.ap[:, :, 128:129], 1.0, [vb])
                accb = S.reserve(3)

                def acc(m, j):
                    i_ = m * 4 + j
                    return accb[i_ // 3], (i_ % 3) * 132

                accs_r = S.ring("accsC", [128, 1056], F32, 2)
                fin_sm = S.ring("finsm", [128, 16], F32, 3)
                o_r = S.ring("oC", [128, 8, 128], F32, 2)
                ya_r = S.ring("yaC2", [128, 4, 128], F32, 2)
                sq_r = S.ring("sqC", [128, 4, 128], F32, 2)
                yb_r = S.ring("ybC2", [128, 4, 128], BF16, 2)
                deferred = []

                def finalize(h, qg):
                    ac = accs_r.next()
                    s_ = fin_sm.next()
                    o = o_r.next()
                    ya = ya_r.next()
                    sq = sq_r.next()
                    yb = yb_r.next()
                    yt = yta.next()
                    for bi in range(3):
                        w = 396 if bi < 2 else 264
                        CP("dve", ac.ap[:, bi * 396:bi * 396 + w], accb[bi].ap[:, 0:w], [accb[bi]], [ac])
                    acv = ac.ap[:].rearrange("p (i c) -> p i c", c=132)
                    S.op("dve", lambda e: e.reciprocal(out=s_.ap[:, 0:8].unsqueeze(2), in_=acv[:, :, 128:129]), [ac], [s_])
                    TS("dve", s_.ap[:, 4:8], s_.ap[:, 4:8], lam.ap[:, 3:4], None, ALU.mult, None, [s_, lam], [s_])
                    TT("dve", o.ap[:], acv[:, :, 0:128], s_.ap[:, 0:8].unsqueeze(2).to_broadcast([128, 8, 128]), ALU.mult,
                       [ac, s_], [o])
                    TT("dve", ya.ap[:], o.ap[:, 0:4, :], o.ap[:, 4:8, :], ALU.subtract, [o], [ya])
                    TT("dve", sq.ap[:], ya.ap[:], ya.ap[:], ALU.mult, [ya], [sq])
                    S.op("dve", lambda e: e.reduce_sum(out=s_.ap[:, 8:12], in_=sq.ap[:], axis=AX.X), [sq, s_], [s_])

                    def F2():
                        rsqrt_act(s_.ap[:, 12:16], s_.ap[:, 8:12], 1.0 / 128, [s_], [s_])

                    def F3():
                        TT("dve", sq.ap[:], ya.ap[:], s_.ap[:, 12:16].unsqueeze(2).to_broadcast([128, 4, 128]), ALU.mult,
                           [ya, s_, sq], [sq])
                        TT("dve", yb.ap[:], sq.ap[:], gsub.ap[:].unsqueeze(1).to_broadcast([128, 4, 128]), ALU.mult,
                           [sq, gsub], [yb])
                        pt_ = S.pbank()
                        ptv = bview(pt_).rearrange("p (k n) -> p k n", k=8)
                        TRN([(ptv[:, j, :], yb.ap[:, j, :]) for j in range(4)], [yb], [pt_])
                        CP("dve", yt.ap[:].rearrange("p (j n) -> p j n", j=4), ptv[:, 0:4, :], [pt_], [yt])
                        LD(yT.ap[512 + h * 128:512 + (h + 1) * 128, qg * 512:(qg + 1) * 512], yt.ap[:], yt, [yt], [yT],
                           q="pool")
                    deferred.append([3, F2])
                    deferred.append([6, F3])

                def tick(flush=False):
                    for d_ in list(deferred):
                        d_[0] -= 1
                        if d_[0] <= 0 or flush:
                            deferred.remove(d_)
                            d_[1]()

                def stage1(h, qg, kb, kt, vv, qt):
                    jj = max(0, kb - 4 * qg)
                    c0_ = jj * 128
                    di = kb - 4 * qg + NT
                    pts = []
                    for m in range(2):
                        pb = S.pbank()
                        MM([(pb.ap[:, c0_:512], kt.ap[m * 64:(m + 1) * 64, kb * 128:(kb + 1) * 128],
                             qt.ap[m * 64:(m + 1) * 64, c0_:512], True, True)], [kt, qt], [pb])
                        pt = PT.next()
                        ACT(pt.ap[:, c0_:512], pb.ap[:, c0_:512], AF.Exp, [pb, abt], [pt], scale=0.125,
                            bias=abt.ap[:, h, di:di + 1])
                        if kb >= 4 * qg:
                            TT("pool", pt.ap[:, c0_:c0_ + 128], pt.ap[:, c0_:c0_ + 128], triU.ap[:], ALU.mult,
                               [pt, triU], [pt])
                        pts.append(pt)

                    def stage2():
                        items = []
                        for m in range(2):
                            for j in range(jj, 4):
                                bk, off = acc(m, j)
                                items.append((bk.ap[:, off:off + 129], pts[m].ap[:, j * 128:(j + 1) * 128],
                                              vv.ap[:, kb, :], kb == 0 and off == 0, kb == 4 * qg + j, True))
                        MM(items, pts + [vv], accb)
                        if kb == 4 * qg + 3:
                            finalize(h, qg)
                    return stage2

                pending = None
                ucount = 0
                for h in range(4):
                    kt = KT.next()
                    vv = VV.next()
                    LD(kt.ap[:], akT.ap[h * 128:(h + 1) * 128, :], kt, [akT], [kt])
                    LDS([(vv.ap[:, :, 0:128], avd.ap[:, h * 128:(h + 1) * 128].rearrange("(i p) c -> p i c", p=128))],
                        vv, [avd], [vv])
                    for qg in range(NG):
                        qt = QT.next()
                        LD(qt.ap[:], aqT.ap[h * 128:(h + 1) * 128, qg * 512:(qg + 1) * 512], qt, [aqT], [qt])
                        for kb in range(4 * qg + 4):
                            s2 = stage1(h, qg, kb, kt, vv, qt)
                            if pending is not None:
                                pending()
                            pending = s2
                            ucount += 1
                            tick()
                            if bg is not None and ucount % bg_every == 0:
                                next(bg, None)
                pending()
                tick(flush=True)
                S.release(accb)

        def phase_D(L, hsrc, hdst):
            moe = (L % 2 == 1)
            with S.scope():
                prm = S.sbuf("prmD", [128, 1], F32, dma=True)
                gff = S.sbuf("gffD", [128, 1024], F32)
                gpl = S.sbuf("gplD", [128, 1024], F32)
                LDS([(gff.ap[:], I["ffn_norm_g"][L].partition_broadcast(128)),
                     (gpl.ap[:], I["ple_norm_g"][L].partition_broadcast(128))], prm, [], [gff, gpl])
                wpp = S.sbuf("wppD", [128, 2, 1024], BF16, dma=True)
                LD(wpp.ap[:], Wb["w_ple_proj"].ap[L].rearrange("(k p) n -> p k n", p=128), wpp, [Wb["w_ple_proj"]], [wpp])
                if moe:
                    wrt = S.sbuf("wrtD", [128, 8, 8], F32, dma=True)
                    LDS([(wrt.ap[:], I["router_w"][0].rearrange("(k p) n -> p k n", p=128))], wrt, [], [wrt])
                wblk = S.ring("wblk", [128, 8, 512], BF16, 4, dma=True)
                wdblk = S.ring("wdblk", [128, 4, 512], BF16, 3, dma=True)
                hin = S.ring("hinD", [128, 1024], F32, 5, dma=True)
                hw = [S.sbuf("hw%d" % j, [128, 1024], F32, dma=True) for j in range(4)]
                yTg = S.ring("yTg", [128, 8, 512], BF16, 2, dma=True)
                junk = S.ring("junkD", [128, 1024], BF16, 2)
                ssr = S.ring("ssD", [128, 4], F32, 2)
                cbf = S.ring("cbfD", [128, 1024], BF16, 2)
                cT = S.sbuf("cTD", [128, 8, 512], BF16)
                FT = (FFE if moe else FFD) // 128
                hT = S.sbuf("hTD", [128, FT, 512], BF16)
                sgr = S.ring("sgD", [128, 512], F32, 3)
                pin = S.ring("pinD", [128, 256], F32, 4, dma=True)
                pbf = S.ring("pbfD", [128, 256], BF16, 2)
                pTt = S.sbuf("pTD", [128, 2, 512], BF16)
                gsb = S.ring("gsbD", [128, 512], F32, 2)
                if moe:
                    cf32 = S.ring("cf32", [128, 1024], F32, 2)
                    cTf = S.ring("cTf", [128, 8, 128], F32, 2)
                    rl = S.ring("rlD", [128, 8], F32, 2)
                    mx8 = S.ring("mx8", [128, 8], F32, 2)
                    rex = S.ring("rexD", [128, 8], F32, 2)
                    rsm = S.ring("rsmD", [128, 4], F32, 2)
                    comb = [S.sbuf("comb%d" % j, [128, 8], F32) for j in range(4)]

                def norm_T(gb_, dst, extra=None):
                    ss = ssr.next()
                    for j in range(4):
                        jk = junk.next()
                        ACT(jk.ap[:], hw[j].ap[:], AF.Square, [hw[j]], [jk, ss], accum_out=ss.ap[:, j:j + 1])
                    rsqrt_act(ss.ap[:], ss.ap[:], 1.0 / 1024, [ss], [ss])
                    for j in range(4):
                        cb = cbf.next()
                        STT("dve", cb.ap[:], hw[j].ap[:], ss.ap[:, j:j + 1], gb_.ap[:], ALU.mult, ALU.mult,
                            [hw[j], ss, gb_], [cb])
                        pb = S.pbank()
                        pv = bview(pb).rearrange("p (k n) -> p k n", k=8)
                        TRN([(pv[:, k, :], cb.ap[:, k * 128:(k + 1) * 128]) for k in range(8)], [cb], [pb])
                        CP("act" if j % 2 == 0 else "dve", dst.ap[:, :, j * 128:(j + 1) * 128], pv, [pb], [dst])
                        if extra is not None:
                            extra(j, ss)

                def ffn_expert(wg_ap, wu_ap, wd_ap, F_, scale_cols):
                    nfb = (F_ + 511) // 512
                    wgv = wg_ap.rearrange("(k p) n -> p k n", p=128)
                    wuv = wu_ap.rearrange("(k p) n -> p k n", p=128)
                    wdv = wd_ap.rearrange("(f p) n -> p f n", p=128)
                    for fb in range(nfb):
                        fw = min(512, F_ - fb * 512)
                        wg = wblk.next()
                        LD(wg.ap[:, :, 0:fw], wgv[:, :, fb * 512:fb * 512 + fw], wg, [WSRC], [wg])
                        wu = wblk.next()
                        LD(wu.ap[:, :, 0:fw], wuv[:, :, fb * 512:fb * 512 + fw], wu, [WSRC], [wu])
                        for ft in range(fw // 128):
                            f = fb * 4 + ft
                            pg = S.pbank()
                            MM([(pg.ap[:], wg.ap[:, k, ft * 128:(ft + 1) * 128], cT.ap[:, k, :], k == 0, k == 7)
                                for k in range(8)], [wg, cT], [pg])
                            pu = S.pbank()
                            MM([(pu.ap[:], wu.ap[:, k, ft * 128:(ft + 1) * 128], cT.ap[:, k, :], k == 0, k == 7)
                                for k in range(8)], [wu, cT], [pu])
                            sg = sgr.next()
                            ACT(sg.ap[:], pg.ap[:], AF.Silu, [pg], [sg])
                            TT("dve", hT.ap[:, f, :], pu.ap[:], sg.ap[:], ALU.mult, [pu, sg], [hT])
                    nft = F_ // 128
                    for half in range(2):
                        accs = [S.pbank() for _ in range(4)]
                        for fb in range(nfb):
                            nf = min(4, nft - fb * 4)
                            wd = wdblk.next()
                            LD(wd.ap[:, 0:nf, :], wdv[:, fb * 4:fb * 4 + nf, half * 512:(half + 1) * 512], wd, [WSRC], [wd])
                            items = []
                            for j in range(4):
                                for ft in range(nf):
                                    f = fb * 4 + ft
                                    items.append((accs[j].ap[:], hT.ap[:, f, j * 128:(j + 1) * 128], wd.ap[:, ft, :],
                                                  f == 0, f == nft - 1))
                            MM(items, [hT, wd], accs)
                        for j in range(4):
                            hs = hw[j].ap[:, half * 512:(half + 1) * 512]
                            if scale_cols is None:
                                TT("dve", hs, accs[j].ap[:], hs, ALU.add, [accs[j], hw[j]], [hw[j]])
                            else:
                                STT("dve", hs, accs[j].ap[:], scale_cols[j], hs, ALU.mult, ALU.add,
                                    [accs[j], hw[j]] + comb, [hw[j]])

                WSRC = Buf("wsrc_all")
                for g in range(NG):
                    t0g = g * 512
                    yg = yTg.next()
                    LD(yg.ap[:], yT.ap[:, t0g:t0g + 512].rearrange("(k p) s -> p k s", p=128), yg, [yT], [yg])
                    hbs = []
                    for j in range(4):
                        hb = hin.next()
                        hbs.append(hb)
                        LD(hb.ap[:], hsrc.ap[t0g + j * 128:t0g + (j + 1) * 128, :], hb, [hsrc], [hb])
                    wos = []
                    for half in range(2):
                        wo = wblk.next()
                        LD(wo.ap[:], Wb["w_out"].ap[L].rearrange("(k p) n -> p k n", p=128)[:, :, half * 512:(half + 1) * 512],
                           wo, [WSRC], [wo])
                        wos.append(wo)
                    for j in range(4):
                        for half in range(2):
                            pb = S.pbank()
                            MM([(pb.ap[:], yg.ap[:, k, j * 128:(j + 1) * 128], wos[half].ap[:, k, :], k == 0, k == 7)
                                for k in range(8)], [yg, wos[half]], [pb])
                            TT("dve", hw[j].ap[:, half * 512:(half + 1) * 512], pb.ap[:],
                               hbs[j].ap[:, half * 512:(half + 1) * 512], ALU.add, [pb, hbs[j]], [hw[j]])
                    if not moe:
                        norm_T(gff, cT)
                        ffn_expert(Wb["dense_w_gate"].ap[0], Wb["dense_w_up"].ap[0], Wb["dense_w_down"].ap[0], FFD, None)
                    else:
                        def router(j, ss):
                            cf = cf32.next()
                            STT("dve", cf.ap[:], hw[j].ap[:], ss.ap[:, j:j + 1], gff.ap[:], ALU.mult, ALU.mult,
                                [hw[j], ss, gff], [cf])
                            ct = cTf.next()
                            for kk in range(2):
                                pb = S.pbank()
                                pv = pb.ap[:].rearrange("p (k n) -> p k n", k=4)

                                def f(e, pv=pv, cf=cf, kk=kk):
                                    r = None
                                    for k in range(4):
                                        r = e.transpose(out=pv[:, k, :], in_=cf.ap[:, (kk * 4 + k) * 128:(kk * 4 + k + 1) * 128],
                                                        identity=identf.ap[:])
                                    return r
                                S.op("pe", f, [cf, identf], [pb])
                                CP("act", ct.ap[:, kk * 4:kk * 4 + 4, :], pv, [pb], [ct])
                            pl = S.pbank()
                            MM([(pl.ap[:, 0:8], ct.ap[:, k, :], wrt.ap[:, k, :], k == 0, k == 7) for k in range(8)],
                               [ct, wrt], [pl])
                            lg = rl.next()
                            CP("act", lg.ap[:], pl.ap[:, 0:8], [pl], [lg])
                            m8 = mx8.next()
                            S.op("dve", (lambda m8=m8, lg=lg: lambda e: e.max(out=m8.ap[:], in_=lg.ap[:]))(), [lg], [m8])
                            ex = rex.next()
                            r4 = rsm.next()
                            TS("dve", r4.ap[:, 0:1], m8.ap[:, 0:1], -1.0, None, ALU.mult, None, [m8], [r4])
                            ACT(ex.ap[:], lg.ap[:], AF.Exp, [lg, r4], [ex], bias=r4.ap[:, 0:1])
                            ACT(r4.ap[:, 1:2], m8.ap[:, 1:2], AF.Exp, [m8, r4], [r4], bias=r4.ap[:, 0:1])
                            TS("dve", r4.ap[:, 1:2], r4.ap[:, 1:2], 1.0, None, ALU.add, None, [r4], [r4])
                            S.op("dve", (lambda r4=r4: lambda e: e.reciprocal(out=r4.ap[:, 2:3], in_=r4.ap[:, 1:2]))(), [r4], [r4])
                            TS("dve", comb[j].ap[:], lg.ap[:], m8.ap[:, 1:2], None, ALU.is_ge, None, [lg, m8], [comb[j]])
                            TT("dve", comb[j].ap[:], comb[j].ap[:], ex.ap[:], ALU.mult, [ex, comb[j]], [comb[j]])
                            TS("dve", comb[j].ap[:], comb[j].ap[:], r4.ap[:, 2:3], None, ALU.mult, None, [r4, comb[j]], [comb[j]])
                        norm_T(gff, cT, router)
                        for ex_ in range(nexp):
                            ffn_expert(Wb["moe_w_gate"].ap[0][ex_], Wb["moe_w_up"].ap[0][ex_], Wb["moe_w_down"].ap[0][ex_],
                                       FFE, [comb[j].ap[:, ex_:ex_ + 1] for j in range(4)])
                    norm_T(gpl, cT)
                    for j in range(4):
                        pi_ = pin.next()
                        LD(pi_.ap[:], I["p"][L][t0g + j * 128:t0g + (j + 1) * 128, :], pi_, [], [pi_])
                        pb_ = pbf.next()
                        CP("pool", pb_.ap[:], pi_.ap[:], [pi_], [pb_])
                        pk_ = S.pbank()
                        pv = bview(pk_).rearrange("p (k n) -> p k n", k=8)
                        TRN([(pv[:, k, :], pb_.ap[:, k * 128:(k + 1) * 128]) for k in range(2)], [pb_], [pk_])
                        CP("act", pTt.ap[:, :, j * 128:(j + 1) * 128], pv[:, 0:2, :], [pk_], [pTt])
                    wgs = []
                    for half in range(2):
                        wo = wblk.next()
                        LD(wo.ap[:], Wb["w_ple_gate"].ap[L].rearrange("(k p) n -> p k n", p=128)[:, :, half * 512:(half + 1) * 512],
                           wo, [WSRC], [wo])
                        wgs.append(wo)
                    for j in range(4):
                        for half in range(2):
                            pg = S.pbank()
                            MM([(pg.ap[:], cT.ap[:, k, j * 128:(j + 1) * 128], wgs[half].ap[:, k, :], k == 0, k == 7)
                                for k in range(8)], [cT, wgs[half]], [pg])
                            pp2 = S.pbank()
                            MM([(pp2.ap[:], pTt.ap[:, k, j * 128:(j + 1) * 128], wpp.ap[:, k, half * 512:(half + 1) * 512],
                                 k == 0, k == 1) for k in range(2)], [pTt, wpp], [pp2])
                            gs = gsb.next()
                            ACT(gs.ap[:], pg.ap[:], AF.Sigmoid, [pg], [gs])
                            TT("dve", gs.ap[:], pp2.ap[:], gs.ap[:], ALU.mult, [pp2, gs], [gs])
                            hs = hw[j].ap[:, half * 512:(half + 1) * 512]
                            TT("dve", hs, hs, gs.ap[:], ALU.add, [gs, hw[j]], [hw[j]])
                        LD(hdst.ap[t0g + j * 128:t0g + (j + 1) * 128, :], hw[j].ap[:], hw[j], [hw[j]], [hdst], q="pool")


        def phase_D_moe(L, hsrc, hdst):
            NTILE = 2 * NG + 8
            NSLOT = NTILE * 512
            hmid = S.dram("hmid", [S_tok, D], F32)
            csd = S.dram("csd", [S_tok, D], BF16)
            xsd = S.dram("xsd", [NSLOT, D], BF16)
            ysd = S.dram("ysd", [NSLOT, D], F32)
            WSRC = Buf("wsrc_all2")
            with S.scope():
                E1 = S.sbuf("E1t", [128, NT, 8], F32)
                E2 = S.sbuf("E2t", [128, NT, 8], F32)
                POS = S.sbuf("POSt", [128, NT, 8], F32)
                GT = S.sbuf("GTt", [128, NT, 2], F32)
                carry = S.sbuf("carry", [128, 8], F32)
                DSTi = S.sbuf("DSTi", [128, NT, 2], I32)
                IGi = S.sbuf("IGi", [128, NTILE, 7], I32)
                IDi = S.sbuf("IDi", [128, NTILE, 14], I32)
                triS = S.sbuf("triS", [128, 128], F32)
                ones128 = S.sbuf("ones128", [128, 128], F32)

                def mk(e):
                    e.memset(triS.ap[:], 1.0)
                    e.affine_select(out=triS.ap[:], in_=triS.ap[:], compare_op=ALU.is_ge, fill=0.0,
                                    base=-1, pattern=[[1, 128]], channel_multiplier=-1)
                    e.memset(carry.ap[:], 0.0)
                    return e.memset(ones128.ap[:], 1.0)
                S.op("pool", mk, (), [triS, carry, ones128])

                with S.scope():
                    prm = S.sbuf("prmM", [128, 1], F32, dma=True)
                    gff = S.sbuf("gffM", [128, 1024], F32)
                    wrt = S.sbuf("wrtM", [128, 8, 8], F32)
                    LDS([(gff.ap[:], I["ffn_norm_g"][L].partition_broadcast(128)),
                         (wrt.ap[:], I["router_w"][0].rearrange("(k p) n -> p k n", p=128))], prm, [], [gff, wrt])
                    wblk = S.ring("wblkM", [128, 8, 512], BF16, 2, dma=True)
                    hin = S.ring("hinM", [128, 1024], F32, 5, dma=True)
                    hw = S.ring("hwM", [128, 1024], F32, 6, dma=True)
                    yTg = S.ring("yTgM", [128, 8, 512], BF16, 2, dma=True)
                    junk = S.ring("junkM", [128, 1024], BF16, 2)
                    ssr = S.ring("ssM", [128, 4], F32, 4)
                    cbf = S.ring("cbfM", [128, 1024], BF16, 3, dma=True)
                    cf32 = S.ring("cf32M", [128, 1024], F32, 2)
                    cTf = S.ring("cTfM", [128, 8, 128], F32, 2)
                    rl = S.ring("rlM", [128, 8], F32, 3)
                    mx8 = S.ring("mx8M", [128, 8], F32, 3)
                    rsm = S.ring("rsmM", [128, 4], F32, 3)
                    selr = S.ring("selM", [128, 8], F32, 3)
                    wos = []
                    for half in range(2):
                        wo = wblk.next()
                        LD(wo.ap[:], Wb["w_out"].ap[L].rearrange("(k p) n -> p k n", p=128)[:, :, half * 512:(half + 1) * 512],
                           wo, [WSRC], [wo])
                        wos.append(wo)
                    for g in range(NG):
                        t0g = g * 512
                        yg = yTg.next()
                        LD(yg.ap[:], yT.ap[:, t0g:t0g + 512].rearrange("(k p) s -> p k s", p=128), yg, [yT], [yg])
                        for j in range(4):
                            t = g * 4 + j
                            r0 = t * 128
                            hb = hin.next()
                            LD(hb.ap[:], hsrc.ap[r0:r0 + 128, :], hb, [hsrc], [hb])
                            hwj = hw.next()
                            for half in range(2):
                                pb = S.pbank()
                                MM([(pb.ap[:], yg.ap[:, k, j * 128:(j + 1) * 128], wos[half].ap[:, k, :], k == 0, k == 7)
                                    for k in range(8)], [yg, wos[half]], [pb])
                                TT("dve", hwj.ap[:, half * 512:(half + 1) * 512], pb.ap[:],
                                   hb.ap[:, half * 512:(half + 1) * 512], ALU.add, [pb, hb], [hwj])
                            LD(hmid.ap[r0:r0 + 128, :], hwj.ap[:], hwj, [hwj], [hmid], q="pool")
                            ss = ssr.next()
                            jk = junk.next()
                            ACT(jk.ap[:], hwj.ap[:], AF.Square, [hwj], [jk, ss], accum_out=ss.ap[:, 0:1])
                            rsqrt_act(ss.ap[:, 0:1], ss.ap[:, 0:1], 1.0 / 1024, [ss], [ss])
                            cb = cbf.next()
                            STT("dve", cb.ap[:], hwj.ap[:], ss.ap[:, 0:1], gff.ap[:], ALU.mult, ALU.mult, [hwj, ss, gff], [cb])
                            LD(csd.ap[r0:r0 + 128, :], cb.ap[:], cb, [cb], [csd], q="pool")
                            cf = cf32.next()
                            STT("dve", cf.ap[:], hwj.ap[:], ss.ap[:, 0:1], gff.ap[:], ALU.mult, ALU.mult, [hwj, ss, gff], [cf])
                            ct = cTf.next()
                            for kk in range(2):
                                pb = S.pbank()
                                pv = pb.ap[:].rearrange("p (k n) -> p k n", k=4)

                                def f(e, pv=pv, cf=cf, kk=kk):
                                    r = None
                                    for k in range(4):
                                        r = e.transpose(out=pv[:, k, :], in_=cf.ap[:, (kk * 4 + k) * 128:(kk * 4 + k + 1) * 128],
                                                        identity=identf.ap[:])
                                    return r
                                S.op("pe", f, [cf, identf], [pb])
                                CP("act", ct.ap[:, kk * 4:kk * 4 + 4, :], pv, [pb], [ct])
                            pl = S.pbank()
                            MM([(pl.ap[:, 0:8], ct.ap[:, k, :], wrt.ap[:, k, :], k == 0, k == 7) for k in range(8)],
                               [ct, wrt], [pl])
                            lg = rl.next()
                            CP("act", lg.ap[:], pl.ap[:, 0:8], [pl], [lg])
                            m8 = mx8.next()
                            S.op("dve", (lambda m8=m8, lg=lg: lambda e: e.max(out=m8.ap[:], in_=lg.ap[:]))(), [lg], [m8])
                            TS("dve", E1.ap[:, t, :], lg.ap[:], m8.ap[:, 0:1], None, ALU.is_equal, None, [lg, m8], [E1])
                            TS("dve", E2.ap[:, t, :], lg.ap[:], m8.ap[:, 1:2], None, ALU.is_equal, None, [lg, m8], [E2])
                            sel = selr.next()
                            TT("dve", sel.ap[:], E1.ap[:, t, :], E2.ap[:, t, :], ALU.add, [E1, E2], [sel])
                            r4 = rsm.next()
                            TS("dve", r4.ap[:, 0:1], m8.ap[:, 0:1], -1.0, None, ALU.mult, None, [m8], [r4])
                            ACT(r4.ap[:, 1:2], m8.ap[:, 1:2], AF.Exp, [m8, r4], [r4], bias=r4.ap[:, 0:1])
                            TS("dve", r4.ap[:, 1:2], r4.ap[:, 1:2], 1.0, None, ALU.add, None, [r4], [r4])
                            S.op("dve", (lambda r4=r4, t=t: lambda e: e.reciprocal(out=GT.ap[:, t, 0:1], in_=r4.ap[:, 1:2]))(),
                                 [r4], [GT])
                            TS("dve", GT.ap[:, t, 1:2], GT.ap[:, t, 0:1], -1.0, 1.0, ALU.mult, ALU.add, [GT], [GT])
                            pp = S.pbank()
                            MM([(pp.ap[:, 0:8], triS.ap[:], sel.ap[:], True, True),
                                (pp.ap[:, 8:16], ones128.ap[:], sel.ap[:], True, True)], [triS, ones128, sel], [pp])
                            TT("dve", POS.ap[:, t, :], pp.ap[:, 0:8], carry.ap[:], ALU.add, [pp, carry], [POS])
                            TT("dve", carry.ap[:], pp.ap[:, 8:16], carry.ap[:], ALU.add, [pp, carry], [carry])

                with S.scope():
                    ci = S.sbuf("ciM", [128, 8], I32)
                    ntf = S.sbuf("ntfM", [128, 8], F32)
                    cum = S.sbuf("cumM", [128, 8], F32)
                    base = S.sbuf("baseM", [128, 8], F32)
                    iot_i = S.sbuf("iotiM", [128, NTILE], I32)
                    iot = S.sbuf("iotM", [128, NTILE], F32)
                    eid = S.sbuf("eidM", [128, NTILE], F32)
                    tmpe = S.sbuf("tmpeM", [128, NTILE], F32)
                    pb_i = S.sbuf("pbiM", [128, 14], I32)
                    pbf_ = S.sbuf("pbfM", [128, 14], F32)
                    igf = S.sbuf("igfM", [128, NTILE, 14], F32)
                    tmp3 = S.sbuf("tmp3M", [128, NT, 8], F32)
                    tmp4 = S.sbuf("tmp4M", [128, NT, 8], F32)
                    dstf = S.sbuf("dstfM", [128, NT, 2], F32)
                    TS("dve", ntf.ap[:], carry.ap[:], 511.0, None, ALU.add, None, [carry], [ntf])
                    CP("dve", ci.ap[:], ntf.ap[:], [ntf], [ci])
                    S.op("dve", lambda e: e.tensor_single_scalar(out=ci.ap[:], in_=ci.ap[:], scalar=9, op=ALU.arith_shift_right),
                         [ci], [ci])
                    CP("dve", ntf.ap[:], ci.ap[:], [ci], [ntf])
                    CP("dve", cum.ap[:, 0:1], ntf.ap[:, 0:1], [ntf], [cum])
                    for e_ in range(1, 8):
                        TT("dve", cum.ap[:, e_:e_ + 1], cum.ap[:, e_ - 1:e_], ntf.ap[:, e_:e_ + 1], ALU.add, [cum, ntf], [cum])
                    TT("dve", base.ap[:], cum.ap[:], ntf.ap[:], ALU.subtract, [cum, ntf], [base])
                    TS("dve", base.ap[:], base.ap[:], 512.0, None, ALU.mult, None, [base], [base])

                    def mk2(e):
                        e.iota(iot_i.ap[:], pattern=[[1, NTILE]], base=0, channel_multiplier=0)
                        return e.iota(pb_i.ap[:], pattern=[[128, 14]], base=0, channel_multiplier=1)
                    S.op("pool", mk2, (), [iot_i, pb_i])
                    CP("dve", iot.ap[:], iot_i.ap[:], [iot_i], [iot])
                    CP("dve", pbf_.ap[:], pb_i.ap[:], [pb_i], [pbf_])
                    MSET("dve", eid.ap[:], 0.0, [eid])
                    for e_ in range(8):
                        TS("dve", tmpe.ap[:], iot.ap[:], cum.ap[:, e_:e_ + 1], None, ALU.is_ge, None, [iot, cum], [tmpe])
                        TT("dve", eid.ap[:], eid.ap[:], tmpe.ap[:], ALU.add, [eid, tmpe], [eid])
                    TS("dve", eid.ap[:], eid.ap[:], 7.0, None, ALU.min, None, [eid], [eid])
                    TS("dve", tmpe.ap[:], eid.ap[:], 896.0, None, ALU.mult, None, [eid], [tmpe])
                    TT("dve", igf.ap[:, :, 0:7], tmpe.ap[:].unsqueeze(2).to_broadcast([128, NTILE, 7]),
                       pbf_.ap[:, 0:7].unsqueeze(1).to_broadcast([128, NTILE, 7]), ALU.add, [tmpe, pbf_], [igf])
                    CP("dve", IGi.ap[:], igf.ap[:, :, 0:7], [igf], [IGi])
                    TS("dve", tmpe.ap[:], eid.ap[:], 1792.0, None, ALU.mult, None, [eid, igf], [tmpe])
                    TT("dve", igf.ap[:], tmpe.ap[:].unsqueeze(2).to_broadcast([128, NTILE, 14]),
                       pbf_.ap[:].unsqueeze(1).to_broadcast([128, NTILE, 14]), ALU.add, [tmpe, pbf_, IGi], [igf])
                    CP("dve", IDi.ap[:], igf.ap[:], [igf], [IDi])
                    TT("dve", tmp3.ap[:], POS.ap[:], base.ap[:].unsqueeze(1).to_broadcast([128, NT, 8]), ALU.add,
                       [POS, base], [tmp3])
                    for (k_, Ek) in ((0, E1), (1, E2)):
                        TT("dve", tmp4.ap[:], tmp3.ap[:], Ek.ap[:], ALU.mult, [tmp3, Ek], [tmp4])
                        S.op("dve", (lambda k_=k_: lambda e: e.reduce_sum(out=dstf.ap[:, :, k_:k_ + 1], in_=tmp4.ap[:], axis=AX.X))(),
                             [tmp4, dstf], [dstf])
                    CP("dve", DSTi.ap[:], dstf.ap[:], [dstf], [DSTi])

                with S.scope():
                    cbt = S.ring("cbtM", [128, 1024], BF16, 4, dma=True)
                    for t in range(NT):
                        cb = cbt.next()
                        LD(cb.ap[:], csd.ap[t * 128:(t + 1) * 128, :], cb, [csd], [cb])
                        for k_ in range(2):
                            S.dma((lambda cb=cb, t=t, k_=k_: lambda e: [e.indirect_dma_start(
                                out=xsd.ap[:, :], out_offset=bass.IndirectOffsetOnAxis(ap=DSTi.ap[:, t, k_:k_ + 1], axis=0),
                                in_=cb.ap[:, :], in_offset=None)])(),
                                cb, [cb, DSTi], [xsd], q="pool")

                with S.scope():
                    wblk = S.ring("wblkE", [128, 8, 512], BF16, 4, dma=True)
                    wdblk = S.ring("wdblkE", [128, 4, 512], BF16, 4, dma=True)
                    xtr = S.ring("xtE", [128, 4, 1024], BF16, 2, dma=True)
                    cTr = S.ring("cTE", [128, 8, 512], BF16, 2)
                    hT = S.sbuf("hTE", [128, 28, 512], BF16)
                    sgr = S.ring("sgE", [128, 512], F32, 3)
                    yor = S.ring("yoE", [128, 1024], F32, 8, dma=True)

                    def gather(dst_ap, srcbuf, idx_ap, semb, nrows):
                        S.dma(lambda e: [e.indirect_dma_start(
                            out=dst_ap, out_offset=None, in_=srcbuf.ap[:, :],
                            in_offset=bass.IndirectOffsetOnAxis(ap=idx_ap, axis=0))], semb, [WSRC, IGi, IDi], [semb], q="pool")

                    for i in range(NTILE):
                        xt = xtr.next()
                        LD(xt.ap[:], xsd.ap[i * 512:(i + 1) * 512, :].rearrange("(j p) d -> p j d", p=128), xt, [xsd], [xt])
                        cT = cTr.next()
                        for j in range(4):
                            pb = S.pbank()
                            pv = bview(pb).rearrange("p (k n) -> p k n", k=8)
                            TRN([(pv[:, k, :], xt.ap[:, j, k * 128:(k + 1) * 128]) for k in range(8)], [xt], [pb])
                            CP("act" if j % 2 == 0 else "dve", cT.ap[:, :, j * 128:(j + 1) * 128], pv, [pb], [cT])
                        for fb in range(7):
                            wg = wblk.next()
                            gather(wg.ap[:].rearrange("p k c -> p (k c)"), Wb["moe_w_gate"], IGi.ap[:, i, fb:fb + 1], wg, 8 * 7 * 128)
                            wu = wblk.next()
                            gather(wu.ap[:].rearrange("p k c -> p (k c)"), Wb["moe_w_up"], IGi.ap[:, i, fb:fb + 1], wu, 8 * 7 * 128)
                            for ft in range(4):
                                f = fb * 4 + ft
                                pg = S.pbank()
                                MM([(pg.ap[:], wg.ap[:, k, ft * 128:(ft + 1) * 128], cT.ap[:, k, :], k == 0, k == 7)
                                    for k in range(8)], [wg, cT], [pg])
                                pu = S.pbank()
                                MM([(pu.ap[:], wu.ap[:, k, ft * 128:(ft + 1) * 128], cT.ap[:, k, :], k == 0, k == 7)
                                    for k in range(8)], [wu, cT], [pu])
                                sg = sgr.next()
                                ACT(sg.ap[:], pg.ap[:], AF.Silu, [pg], [sg])
                                TT("dve", hT.ap[:, f, :], pu.ap[:], sg.ap[:], ALU.mult, [pu, sg], [hT])
                        yos = [yor.next() for _ in range(4)]
                        for half in range(2):
                            accs = [S.pbank() for _ in range(4)]
                            for fb in range(7):
                                wd = wdblk.next()
                                gather(wd.ap[:].rearrange("p k c -> p (k c)"), Wb["moe_w_down"],
                                       IDi.ap[:, i, half * 7 + fb:half * 7 + fb + 1], wd, 8 * 2 * 7 * 128)
                                items = []
                                for j in range(4):
                                    for ft in range(4):
                                        f = fb * 4 + ft
                                        items.append((accs[j].ap[:], hT.ap[:, f, j * 128:(j + 1) * 128], wd.ap[:, ft, :],
                                                      f == 0, f == 27))
                                MM(items, [hT, wd], accs)
                            for j in range(4):
                                CP("act" if j % 2 == 0 else "dve", yos[j].ap[:, half * 512:(half + 1) * 512], accs[j].ap[:],
                                   [accs[j]], [yos[j]])
                        for j in range(4):
                            r0 = i * 512 + j * 128
                            LD(ysd.ap[r0:r0 + 128, :], yos[j].ap[:], yos[j], [yos[j]], [ysd], q="sp")

                with S.scope():
                    prm = S.sbuf("prmP", [128, 1], F32, dma=True)
                    gpl = S.sbuf("gplP", [128, 1024], F32)
                    LDS([(gpl.ap[:], I["ple_norm_g"][L].partition_broadcast(128))], prm, [], [gpl])
                    wpp = S.sbuf("wppP", [128, 2, 1024], BF16, dma=True)
                    LD(wpp.ap[:], Wb["w_ple_proj"].ap[L].rearrange("(k p) n -> p k n", p=128), wpp, [Wb["w_ple_proj"]], [wpp])
                    wgs = []
                    wgr = S.ring("wgP", [128, 8, 512], BF16, 2, dma=True)
                    for half in range(2):
                        wo = wgr.next()
                        LD(wo.ap[:], Wb["w_ple_gate"].ap[L].rearrange("(k p) n -> p k n", p=128)[:, :, half * 512:(half + 1) * 512],
                           wo, [WSRC], [wo])
                        wgs.append(wo)
                    hw = S.ring("hwP", [128, 1024], F32, 8, dma=True)
                    y12 = S.ring("y12P", [128, 1024], F32, 6, dma=True)
                    junk = S.ring("junkP", [128, 1024], BF16, 2)
                    ssr = S.ring("ssP", [128, 4], F32, 3)
                    cbf = S.ring("cbfP", [128, 1024], BF16, 2)
                    cTr = S.ring("cTP", [128, 8, 512], BF16, 2)
                    pin = S.ring("pinP", [128, 256], F32, 4, dma=True)
                    pbf = S.ring("pbfP", [128, 256], BF16, 2)
                    pTr = S.ring("pTP", [128, 2, 512], BF16, 2)
                    gsb = S.ring("gsbP", [128, 512], F32, 3)
                    for g in range(NG):
                        t0g = g * 512
                        hws = []
                        cT = cTr.next()
                        pTt = pTr.next()
                        ss = ssr.next()
                        for j in range(4):
                            t = g * 4 + j
                            r0 = t * 128
                            hwj = hw.next()
                            hws.append(hwj)
                            LD(hwj.ap[:], hmid.ap[r0:r0 + 128, :], hwj, [hmid], [hwj])
                            for k_ in range(2):
                                yk = y12.next()
                                S.dma((lambda yk=yk, t=t, k_=k_: lambda e: [e.indirect_dma_start(
                                    out=yk.ap[:, :], out_offset=None, in_=ysd.ap[:, :],
                                    in_offset=bass.IndirectOffsetOnAxis(ap=DSTi.ap[:, t, k_:k_ + 1], axis=0))])(), yk, [ysd, DSTi], [yk], q="pool")
                                STT("dve", hwj.ap[:], yk.ap[:], GT.ap[:, t, k_:k_ + 1], hwj.ap[:], ALU.mult, ALU.add,
                                    [yk, GT, hwj], [hwj])
                            jk = junk.next()
                            ACT(jk.ap[:], hwj.ap[:], AF.Square, [hwj], [jk, ss], accum_out=ss.ap[:, j:j + 1])
                        rsqrt_act(ss.ap[:], ss.ap[:], 1.0 / 1024, [ss], [ss])
                        for j in range(4):
                            cb = cbf.next()
                            STT("dve", cb.ap[:], hws[j].ap[:], ss.ap[:, j:j + 1], gpl.ap[:], ALU.mult, ALU.mult,
                                [hws[j], ss, gpl], [cb])
                            pb = S.pbank()
                            pv = bview(pb).rearrange("p (k n) -> p k n", k=8)
                            TRN([(pv[:, k, :], cb.ap[:, k * 128:(k + 1) * 128]) for k in range(8)], [cb], [pb])
                            CP("act" if j % 2 == 0 else "dve", cT.ap[:, :, j * 128:(j + 1) * 128], pv, [pb], [cT])
                            pi_ = pin.next()
                            LD(pi_.ap[:], I["p"][L][t0g + j * 128:t0g + (j + 1) * 128, :], pi_, [], [pi_])
                            pb_ = pbf.next()
                            CP("pool", pb_.ap[:], pi_.ap[:], [pi_], [pb_])
                            pk_ = S.pbank()
                            pv2 = bview(pk_).rearrange("p (k n) -> p k n", k=8)
                            TRN([(pv2[:, k, :], pb_.ap[:, k * 128:(k + 1) * 128]) for k in range(2)], [pb_], [pk_])
                            CP("act", pTt.ap[:, :, j * 128:(j + 1) * 128], pv2[:, 0:2, :], [pk_], [pTt])
                        for j in range(4):
                            for half in range(2):
                                pg = S.pbank()
                                MM([(pg.ap[:], cT.ap[:, k, j * 128:(j + 1) * 128], wgs[half].ap[:, k, :], k == 0, k == 7)
                                    for k in range(8)], [cT, wgs[half]], [pg])
                                pp2 = S.pbank()
                                MM([(pp2.ap[:], pTt.ap[:, k, j * 128:(j + 1) * 128], wpp.ap[:, k, half * 512:(half + 1) * 512],
                                     k == 0, k == 1) for k in range(2)], [pTt, wpp], [pp2])
                                gs = gsb.next()
                                ACT(gs.ap[:], pg.ap[:], AF.Sigmoid, [pg], [gs])
                                TT("dve", gs.ap[:], pp2.ap[:], gs.ap[:], ALU.mult, [pp2, gs], [gs])
                                hs = hws[j].ap[:, half * 512:(half + 1) * 512]
                                TT("dve", hs, hs, gs.ap[:], ALU.add, [gs, hws[j]], [hws[j]])
                            LD(hdst.ap[t0g + j * 128:t0g + (j + 1) * 128, :], hws[j].ap[:], hws[j], [hws[j]], [hdst], q="sp")

        conv_list = []
        conv_moe = []
        for L in layers:
            conv_list += [("w_in", (L,)), ("w_out", (L,)), ("w_ple_gate", (L,)), ("w_ple_proj", (L,))]
            if L % 2 == 0:
                conv_list += [("dense_w_gate", (0,)), ("dense_w_up", (0,)), ("dense_w_down", (0,))]
            else:
                for ex_ in range(nexp):
                    conv_moe += [("moe_w_gate", (0, ex_)), ("moe_w_up", (0, ex_)), ("moe_w_down", (0, ex_))]
        bg_ok = (len(layers) == 2 and "C" not in skip and "V" not in skip)
        if not bg_ok:
            conv_list += conv_moe
        if 'V' not in skip:
            convert(conv_list)
        hcur = IN["x"]
        for li, L in enumerate(layers):
            hnext = OUT if li == len(layers) - 1 else h1
            if "A" not in skip:
                phase_A(L, hcur)
            if "B" not in skip:
                phase_B(L)
            if "C" not in skip:
                if bg_ok and li == 0:
                    with S.scope():
                        n_chunks = nexp * (8 + 8 + 28)
                        n_units = 4 * sum(4 * q + 4 for q in range(NG))
                        gen = convert_gen(conv_moe, convert_bufs(), engs=("dve", "pool"))
                        phase_C(L, bg=gen, bg_every=max(1, n_units // (n_chunks + 8)))
                        for _ in gen:
                            pass
                else:
                    phase_C(L)
            if "yT" in taps and li == len(layers) - 1:
                break
            if "D" not in skip:
                if sparse and L % 2 == 1:
                    phase_D_moe(L, hcur, hnext)
                else:
                    phase_D(L, hcur, hnext)
            hcur = hnext
        if "yT" in taps:
            tp = tap_aps["yT"]
            with S.scope():
                tb = S.sbuf("tapb", [128, 8, S_tok], BF16, dma=True)
                LD(tb.ap[:], yT.ap.rearrange("(k p) s -> p k s", p=128), tb, [yT], [tb])
                LD(tp.rearrange("(k p) s -> p k s", p=128), tb.ap[:], tb, [tb], [OUT])
        S.barrier()
        S.emit(block)
    return nc


_CACHE = {}


def kernel(**inputs):
    x = np.asarray(inputs["x"], dtype=np.float32)
    B, S_tok, _ = x.shape
    p = np.asarray(inputs["p"], dtype=np.float32)
    key = S_tok
    if key not in _CACHE:
        _CACHE[key] = build(S_tok)
    nc = _CACHE[key]
    shared = {name: np.ascontiguousarray(np.asarray(inputs[name], dtype=np.float32)) for name, _ in PARAMS}
    ncores = 8
    active = [0, 1, 4, 5][:B] if B <= 4 else list(range(B))
    zeros = None
    in_maps = []
    for c in range(ncores):
        if c in active:
            b = active.index(c)
            m = dict(shared)
            m["x"] = np.ascontiguousarray(x[b])
            m["p"] = np.ascontiguousarray(p[:, b])
        else:
            if zeros is None:
                zeros = {name: np.zeros(shp, np.float32) for name, shp in PARAMS}
                zeros["x"] = np.zeros((S_tok, D), np.float32)
                zeros["p"] = np.zeros((2, S_tok, 256), np.float32)
            m = zeros
        in_maps.append(m)
    res = run_bass_kernel_spmd(nc, in_maps, core_ids=list(range(ncores)))
    out = np.stack([np.asarray(res.results[active[b]]["out"], dtype=np.float32) for b in range(B)], axis=0)
    return out.astype(np.float32)
```
